# Optimizing a Trainium2 kernel written in Bass

```python
import jax
import jax.numpy as jnp
from jax import lax
import numpy as np

D_MODEL = 1024
BATCH = 8
SEQ = 8192
DEPTH = 2

N_META = 16
HG_HEADS = 4
HG_DK = 128
HG_DV = 128
HG_CHUNK = 64
AT_HEADS = 8
AT_DH = 64
AT_QRANK = 256
IDX_HEADS = 4
IDX_DIM = 64
TOPK_MAX = 256
Q_BLOCK = 128
CV_CH = 512
CV_WIDTH = 31
FF_DIM = 2816
FF_CONV = 3
N_BRANCH = 3
EPS = 1e-6
MASK_BIG = 1e30

IN_SPLITS = (
    HG_HEADS * HG_DK,
    HG_HEADS * HG_DK,
    HG_HEADS * HG_DV,
    HG_HEADS * HG_DV,
    AT_QRANK,
    AT_DH,
    AT_DH,
    IDX_DIM,
    IDX_HEADS,
    2 * CV_CH,
    N_BRANCH * D_MODEL,
)
N_IN = 2 * HG_HEADS * HG_DK + 2 * HG_HEADS * HG_DV + AT_QRANK + 2 * AT_DH + IDX_DIM + IDX_HEADS + 2 * CV_CH + N_BRANCH * D_MODEL

kernel_name = 'hybrid_hgrn2_dsa_conformer_block'


def rms_norm(x, g):
    x32 = x.astype(jnp.float32)
    y = x32 * lax.rsqrt(jnp.mean(x32 * x32, axis=-1, keepdims=True) + EPS)
    return (y * g.astype(jnp.float32)).astype(x.dtype)


def layer_norm(x, g, b):
    x32 = x.astype(jnp.float32)
    mu = jnp.mean(x32, axis=-1, keepdims=True)
    xc = x32 - mu
    y = xc * lax.rsqrt(jnp.mean(xc * xc, axis=-1, keepdims=True) + EPS)
    return (y * g.astype(jnp.float32) + b.astype(jnp.float32)).astype(x.dtype)


def causal_dwconv(x, w, b):
    K = w.shape[0]
    y = lax.conv_general_dilated(
        x, w[:, None, :].astype(x.dtype), window_strides=(1,), padding=((K - 1, 0),),
        dimension_numbers=('NWC', 'WIO', 'NWC'), feature_group_count=x.shape[-1])
    return y + b.astype(x.dtype)


def _split(z, sizes):
    cuts = np.cumsum(np.array(sizes))[:-1].tolist()
    return jnp.split(z, cuts, axis=-1)


def hgrn_lower_bounds(p):
    p = jax.nn.softmax(p.astype(jnp.float32), axis=0)
    return jnp.cumsum(p, axis=0) - p[0]


def hgrn2_mixer(q_raw, f_raw, i_raw, g_raw, lb, norm_g, w_out):
    B, T, _ = q_raw.shape
    dt = q_raw.dtype
    H, DK, DV, C = HG_HEADS, HG_DK, HG_DV, HG_CHUNK
    f = lb + (1.0 - lb) * jax.nn.sigmoid(f_raw.astype(jnp.float32))
    log_f = jnp.log(f)
    k = 1.0 - f
    q = jax.nn.silu(q_raw.astype(jnp.float32)) * DK ** -0.5
    v = i_raw.astype(jnp.float32)
    pad_front = (-N_META) % C
    pad_back = (-(pad_front + T)) % C
    n_chunks = (pad_front + T + pad_back) // C

    def to_chunks(a, d):
        a = a.reshape(B, T, H, d).transpose(0, 2, 1, 3)
        a = jnp.pad(a, ((0, 0), (0, 0), (pad_front, pad_back), (0, 0)))
        return jnp.moveaxis(a.reshape(B, H, n_chunks, C, d), 2, 0)

    qc, kc, lfc, vc = to_chunks(q, DK), to_chunks(k, DK), to_chunks(log_f, DK), to_chunks(v, DV)
    tri = jnp.tril(jnp.ones((C, C), dtype=bool))

    def step(S, inp):
        q_, k_, lf_, v_ = inp
        b = jnp.cumsum(lf_, axis=2)
        diff = b[:, :, :, None, :] - b[:, :, None, :, :]
        decay = jnp.where(tri[:, :, None], jnp.exp(jnp.minimum(diff, 0.0)), 0.0)
        att = jnp.einsum('bhtd,bhtsd,bhsd->bhts', q_, decay, k_)
        o = (jnp.einsum('bhts,bhsv->bhtv', att, v_)
             + jnp.einsum('bhtd,bhdv->bhtv', q_ * jnp.exp(b), S))
        b_last = b[:, :, -1:, :]
        S = (jnp.exp(b_last[:, :, 0, :])[..., None] * S
             + jnp.einsum('bhsd,bhsv->bhdv', k_ * jnp.exp(b_last - b), v_))
        return S, o

    S0 = jnp.zeros((B, H, DK, DV), jnp.float32)
    _, o = lax.scan(step, S0, (qc, kc, lfc, vc))
    o = jnp.moveaxis(o, 0, 2).reshape(B, H, n_chunks * C, DV)[:, :, pad_front:pad_front + T]
    o = o.transpose(0, 2, 1, 3)
    o = rms_norm(o, norm_g) * jax.nn.silu(g_raw.astype(jnp.float32).reshape(B, T, H, DV))
    return o.reshape(B, T, H * DV).astype(dt) @ w_out


def dsa_mixer(cq, k_raw, v, k_idx, w_idx, cq_norm_g, w_uq, w_qi, q_norm_g, k_norm_g, w_out):
    B, T, _ = cq.shape
    dt = cq.dtype
    n_sel = min(TOPK_MAX, (T - N_META) // 4)
    cqn = rms_norm(cq, cq_norm_g)
    q = rms_norm((cqn @ w_uq).reshape(B, T, AT_HEADS, AT_DH), q_norm_g)
    qi = (cqn @ w_qi).reshape(B, T, IDX_HEADS, IDX_DIM)
    k = rms_norm(k_raw, k_norm_g)
    wts = w_idx * (IDX_HEADS * IDX_DIM) ** -0.5
    scale = AT_DH ** -0.5
    kidx32 = k_idx.astype(jnp.float32)
    kpos = jnp.arange(T, dtype=jnp.int32)
    pad_front = (-N_META) % Q_BLOCK
    pad_back = (-(pad_front + T)) % Q_BLOCK
    Tq = pad_front + T + pad_back
    n_blk = Tq // Q_BLOCK

    def to_blocks(a):
        a = jnp.pad(a, ((0, 0), (pad_front, pad_back)) + ((0, 0),) * (a.ndim - 2))
        return jnp.moveaxis(a.reshape((B, n_blk, Q_BLOCK) + a.shape[2:]), 1, 0)

    qpos = (jnp.arange(Tq, dtype=jnp.int32) - pad_front).reshape(n_blk, Q_BLOCK)

    def block(args):
        qb, qib, wb, pb = args
        s = jax.nn.relu(jnp.einsum('bqhd,bsd->bqhs', qib.astype(jnp.float32), kidx32))
        score = jnp.einsum('bqhs,bqh->bqs', s, wb.astype(jnp.float32))
        causal = kpos[None, :] <= pb[:, None]
        score = jnp.where(causal, jnp.where(kpos[None, :] < N_META, MASK_BIG, score), -MASK_BIG)
        _, idx = lax.top_k(score, n_sel)
        kg = jax.vmap(lambda kb, ib: kb[ib])(k, idx)
        vg = jax.vmap(lambda vb, ib: vb[ib])(v, idx)
        logits = jnp.einsum('bqhd,bqkd->bqhk', qb, kg).astype(jnp.float32) * scale
        valid = idx <= pb[None, :, None]
        logits = jnp.where(valid[:, :, None, :], logits, -MASK_BIG)
        p = jax.nn.softmax(logits, axis=-1).astype(dt)
        return jnp.einsum('bqhk,bqkd->bqhd', p, vg)

    o = lax.map(block, (to_blocks(q), to_blocks(qi), to_blocks(wts), qpos))
    o = jnp.moveaxis(o, 0, 1).reshape(B, Tq, AT_HEADS * AT_DH)[:, pad_front:pad_front + T]
    return o @ w_out


def conformer_conv_mixer(u, dw_w, dw_b, ln_g, ln_b, w_out):
    a, g = jnp.split(u, 2, axis=-1)
    h = a * jax.nn.sigmoid(g)
    h = causal_dwconv(h, dw_w, dw_b)
    h = jax.nn.silu(layer_norm(h, ln_g, ln_b))
    return h @ w_out


def conv_ffn(x, w_up, dw_w, dw_b, w_down):
    h = causal_dwconv(x @ w_up, dw_w, dw_b)
    a, b = jnp.split(h, 2, axis=-1)
    return (jax.nn.silu(a) * b) @ w_down


def setup_inputs(seed: int = 0) -> dict:
    key = jax.random.key(seed)
    ks = jax.random.split(key, 24)
    f32 = jnp.float32
    L = DEPTH

    def w(k, shape, fan_in):
        return jax.random.normal(k, shape, f32) * fan_in ** -0.5

    def gain(k, shape):
        return 1.0 + 0.01 * jax.random.normal(k, shape, f32)

    def bias(k, shape):
        return 0.01 * jax.random.normal(k, shape, f32)

    return {
        'x': jax.random.normal(ks[0], (BATCH, SEQ, D_MODEL), f32),
        'meta_tokens': jax.random.normal(ks[1], (N_META, D_MODEL), f32),
        'hgrn_lb': 0.5 * jax.random.normal(ks[2], (L, HG_HEADS * HG_DK), f32),
        'norm1_g': gain(ks[3], (L, D_MODEL)),
        'w_in': w(ks[4], (L, D_MODEL, N_IN), D_MODEL),
        'hg_norm_g': gain(ks[5], (L, HG_DV)),
        'w_hg_out': w(ks[6], (L, HG_HEADS * HG_DV, D_MODEL), HG_HEADS * HG_DV),
        'cq_norm_g': gain(ks[7], (L, AT_QRANK)),
        'w_uq': w(ks[8], (L, AT_QRANK, AT_HEADS * AT_DH), AT_QRANK),
        'w_qi': w(ks[9], (L, AT_QRANK, IDX_HEADS * IDX_DIM), AT_QRANK),
        'q_norm_g': gain(ks[10], (L, AT_DH)),
        'k_norm_g': gain(ks[11], (L, AT_DH)),
        'w_at_out': w(ks[12], (L, AT_HEADS * AT_DH, D_MODEL), AT_HEADS * AT_DH),
        'cv_dw_w': w(ks[13], (L, CV_WIDTH, CV_CH), CV_WIDTH),
        'cv_dw_b': bias(ks[14], (L, CV_CH)),
        'cv_ln_g': gain(ks[15], (L, CV_CH)),
        'cv_ln_b': bias(ks[16], (L, CV_CH)),
        'w_cv_out': w(ks[17], (L, CV_CH, D_MODEL), CV_CH),
        'w_mix_out': w(ks[18], (L, D_MODEL, D_MODEL), D_MODEL),
        'norm2_g': gain(ks[19], (L, D_MODEL)),
        'w_ffn_up': w(ks[20], (L, D_MODEL, 2 * FF_DIM), D_MODEL),
        'ffn_dw_w': w(ks[21], (L, FF_CONV, 2 * FF_DIM), FF_CONV),
        'ffn_dw_b': bias(ks[22], (L, 2 * FF_DIM)),
        'w_ffn_down': w(ks[23], (L, FF_DIM, D_MODEL), FF_DIM),
    }


def reference(x, meta_tokens, hgrn_lb, norm1_g, w_in, hg_norm_g, w_hg_out, cq_norm_g, w_uq, w_qi,
              q_norm_g, k_norm_g, w_at_out, cv_dw_w, cv_dw_b, cv_ln_g, cv_ln_b, w_cv_out, w_mix_out,
              norm2_g, w_ffn_up, ffn_dw_w, ffn_dw_b, w_ffn_down):
    B = x.shape[0]
    meta = jnp.broadcast_to(meta_tokens.astype(x.dtype)[None], (B, N_META, D_MODEL))
    h = jnp.concatenate([meta, x], axis=1)
    lbs = hgrn_lower_bounds(hgrn_lb)
    for l in range(DEPTH):
        xn = rms_norm(h, norm1_g[l])
        z = xn @ w_in[l]
        (q_hg, f_hg, i_hg, g_hg, cq, k_at, v_at, k_ix, w_ix, u_cv, gate_logits) = _split(z, IN_SPLITS)
        y_hg = hgrn2_mixer(q_hg, f_hg, i_hg, g_hg, lbs[l], hg_norm_g[l], w_hg_out[l])
        y_at = dsa_mixer(cq, k_at, v_at, k_ix, w_ix, cq_norm_g[l], w_uq[l], w_qi[l],
                         q_norm_g[l], k_norm_g[l], w_at_out[l])
        y_cv = conformer_conv_mixer(u_cv, cv_dw_w[l], cv_dw_b[l], cv_ln_g[l], cv_ln_b[l], w_cv_out[l])
        g1, g2, g3 = jnp.split(jax.nn.sigmoid(gate_logits), N_BRANCH, axis=-1)
        h = h + (g1 * y_hg + g2 * y_at + g3 * y_cv) @ w_mix_out[l]
        h = h + conv_ffn(rms_norm(h, norm2_g[l]), w_ffn_up[l], ffn_dw_w[l], ffn_dw_b[l], w_ffn_down[l])
    return h[:, N_META:]
```

```python
from contextlib import ExitStack

import numpy as np

import concourse.bass as bass
import concourse.mybir as mybir
from concourse.bass_utils import run_bass_kernel_spmd

F32 = mybir.dt.float32
BF16 = mybir.dt.bfloat16
AF = mybir.ActivationFunctionType
ALU = mybir.AluOpType
AX = mybir.AxisListType

SEM_LIMIT = 30000
D = 1024
NIN = 6596
FF = 2816
EPS = 1e-6
CQ_, CF_, CI_, CG_, CCQ_, CKA_, CVA_, CKI_, CWI_, CU_, CGT_ = 0, 512, 1024, 1536, 2048, 2304, 2368, 2432, 2496, 2500, 3524
ZQ, ZF, ZG, ZCQ, ZU, ZGT, ZROWS = 0, 512, 1024, 1536, 1792, 2816, 5888
NIT = 14
NEG = -30000.0
KSEL = 240


class Ev:
    __slots__ = ("sem", "val", "eng")

    def __init__(self, sem, val, eng):
        self.sem = sem
        self.val = val
        self.eng = eng


class Buf:
    def __init__(self, name):
        self.name = name
        self.w = None
        self.r = {}


class Sched:
    def __init__(self, nc, es):
        self.nc = nc
        self.es = es
        self.eng = {"pe": nc.tensor, "act": nc.scalar, "dve": nc.vector,
                    "pool": nc.gpsimd, "sp": nc.sync}
        self.sem = {}
        self.cnt = {}
        self.nsem = 0
        for e in self.eng:
            self._new_sem(e)
        self.waited = {e: {} for e in self.eng}
        self.dsems = {}
        self.all_dsems = []
        self.n_inst = 0

    def _alloc_sem(self, name):
        self.nsem += 1
        return self.es.enter_context(self.nc.semaphore(f"{name}_{self.nsem}"))

    def _new_sem(self, e):
        self.sem[e] = self._alloc_sem("s" + e)
        self.cnt[e] = 0

    def _wait(self, e, ev):
        if ev is None:
            return
        key = id(ev.sem)
        if self.waited[e].get(key, 0) >= ev.val:
            return
        self.eng[e].wait_ge(ev.sem, ev.val)
        self.waited[e][key] = ev.val
        self.n_inst += 1

    def _deps(self, e, reads, writes, is_dma=False):
        for b in reads:
            ev = b.w
            if ev is None:
                continue
            if ev.eng == e and e == "pe" and not is_dma:
                continue
            self._wait(e, ev)
        for b in writes:
            ev = b.w
            if ev is not None and (ev.eng != e or is_dma):
                self._wait(e, ev)
            for rev in b.r.values():
                if rev.eng != e or is_dma:
                    self._wait(e, rev)

    def op(self, e, fn, reads=(), writes=()):
        self._deps(e, reads, writes)
        inst = fn(self.eng[e])
        if self.cnt[e] >= SEM_LIMIT:
            self._new_sem(e)
        inst.then_inc(self.sem[e], 1)
        self.cnt[e] += 1
        self.n_inst += 1
        ev = Ev(self.sem[e], self.cnt[e], e)
        for b in reads:
            b.r[e] = ev
        for b in writes:
            b.w = ev
            b.r = {}
        return ev

    def dma(self, q, out, in_, reads=(), writes=(), semname=None, **kw):
        self._deps(q, reads, writes, is_dma=True)
        if semname is None:
            semname = (writes[0].name if writes else reads[0].name)
        ent = self.dsems.get(semname)
        if ent is None or ent[1] + 16 > SEM_LIMIT:
            ent = [self._alloc_sem("d"), 0]
            self.dsems[semname] = ent
            self.all_dsems.append(ent)
        inst = self.eng[q].dma_start(out=out, in_=in_, **kw)
        inst.then_inc(ent[0], 16)
        ent[1] += 16
        self.n_inst += 1
        ev = Ev(ent[0], ent[1], "dma")
        for b in reads:
            b.r["dma" + semname] = ev
        for b in writes:
            b.w = ev
            b.r = {}
        return ev

    def barrier(self):
        evs = [Ev(self.sem[e], self.cnt[e], e) for e in self.eng if self.cnt[e] > 0]
        for e in self.eng:
            for ev in evs:
                if ev.eng != e:
                    self._wait(e, ev)
            for ent in self.all_dsems:
                if ent[1] > 0:
                    self._wait(e, Ev(ent[0], ent[1], "dma"))


def tiles(nblk, n):
    out = [(0, 128)]
    t = 128
    tp = nblk * 128
    while t < tp:
        w = min(n, tp - t)
        out.append((t, w))
        t += w
    return out


class _Stop(Exception):
    pass


def build(nblk, depth=2, stop=None):
    TP = nblk * 128
    NX = (nblk - 1) * 128
    nc = bass.Bass("TRN2", target_bir_lowering=False)

    def din(name, shape):
        return nc.dram_tensor(name, shape, F32, kind="ExternalInput").ap()

    x = din("x", [NX, D])
    meta = din("meta_tokens", [16, D])
    hgrn_lb = din("hgrn_lb", [depth, 512])
    norm1_g = din("norm1_g", [depth, D])
    w_in = din("w_in", [depth, D, NIN])
    hg_norm_g = din("hg_norm_g", [depth, 128])
    w_hg_out = din("w_hg_out", [depth, 512, D])
    cq_norm_g = din("cq_norm_g", [depth, 256])
    w_uq = din("w_uq", [depth, 256, 512])
    w_qi = din("w_qi", [depth, 256, 256])
    q_norm_g = din("q_norm_g", [depth, 64])
    k_norm_g = din("k_norm_g", [depth, 64])
    w_at_out = din("w_at_out", [depth, 512, D])
    cv_dw_w = din("cv_dw_w", [depth, 31, 512])
    cv_dw_b = din("cv_dw_b", [depth, 512])
    cv_ln_g = din("cv_ln_g", [depth, 512])
    cv_ln_b = din("cv_ln_b", [depth, 512])
    w_cv_out = din("w_cv_out", [depth, 512, D])
    w_mix_out = din("w_mix_out", [depth, D, D])
    norm2_g = din("norm2_g", [depth, D])
    w_ffn_up = din("w_ffn_up", [depth, D, 2 * FF])
    ffn_dw_w = din("ffn_dw_w", [depth, 3, 2 * FF])
    ffn_dw_b = din("ffn_dw_b", [depth, 2 * FF])
    w_ffn_down = din("w_ffn_down", [depth, FF, D])
    y = nc.dram_tensor("y", [NX, D], F32, kind="ExternalOutput").ap()

    hT = nc.dram_tensor("hT_scr", [D, TP], F32).ap()
    zT = nc.dram_tensor("zT_scr", [ZROWS, TP], F32).ap()
    vhg = nc.dram_tensor("vhg_scr", [TP, 512], BF16).ap()
    aos = nc.dram_tensor("ao_scr", [64, 8, TP], BF16).ap()
    hT3 = hT.rearrange("(c p) t -> p c t", p=128)

    ges = ExitStack()
    try:
      with ges:
        S = Sched(nc, ges)

        uniq = [0]

        def alloc(es, name, shape, dt=F32):
            uniq[0] += 1
            return es.enter_context(nc.sbuf_tensor(f"{name}_{uniq[0]}", shape, dt)), Buf(name)

        ps = ges.enter_context(nc.psum_tensor("ps", [128, 8, 512], F32))
        pb = [Buf(f"psb{i}") for i in range(8)]

        idf, idfB = alloc(ges, "idf", [128, 128])
        idb, idbB = alloc(ges, "idb", [128, 128], BF16)
        id4, id4B = alloc(ges, "id4", [128, 4, 128], BF16)
        onesb, onesB = alloc(ges, "onesb", [128, 128], BF16)
        shf, shfB = alloc(ges, "shf", [128, 64])
        cmask, cmaskB = alloc(ges, "cmask", [128, 128])
        tri, triB = alloc(ges, "tri", [64, 64])
        mb0f, mb0fB = alloc(ges, "mb0f", [128, 128])
        mb0, mb0B = alloc(ges, "mb0", [128, 128], BF16)
        padb, padbB = alloc(ges, "padb", [128, 1])
        rm, rmB = alloc(ges, "rm", [128, 512])
        p2t, p2tB = alloc(ges, "p2t", [128, NIT])
        epsc, epsB = alloc(ges, "epsc", [128, 1])
        NCOL = 340
        cols, colsB = alloc(ges, "cols", [128, depth, NCOL])
        lbc, lbB = alloc(ges, "lbc", [128, depth, 4])
        omlc, omlB = alloc(ges, "omlc", [128, depth, 4])

        def asel(t, B, pattern, cmp, fill, base, cm):
            S.op("pool", lambda e: e.affine_select(out=t, in_=t, pattern=pattern, compare_op=cmp,
                                                   fill=fill, base=base, channel_multiplier=cm),
                 reads=[B], writes=[B])

        S.op("pool", lambda e: e.memset(idf[:], 0.0), writes=[idfB])
        asel(idf[:], idfB, [[-1, 128]], ALU.not_equal, 1.0, 0, 1)
        S.op("dve", lambda e: e.tensor_copy(out=idb[:], in_=idf[:]), reads=[idfB], writes=[idbB])
        for j in range(4):
            S.op("dve", lambda e: e.tensor_copy(out=id4[:, j, :], in_=idf[:]), reads=[idfB], writes=[id4B])
        S.op("pool", lambda e: e.memset(onesb[:], 1.0), writes=[onesB])
        S.op("pool", lambda e: e.memset(shf[:], 0.0), writes=[shfB])
        asel(shf[:], shfB, [[-1, 64]], ALU.not_equal, 1.0, -64, 1)
        S.op("pool", lambda e: e.memset(cmask[:], 0.0), writes=[cmaskB])
        asel(cmask[:], cmaskB, [[-1, 128]], ALU.is_ge, -1e30, 0, 1)
        S.op("pool", lambda e: e.memset(tri[:], 1.0), writes=[triB])
        asel(tri[:], triB, [[1, 64]], ALU.is_ge, 0.0, 0, -1)
        S.op("pool", lambda e: e.memset(mb0f[:], 0.0), writes=[mb0fB])
        asel(mb0f[:], mb0fB, [[-1, 128]], ALU.is_ge, NEG, 0, 1)
        asel(mb0f[:], mb0fB, [[1, 128]], ALU.is_ge, NEG, -112, 0)
        asel(mb0f[:], mb0fB, [[-1, 128]], ALU.not_equal, 0.0, 0, 1)
        S.op("dve", lambda e: e.tensor_copy(out=mb0[:], in_=mb0f[:]), reads=[mb0fB], writes=[mb0B])
        S.op("pool", lambda e: e.memset(padb[:], 0.0), writes=[padbB])
        asel(padb[:], padbB, [[0, 1]], ALU.is_ge, NEG, -112, 1)
        S.op("pool", lambda e: e.memset(rm[:], 1.0), writes=[rmB])
        S.op("pool", lambda e: e.memset(rm[:].rearrange("p (c k) -> p c k", k=64)[:, :, 0:1], 0.0),
             reads=[rmB], writes=[rmB])
        for i in range(NIT):
            S.op("pool", lambda e: e.memset(p2t[:, i:i + 1], 2.0 ** -(i + 1)), writes=[p2tB])
        S.op("pool", lambda e: e.memset(epsc[:], EPS), writes=[epsB])
        S.op("pool", lambda e: e.memset(cols[:], 0.0), writes=[colsB])

        C_N1, C_N2, C_HGG, C_CQG, C_QG, C_KG = 0, 8, 16, 17, 19, 20
        C_CVW, C_CVB, C_LNG, C_LNB, C_FW, C_FB = 21, 145, 149, 153, 157, 289
        C_LB = 333
        pst, pstB = alloc(ges, "pstage", [128, 128])
        bank_rr = [0]

        def load_cols(l, src2d, n, off, npart=128):
            S.dma("sp", pst[0:n, 0:npart], src2d, writes=[pstB])
            b = bank_rr[0] % 4
            bank_rr[0] += 1
            S.op("pe", lambda e: e.transpose(ps[0:npart, b, 0:n], pst[0:n, 0:npart], idf[0:n, 0:n]),
                 reads=[pstB, idfB], writes=[pb[b]])
            S.op("dve", lambda e: e.tensor_copy(out=cols[0:npart, l, off:off + n], in_=ps[0:npart, b, 0:n]),
                 reads=[pb[b]], writes=[colsB])

        for l in range(depth):
            load_cols(l, norm1_g[l].rearrange("(c p) -> c p", p=128), 8, C_N1)
            load_cols(l, norm2_g[l].rearrange("(c p) -> c p", p=128), 8, C_N2)
            load_cols(l, hg_norm_g[l].rearrange("(c p) -> c p", p=128), 1, C_HGG)
            load_cols(l, cq_norm_g[l].rearrange("(c p) -> c p", p=128), 2, C_CQG)
            load_cols(l, q_norm_g[l].rearrange("(c p) -> c p", p=64), 1, C_QG, npart=64)
            load_cols(l, k_norm_g[l].rearrange("(c p) -> c p", p=64), 1, C_KG, npart=64)
            load_cols(l, cv_dw_w[l].rearrange("j (c p) -> (j c) p", p=128), 124, C_CVW)
            load_cols(l, cv_dw_b[l].rearrange("(c p) -> c p", p=128), 4, C_CVB)
            load_cols(l, cv_ln_g[l].rearrange("(c p) -> c p", p=128), 4, C_LNG)
            load_cols(l, cv_ln_b[l].rearrange("(c p) -> c p", p=128), 4, C_LNB)
            for j in range(3):
                load_cols(l, ffn_dw_w[l, j].rearrange("(c p) -> c p", p=128), 44, C_FW + 44 * j)
            load_cols(l, ffn_dw_b[l].rearrange("(c p) -> c p", p=128), 44, C_FB)
            load_cols(l, hgrn_lb[l].rearrange("(c p) -> c p", p=128), 4, C_LB)
            S.op("dve", lambda e: e.tensor_scalar(out=cols[0:64, l, C_QG:C_QG + 1], in0=cols[0:64, l, C_QG:C_QG + 1],
                                                  scalar1=0.125, scalar2=None, op0=ALU.mult),
                 reads=[colsB], writes=[colsB])
        S.op("pool", lambda e: e.memset(lbc[:], 0.0), writes=[lbB])
        S.op("pool", lambda e: e.memset(omlc[:], 1.0), writes=[omlB])
        if depth == 2:
            S.op("dve", lambda e: e.tensor_tensor(out=lbc[:, 1, :], in0=cols[:, 1, C_LB:C_LB + 4],
                                                  in1=cols[:, 0, C_LB:C_LB + 4], op=ALU.subtract),
                 reads=[colsB], writes=[lbB])
            S.op("act", lambda e: e.activation(out=lbc[:, 1, :], in_=lbc[:, 1, :], func=AF.Sigmoid),
                 reads=[lbB], writes=[lbB])
            S.op("dve", lambda e: e.tensor_scalar(out=omlc[:, 1, :], in0=lbc[:, 1, :], scalar1=-1.0, scalar2=1.0,
                                                  op0=ALU.mult, op1=ALU.add), reads=[lbB], writes=[omlB])

        def chk(tag):
            if stop == tag:
                S.barrier()
                raise _Stop()

        chk("prologue")

        def col(l, c, n=1, npart=128):
            return cols[0:npart, l, c:c + n]

        rr = {"ev": 0}

        def evac_eng():
            rr["ev"] += 1
            return "act" if rr["ev"] % 2 == 0 else "dve"

        def copy_op(e_name, out, in_, reads, writes):
            if e_name == "act":
                S.op("act", lambda e: e.activation(out=out, in_=in_, func=AF.Copy), reads=reads, writes=writes)
            else:
                S.op(e_name, lambda e: e.tensor_copy(out=out, in_=in_), reads=reads, writes=writes)

        def load_weight(es, name, src3, kp, kc, ncols, stage, stageB, piece):
            wt, wB = alloc(es, name, [kp, kc, ncols], BF16)
            i = 0
            for c in range(kc):
                for c0 in range(0, ncols, piece):
                    w = min(piece, ncols - c0)
                    sl = i % 2
                    S.dma("sp", stage[sl][0:kp, 0:w], src3[:, c, c0:c0 + w], writes=[stageB[sl]])
                    eng = "pool" if i % 2 == 0 else "dve"
                    S.op(eng, lambda e: e.tensor_copy(out=wt[:, c, c0:c0 + w], in_=stage[sl][0:kp, 0:w]),
                         reads=[stageB[sl]], writes=[wB])
                    i += 1
            return wt, wB

        with ExitStack() as pes:
            xs, xsB = alloc(pes, "xs", [128, 4, D])
            hb = [alloc(pes, f"hbI{i}", [128, 8, 512]) for i in range(2)]
            for ti, (t0, N) in enumerate(tiles(nblk, 512)):
                h, hB = hb[ti % 2]
                nb = N // 128
                if ti == 0:
                    S.op("pool", lambda e: e.memset(h[:, :, 0:128], 0.0), writes=[hB])
                    S.dma("sp", xs[0:16, 0, :], meta[:, :], writes=[xsB])
                    for c in range(8):
                        b = c % 4
                        S.op("pe", lambda e: e.transpose(ps[:, b, 0:16], xs[0:16, 0, c * 128:(c + 1) * 128], idf[0:16, 0:16]),
                             reads=[xsB, idfB], writes=[pb[b]])
                        copy_op(evac_eng(), h[:, c, 112:128], ps[:, b, 0:16], [pb[b]], [hB])
                else:
                    r0 = t0 - 128
                    S.dma("sp", xs[:, 0:nb, :], x[r0:r0 + N, :].rearrange("(j p) d -> p j d", p=128), writes=[xsB])
                    for c in range(8):
                        b = c % 4
                        for j in range(nb):
                            S.op("pe", lambda e: e.transpose(ps[:, b, j * 128:(j + 1) * 128], xs[:, j, c * 128:(c + 1) * 128], idf[:]),
                                 reads=[xsB, idfB], writes=[pb[b]])
                        copy_op(evac_eng(), h[:, c, 0:N], ps[:, b, 0:N], [pb[b]], [hB])
                S.dma("pool", hT3[:, :, t0:t0 + N], h[:, :, 0:N], reads=[hB])
            S.barrier()
        chk("I")

        for l in range(depth):
            last = (l == depth - 1)
            with ExitStack() as les:
                kT, kTB = alloc(les, "kT", [64, TP], BF16)
                kiT, kiTB = alloc(les, "kiT", [64, TP], BF16)
                Va, VaB = alloc(les, "Va", [128, nblk, 128], BF16)
                Wx, WxB = alloc(les, "Wx", [128, nblk, 4])
                S.op("pool", lambda e: e.memset(Va[:], 1.0), writes=[VaB])

                with ExitStack() as pes:
                    h, hB = alloc(pes, "hA", [128, 8, 512])
                    hflat = h[:].rearrange("p c n -> p (c n)")
                    stage = [hflat[:, 0:1649], hflat[:, 2048:2048 + 1649]]
                    wb, wbB = load_weight(pes, "w_in_bf", w_in[l].rearrange("(c p) n -> p c n", p=128), 128, 8, NIN,
                                          stage, [hB, hB], 1649)
                    xn, xnB = alloc(pes, "xnA", [128, 8, 512], BF16)
                    rt, rtB = alloc(pes, "rtA", [128, 512])
                    rs, rsB = alloc(pes, "rsA", [128, 512])
                    zs = [alloc(pes, f"zsA{i}", [128, 512]) for i in range(4)]
                    vst = [alloc(pes, f"vstA{i}", [128, 512], BF16) for i in range(2)]
                    kraw, krawB = alloc(pes, "krawA", [64, 512])
                    ksq, ksqB = alloc(pes, "ksqA", [64, 512], BF16)
                    groups = []
                    for (wc, zr, n) in ((CQ_, ZQ, 4), (CF_, ZF, 4), (CG_, ZG, 4), (CCQ_, ZCQ, 2), (CU_, ZU, 8), (CGT_, ZGT, 24)):
                        for i in range(n):
                            groups.append((wc + i * 128, zr + i * 128))
                    gi = 0
                    for ti, (t0, N) in enumerate(tiles(nblk, 512)):
                        nb = N // 128
                        S.dma("sp", h[:, :, 0:N], hT3[:, :, t0:t0 + N], writes=[hB])
                        S.op("act", lambda e: e.activation(out=xn[:, :, 0:N], in_=h[:, :, 0:N], func=AF.Square),
                             reads=[hB], writes=[xnB])
                        for c in range(8):
                            S.op("pe", lambda e: e.matmul(ps[:, 7, 0:N], lhsT=onesb[:], rhs=xn[:, c, 0:N], start=(c == 0), stop=(c == 7)),
                                 reads=[onesB, xnB], writes=[pb[7]])
                        S.op("act", lambda e: e.activation(out=rt[:, 0:N], in_=ps[:, 7, 0:N], func=AF.Sqrt, bias=epsc[:], scale=1.0 / D),
                             reads=[pb[7], epsB], writes=[rtB])
                        S.op("dve", lambda e: e.reciprocal(out=rs[:, 0:N], in_=rt[:, 0:N]), reads=[rtB], writes=[rsB])
                        for c in range(8):
                            S.op("dve", lambda e: e.scalar_tensor_tensor(out=xn[:, c, 0:N], in0=h[:, c, 0:N], scalar=col(l, C_N1 + c),
                                                                         in1=rs[:, 0:N], op0=ALU.mult, op1=ALU.mult),
                                 reads=[hB, colsB, rsB], writes=[xnB])
                        for (wc, zr) in groups:
                            b = gi % 4
                            z, zB = zs[gi % 4]
                            gi += 1
                            for c in range(8):
                                S.op("pe", lambda e: e.matmul(ps[:, b, 0:N], lhsT=wb[:, c, wc:wc + 128], rhs=xn[:, c, 0:N], start=(c == 0), stop=(c == 7)),
                                     reads=[wbB, xnB], writes=[pb[b]])
                            copy_op(evac_eng(), z[:, 0:N], ps[:, b, 0:N], [pb[b]], [zB])
                            S.dma("pool", zT[zr:zr + 128, t0:t0 + N], z[:, 0:N], reads=[zB])
                        for c in range(8):
                            S.op("pe", lambda e: e.matmul(ps[0:64, 4, 0:N], lhsT=wb[:, c, CKA_:CKA_ + 64], rhs=xn[:, c, 0:N], start=(c == 0), stop=(c == 7)),
                                 reads=[wbB, xnB], writes=[pb[4]])
                        S.op("act", lambda e: e.activation(out=kraw[:, 0:N], in_=ps[0:64, 4, 0:N], func=AF.Copy), reads=[pb[4]], writes=[krawB])
                        S.op("act", lambda e: e.activation(out=ksq[:, 0:N], in_=kraw[:, 0:N], func=AF.Square), reads=[krawB], writes=[ksqB])
                        S.op("pe", lambda e: e.matmul(ps[0:64, 5, 0:N], lhsT=onesb[0:64, 0:64], rhs=ksq[:, 0:N], start=True, stop=True),
                             reads=[onesB, ksqB], writes=[pb[5]])
                        S.op("act", lambda e: e.activation(out=rt[0:64, 0:N], in_=ps[0:64, 5, 0:N], func=AF.Sqrt, bias=epsc[0:64, :], scale=1.0 / 64),
                             reads=[pb[5], epsB], writes=[rtB])
                        S.op("dve", lambda e: e.reciprocal(out=rs[0:64, 0:N], in_=rt[0:64, 0:N]), reads=[rtB], writes=[rsB])
                        S.op("dve", lambda e: e.scalar_tensor_tensor(out=kT[:, t0:t0 + N], in0=kraw[:, 0:N], scalar=col(l, C_KG, 1, 64),
                                                                     in1=rs[0:64, 0:N], op0=ALU.mult, op1=ALU.mult),
                             reads=[krawB, colsB, rsB], writes=[kTB])
                        for c in range(8):
                            S.op("pe", lambda e: e.matmul(ps[0:64, 6, 0:N], lhsT=wb[:, c, CKI_:CKI_ + 64], rhs=xn[:, c, 0:N], start=(c == 0), stop=(c == 7)),
                                 reads=[wbB, xnB], writes=[pb[6]])
                        S.op("act", lambda e: e.activation(out=kiT[:, t0:t0 + N], in_=ps[0:64, 6, 0:N], func=AF.Copy), reads=[pb[6]], writes=[kiTB])
                        for j in range(nb):
                            blk = t0 // 128 + j
                            b = gi % 4
                            vs, vsB = vst[gi % 2]
                            gi += 1
                            for c in range(8):
                                S.op("pe", lambda e: e.matmul(ps[:, b, :], lhsT=xn[:, c, j * 128:(j + 1) * 128], rhs=wb[:, c, CI_:CI_ + 512], start=(c == 0), stop=(c == 7)),
                                     reads=[wbB, xnB], writes=[pb[b]])
                            copy_op(evac_eng(), vs[:], ps[:, b, :], [pb[b]], [vsB])
                            S.dma("pool", vhg[t0 + j * 128:t0 + (j + 1) * 128, :], vs[:], reads=[vsB])
                            b = gi % 4
                            gi += 1
                            for c in range(8):
                                S.op("pe", lambda e: e.matmul(ps[:, b, 0:64], lhsT=xn[:, c, j * 128:(j + 1) * 128], rhs=wb[:, c, CVA_:CVA_ + 64], start=(c == 0), stop=(c == 7)),
                                     reads=[wbB, xnB], writes=[pb[b]])
                            for c in range(8):
                                S.op("pe", lambda e: e.matmul(ps[:, b, 64:68], lhsT=xn[:, c, j * 128:(j + 1) * 128], rhs=wb[:, c, CWI_:CWI_ + 4], start=(c == 0), stop=(c == 7)),
                                     reads=[wbB, xnB], writes=[pb[b]])
                            S.op("act", lambda e: e.activation(out=Va[:, blk, 0:64], in_=ps[:, b, 0:64], func=AF.Copy), reads=[pb[b]], writes=[VaB])
                            S.op("dve", lambda e: e.tensor_scalar(out=Wx[:, blk, :], in0=ps[:, b, 64:68], scalar1=1.0 / 16, scalar2=None, op0=ALU.mult),
                                 reads=[pb[b]], writes=[WxB])
                    S.barrier()
                chk(f"A{l}")

                with ExitStack() as pes:
                    stg = [alloc(pes, f"stgB2{i}", [128, 512]) for i in range(2)]
                    wuq, wuqB = load_weight(pes, "wuq", w_uq[l].rearrange("(c p) n -> p c n", p=128), 128, 2, 512,
                                            [s[0] for s in stg], [s[1] for s in stg], 512)
                    wqi, wqiB = load_weight(pes, "wqi", w_qi[l].rearrange("(c p) n -> p c n", p=128), 128, 2, 256,
                                            [s[0] for s in stg], [s[1] for s in stg], 512)
                    LMAX = max(128, (nblk - 1) * 128)
                    I, IB = alloc(pes, "Isc", [128, LMAX])
                    mb, mbB = alloc(pes, "mb", [128, LMAX], BF16)
                    cq, cqB = alloc(pes, "cq", [128, 2, 512])
                    cqn, cqnB = alloc(pes, "cqn", [128, 2, 512], BF16)
                    rt, rtB = alloc(pes, "rtB2", [128, 512])
                    rs, rsB = alloc(pes, "rsB2", [128, 512])
                    qraw, qrawB = alloc(pes, "qraw", [64, 512])
                    qsq, qsqB = alloc(pes, "qsq", [64, 512], BF16)
                    qd, qdB = alloc(pes, "qd", [64, 8, 512], BF16)
                    qid, qidB = alloc(pes, "qid", [64, 4, 512], BF16)
                    rb = [alloc(pes, f"rb{i}", [128, 512]) for i in range(2)]
                    pt = [alloc(pes, f"pt{i}", [128, 1024], BF16) for i in range(2)]
                    rden, rdenB = alloc(pes, "rden", [128, 1024])
                    rsh, rshB = alloc(pes, "rsh", [64, 1024])
                    aot, aotB = alloc(pes, "aot", [64, 8, 512], BF16)
                    sm, smB = alloc(pes, "smB2", [128, 8])
                    steps, stepsB = alloc(pes, "steps", [128, NIT])
                    S.op("pool", lambda e: e.memset(rden[:], 0.0), writes=[rdenB])
                    bi = 0
                    pti = 0
                    for ti, (t0, N) in enumerate(tiles(nblk, 512)):
                        nb = N // 128
                        S.dma("sp", cq[:, :, 0:N], zT[ZCQ:ZCQ + 256, t0:t0 + N].rearrange("(c p) t -> p c t", p=128), writes=[cqB])
                        S.op("act", lambda e: e.activation(out=cqn[:, :, 0:N], in_=cq[:, :, 0:N], func=AF.Square), reads=[cqB], writes=[cqnB])
                        for c in range(2):
                            S.op("pe", lambda e: e.matmul(ps[:, 0, 0:N], lhsT=onesb[:], rhs=cqn[:, c, 0:N], start=(c == 0), stop=(c == 1)),
                                 reads=[onesB, cqnB], writes=[pb[0]])
                        S.op("act", lambda e: e.activation(out=rt[:, 0:N], in_=ps[:, 0, 0:N], func=AF.Sqrt, bias=epsc[:], scale=1.0 / 256),
                             reads=[pb[0], epsB], writes=[rtB])
                        S.op("dve", lambda e: e.reciprocal(out=rs[:, 0:N], in_=rt[:, 0:N]), reads=[rtB], writes=[rsB])
                        for c in range(2):
                            S.op("dve", lambda e: e.scalar_tensor_tensor(out=cqn[:, c, 0:N], in0=cq[:, c, 0:N], scalar=col(l, C_CQG + c),
                                                                         in1=rs[:, 0:N], op0=ALU.mult, op1=ALU.mult),
                                 reads=[cqB, colsB, rsB], writes=[cqnB])
                        for hh in range(8):
                            b = bi % 2
                            bi += 1
                            for c in range(2):
                                S.op("pe", lambda e: e.matmul(ps[0:64, b, 0:N], lhsT=wuq[:, c, hh * 64:(hh + 1) * 64], rhs=cqn[:, c, 0:N], start=(c == 0), stop=(c == 1)),
                                     reads=[wuqB, cqnB], writes=[pb[b]])
                            S.op("act", lambda e: e.activation(out=qraw[:, 0:N], in_=ps[0:64, b, 0:N], func=AF.Copy), reads=[pb[b]], writes=[qrawB])
                            S.op("act", lambda e: e.activation(out=qsq[:, 0:N], in_=qraw[:, 0:N], func=AF.Square), reads=[qrawB], writes=[qsqB])
                            S.op("pe", lambda e: e.matmul(ps[0:64, 2, 0:N], lhsT=onesb[0:64, 0:64], rhs=qsq[:, 0:N], start=True, stop=True),
                                 reads=[onesB, qsqB], writes=[pb[2]])
                            S.op("act", lambda e: e.activation(out=rt[0:64, 0:N], in_=ps[0:64, 2, 0:N], func=AF.Sqrt, bias=epsc[0:64, :], scale=1.0 / 64),
                                 reads=[pb[2], epsB], writes=[rtB])
                            S.op("dve", lambda e: e.reciprocal(out=rs[0:64, 0:N], in_=rt[0:64, 0:N]), reads=[rtB], writes=[rsB])
                            S.op("dve", lambda e: e.scalar_tensor_tensor(out=qd[:, hh, 0:N], in0=qraw[:, 0:N], scalar=col(l, C_QG, 1, 64),
                                                                         in1=rs[0:64, 0:N], op0=ALU.mult, op1=ALU.mult),
                                 reads=[qrawB, colsB, rsB], writes=[qdB])
                        for hh in range(4):
                            b = bi % 2
                            bi += 1
                            for c in range(2):
                                S.op("pe", lambda e: e.matmul(ps[0:64, b, 0:N], lhsT=wqi[:, c, hh * 64:(hh + 1) * 64], rhs=cqn[:, c, 0:N], start=(c == 0), stop=(c == 1)),
                                     reads=[wqiB, cqnB], writes=[pb[b]])
                            copy_op(evac_eng(), qid[:, hh, 0:N], ps[0:64, b, 0:N], [pb[b]], [qidB])
                        for j in range(nb):
                            bq = t0 // 128 + j
                            qc0, qc1 = j * 128, (j + 1) * 128
                            L = 128 * bq
                            if bq >= 1:
                                for st in range(0, L, 512):
                                    w = min(512, L - st)
                                    for hh in range(4):
                                        b = bi % 4
                                        bi += 1
                                        r, rB = rb[bi % 2]
                                        S.op("pe", lambda e: e.matmul(ps[:, b, 0:w], lhsT=qid[:, hh, qc0:qc1], rhs=kiT[:, 128 + st:128 + st + w], start=True, stop=True),
                                             reads=[qidB, kiTB], writes=[pb[b]])
                                        S.op("act", lambda e: e.activation(out=r[:, 0:w], in_=ps[:, b, 0:w], func=AF.Relu), reads=[pb[b]], writes=[rB])
                                        if hh == 0:
                                            S.op("dve", lambda e: e.tensor_scalar(out=I[:, st:st + w], in0=r[:, 0:w], scalar1=Wx[:, bq, 0:1], scalar2=None, op0=ALU.mult),
                                                 reads=[rB, WxB], writes=[IB])
                                        else:
                                            S.op("dve", lambda e: e.scalar_tensor_tensor(out=I[:, st:st + w], in0=r[:, 0:w], scalar=Wx[:, bq, hh:hh + 1],
                                                                                         in1=I[:, st:st + w], op0=ALU.mult, op1=ALU.add),
                                                 reads=[rB, WxB, IB], writes=[IB])
                                S.op("dve", lambda e: e.tensor_reduce(out=sm[:, 0:1], in_=I[:, 0:L], axis=AX.X, op=ALU.max, apply_absolute_value=True),
                                     reads=[IB], writes=[smB])
                                S.op("dve", lambda e: e.tensor_tensor(out=I[:, L - 128:L], in0=I[:, L - 128:L], in1=cmask[:], op=ALU.add),
                                     reads=[IB, cmaskB], writes=[IB])
                                S.op("dve", lambda e: e.tensor_scalar(out=sm[:, 1:2], in0=sm[:, 0:1], scalar1=-1.001, scalar2=-1e-6, op0=ALU.mult, op1=ALU.add),
                                     reads=[smB], writes=[smB])
                                S.op("dve", lambda e: e.tensor_scalar(out=sm[:, 2:3], in0=sm[:, 0:1], scalar1=2.002, scalar2=2e-6, op0=ALU.mult, op1=ALU.add),
                                     reads=[smB], writes=[smB])
                                S.op("dve", lambda e: e.tensor_scalar(out=steps[:], in0=p2t[:], scalar1=sm[:, 2:3], scalar2=None, op0=ALU.mult),
                                     reads=[p2tB, smB], writes=[stepsB])
                                for it in range(NIT):
                                    S.op("dve", lambda e: e.tensor_tensor(out=sm[:, 3:4], in0=sm[:, 1:2], in1=steps[:, it:it + 1], op=ALU.add),
                                         reads=[smB, stepsB], writes=[smB])
                                    S.op("dve", lambda e: e.tensor_scalar(out=mb[:, 0:L], in0=I[:, 0:L], scalar1=sm[:, 3:4], scalar2=None,
                                                                          op0=ALU.is_ge, op1=ALU.add, accum_out=sm[:, 4:5]),
                                         reads=[IB, smB], writes=[mbB, smB])
                                    S.op("dve", lambda e: e.tensor_scalar(out=sm[:, 5:6], in0=sm[:, 4:5], scalar1=KSEL - 0.5, scalar2=steps[:, it:it + 1],
                                                                          op0=ALU.is_ge, op1=ALU.mult),
                                         reads=[smB, stepsB], writes=[smB])
                                    S.op("dve", lambda e: e.tensor_tensor(out=sm[:, 1:2], in0=sm[:, 1:2], in1=sm[:, 5:6], op=ALU.add),
                                         reads=[smB], writes=[smB])
                                S.op("dve", lambda e: e.tensor_scalar(out=mb[:, 0:L], in0=I[:, 0:L], scalar1=sm[:, 1:2], scalar2=NEG,
                                                                      op0=ALU.is_lt, op1=ALU.mult),
                                     reads=[IB, smB], writes=[mbB])
                            for c in range(bq + 1):
                                has_mask = (bq == 0) or (c >= 1)
                                for half in range(2):
                                    S.op("pe", lambda e: e.matmul(ps[:, 4 + half, :].rearrange("p (a b) -> p a b", b=128),
                                                                  lhsT=kT[:, c * 128:(c + 1) * 128], rhs=qd[:, half * 4:half * 4 + 4, qc0:qc1],
                                                                  start=True, stop=(not has_mask)),
                                         reads=[kTB, qdB], writes=[pb[4 + half]])
                                    if has_mask:
                                        if bq == 0:
                                            S.op("pe", lambda e: e.matmul(ps[:, 4 + half, :].rearrange("p (a b) -> p a b", b=128),
                                                                          lhsT=mb0[:], rhs=id4[:], start=False, stop=True),
                                                 reads=[mb0B, id4B], writes=[pb[4 + half]])
                                        else:
                                            S.op("pe", lambda e: e.matmul(ps[:, 4 + half, :].rearrange("p (a b) -> p a b", b=128),
                                                                          lhsT=mb[:, (c - 1) * 128:c * 128], rhs=id4[:], start=False, stop=True),
                                                 reads=[mbB, id4B], writes=[pb[4 + half]])
                                p, pB = pt[pti % 2]
                                pti += 1
                                if c == 0 and bq >= 1:
                                    S.op("act", lambda e: e.activation(out=p[:].rearrange("p (a b) -> p a b", b=512), in_=ps[:, 4:6, :], func=AF.Exp, bias=padb[:]),
                                         reads=[pb[4], pb[5], padbB], writes=[pB])
                                else:
                                    S.op("act", lambda e: e.activation(out=p[:].rearrange("p (a b) -> p a b", b=512), in_=ps[:, 4:6, :], func=AF.Exp),
                                         reads=[pb[4], pb[5]], writes=[pB])
                                for half in range(2):
                                    S.op("pe", lambda e: e.matmul(ps[:, 6 + half, :], lhsT=Va[:, c, :], rhs=p[:, half * 512:(half + 1) * 512],
                                                                  start=(c == 0), stop=(c == bq)),
                                         reads=[VaB, pB], writes=[pb[6 + half]])
                            S.op("dve", lambda e: e.reciprocal(out=rden[64:128, :].rearrange("p (a b) -> p a b", b=512), in_=ps[64:128, 6:8, :]),
                                 reads=[pb[6], pb[7]], writes=[rdenB])
                            for half in range(2):
                                S.op("pe", lambda e: e.matmul(ps[0:64, 4 + half, :], lhsT=shf[:], rhs=rden[:, half * 512:(half + 1) * 512], start=True, stop=True),
                                     reads=[shfB, rdenB], writes=[pb[4 + half]])
                            S.op("act", lambda e: e.activation(out=rsh[:].rearrange("p (a b) -> p a b", b=512), in_=ps[0:64, 4:6, :], func=AF.Copy),
                                 reads=[pb[4], pb[5]], writes=[rshB])
                            for half in range(2):
                                S.op("dve", lambda e: e.tensor_tensor(out=aot[:, half * 4:half * 4 + 4, qc0:qc1],
                                                                      in0=ps[0:64, 6 + half, :].rearrange("p (a b) -> p a b", b=128),
                                                                      in1=rsh[:, half * 512:(half + 1) * 512].rearrange("p (a b) -> p a b", b=128), op=ALU.mult),
                                     reads=[pb[6 + half], rshB], writes=[aotB])
                        S.dma("pool", aos[:, :, t0:t0 + N], aot[:, :, 0:N], reads=[aotB])
                    S.barrier()
                chk(f"B2{l}")

            NB1 = 256
            with ExitStack() as pes:
                stg = [alloc(pes, f"stgB1{i}", [128, 1024]) for i in range(2)]
                sT = [s[0] for s in stg]
                sB = [s[1] for s in stg]
                whg, whgB = load_weight(pes, "whg", w_hg_out[l].rearrange("(c p) n -> p c n", p=128), 128, 4, D, sT, sB, 1024)
                wat, watB = load_weight(pes, "wat", w_at_out[l].rearrange("(c p) n -> p c n", p=64), 64, 8, D, sT, sB, 1024)
                wcv, wcvB = load_weight(pes, "wcv", w_cv_out[l].rearrange("(c p) n -> p c n", p=128), 128, 4, D, sT, sB, 1024)
                wmx, wmxB = load_weight(pes, "wmx", w_mix_out[l].rearrange("(c p) n -> p c n", p=128), 128, 8, D, sT, sB, 1024)
                Sst, SstB = alloc(pes, "Sst", [128, 4, 128])
                Sp, SpB = alloc(pes, "Sp", [128, 128], BF16)
                HB, HBB = alloc(pes, "HB", [128, 4, 30 + NB1])
                S.op("pool", lambda e: e.memset(Sst[:], 0.0), writes=[SstB])
                S.op("pool", lambda e: e.memset(HB[:], 0.0), writes=[HBB])
                fq, fqB = alloc(pes, "fq", [128, 3, 4, NB1])
                vt, vtB = alloc(pes, "vt", [64, NB1 // 64, 512], BF16)
                tA, tAB = alloc(pes, "tA", [128, NB1])
                tB_, tBB = alloc(pes, "tB", [128, NB1])
                tb, tbB = alloc(pes, "tb", [128, NB1])
                tbm, tbmB = alloc(pes, "tbm", [128, NB1])
                teq, teqB = alloc(pes, "teq", [128, NB1])
                tek, tekB = alloc(pes, "tek", [128, NB1])
                tqs, tqsB = alloc(pes, "tqs", [128, NB1])
                qtl, qtlB = alloc(pes, "qtl", [128, NB1], BF16)
                ktl, ktlB = alloc(pes, "ktl", [128, NB1], BF16)
                ee, eeB = alloc(pes, "ee", [128, 3, NB1 // 64])
                ktok = [alloc(pes, f"ktok{i}", [64, 128], BF16) for i in range(2)]
                att = [alloc(pes, f"att{i}", [64, 64], BF16) for i in range(2)]
                osq, osqB = alloc(pes, "osq", [128, NB1], BF16)
                rt, rtB = alloc(pes, "rtB1", [128, NB1])
                rs, rsB = alloc(pes, "rsB1", [128, NB1])
                gs, gsB = alloc(pes, "gs", [128, NB1])
                on, onB = alloc(pes, "on", [128, NB1])
                oT, oTB = alloc(pes, "oT", [128, 4, NB1], BF16)
                u, uB = alloc(pes, "u", [128, 8, NB1])
                sgb, sgbB = alloc(pes, "sgb", [128, 4, NB1])
                acc, accB = alloc(pes, "acc", [128, 4, NB1])
                accb, accbB = alloc(pes, "accb", [128, 4, NB1], BF16)
                sqb, sqbB = alloc(pes, "sqb", [128, 4, NB1], BF16)
                mu, muB = alloc(pes, "mu", [128, NB1])
                var, varB = alloc(pes, "var", [128, NB1])
                cvo, cvoB = alloc(pes, "cvo", [128, 4, NB1], BF16)
                aot, aotB = alloc(pes, "aotB1", [64, 8, NB1], BF16)
                gl = [alloc(pes, f"gl{i}", [128, 3, NB1]) for i in range(2)]
                m1, m1B = alloc(pes, "m1", [128, NB1])
                m2, m2B = alloc(pes, "m2", [128, NB1])
                m3, m3B = alloc(pes, "m3", [128, NB1])
                mT, mTB = alloc(pes, "mT", [128, 8, NB1], BF16)
                h, hB = alloc(pes, "hB1", [128, 8, NB1])
                zgt = zT[ZGT:ZGT + 3072, :].rearrange("(br fo p) t -> p br fo t", br=3, fo=8, p=128)
                ki = 0
                gli = 0
                for ti, (t0, N) in enumerate(tiles(nblk, NB1)):
                    nch = N // 64
                    S.dma("sp", fq[:, 0, :, 0:N], zT[ZF:ZF + 512, t0:t0 + N].rearrange("(c p) t -> p c t", p=128), writes=[fqB])
                    S.dma("sp", fq[:, 1, :, 0:N], zT[ZQ:ZQ + 512, t0:t0 + N].rearrange("(c p) t -> p c t", p=128), writes=[fqB])
                    S.dma("sp", fq[:, 2, :, 0:N], zT[ZG:ZG + 512, t0:t0 + N].rearrange("(c p) t -> p c t", p=128), writes=[fqB])
                    S.dma("sp", vt[:, 0:nch, :], vhg[t0:t0 + N, :].rearrange("(c p) v -> p c v", p=64), writes=[vtB])
                    S.dma("sp", u[:, :, 0:N], zT[ZU:ZU + 1024, t0:t0 + N].rearrange("(c p) t -> p c t", p=128), writes=[uB])
                    S.dma("sp", aot[:, :, 0:N], aos[:, :, t0:t0 + N], writes=[aotB])
                    S.dma("sp", h[:, :, 0:N], hT3[:, :, t0:t0 + N], writes=[hB])
                    for hh in range(4):
                        S.op("act", lambda e: e.activation(out=tA[:, 0:N], in_=fq[:, 0, hh, 0:N], func=AF.Sigmoid), reads=[fqB], writes=[tAB])
                        S.op("dve", lambda e: e.tensor_scalar(out=tA[:, 0:N], in0=tA[:, 0:N], scalar1=omlc[:, l, hh:hh + 1], scalar2=lbc[:, l, hh:hh + 1],
                                                              op0=ALU.mult, op1=ALU.add), reads=[tAB, omlB, lbB], writes=[tAB])
                        S.op("act", lambda e: e.activation(out=tB_[:, 0:N], in_=tA[:, 0:N], func=AF.Ln), reads=[tAB], writes=[tBB])
                        S.op("dve", lambda e: e.tensor_scalar(out=tA[:, 0:N], in0=tA[:, 0:N], scalar1=-1.0, scalar2=1.0, op0=ALU.mult, op1=ALU.add),
                             reads=[tAB], writes=[tAB])
                        S.op("dve", lambda e: e.tensor_tensor_scan(out=tb[:, 0:N], data0=rm[:, 0:N], data1=tB_[:, 0:N], initial=0.0, op0=ALU.mult, op1=ALU.add),
                             reads=[rmB, tBB], writes=[tbB])
                        b3 = tb[:, 0:N].rearrange("p (c k) -> p c k", k=64)
                        bmid = bass.AP(tb, 31, [[NB1, 128], [64, nch], [0, 64]])
                        S.op("dve", lambda e: e.tensor_tensor(out=tbm[:, 0:N].rearrange("p (c k) -> p c k", k=64), in0=b3, in1=bmid, op=ALU.subtract),
                             reads=[tbB], writes=[tbmB])
                        S.op("act", lambda e: e.activation(out=teq[:, 0:N], in_=tbm[:, 0:N], func=AF.Exp), reads=[tbmB], writes=[teqB])
                        S.op("act", lambda e: e.activation(out=tek[:, 0:N], in_=tbm[:, 0:N], func=AF.Exp, scale=-1.0), reads=[tbmB], writes=[tekB])
                        S.op("act", lambda e: e.activation(out=tqs[:, 0:N], in_=fq[:, 1, hh, 0:N], func=AF.Silu), reads=[fqB], writes=[tqsB])
                        S.op("dve", lambda e: e.scalar_tensor_tensor(out=qtl[:, 0:N], in0=tqs[:, 0:N], scalar=128.0 ** -0.5, in1=teq[:, 0:N], op0=ALU.mult, op1=ALU.mult),
                             reads=[tqsB, teqB], writes=[qtlB])
                        S.op("dve", lambda e: e.tensor_tensor(out=ktl[:, 0:N], in0=tA[:, 0:N], in1=tek[:, 0:N], op=ALU.mult), reads=[tAB, tekB], writes=[ktlB])
                        bm3 = tbm[:, 0:N].rearrange("p (c k) -> p c k", k=64)
                        S.op("act", lambda e: e.activation(out=ee[:, 0, 0:nch], in_=b3[:, :, 31], func=AF.Exp), reads=[tbB], writes=[eeB])
                        S.op("act", lambda e: e.activation(out=ee[:, 1, 0:nch], in_=b3[:, :, 63], func=AF.Exp), reads=[tbB], writes=[eeB])
                        S.op("act", lambda e: e.activation(out=ee[:, 2, 0:nch], in_=bm3[:, :, 63], func=AF.Exp), reads=[tbmB], writes=[eeB])
                        ob = hh % 2
                        for ch in range(nch):
                            c0, c1 = ch * 64, (ch + 1) * 64
                            kt_, ktB_ = ktok[ki % 2]
                            at_, atB_ = att[ki % 2]
                            ki += 1
                            psbf = ps[:, 2, :].bitcast(BF16)
                            S.op("pe", lambda e: e.transpose(psbf[0:64, 0:128], ktl[:, c0:c1], idb[:]), reads=[ktlB, idbB], writes=[pb[2]])
                            S.op("act", lambda e: e.activation(out=kt_[:], in_=psbf[0:64, 0:128], func=AF.Copy), reads=[pb[2]], writes=[ktB_])
                            S.op("pe", lambda e: e.matmul(ps[0:64, 3, 0:64], lhsT=ktl[:, c0:c1], rhs=qtl[:, c0:c1], start=True, stop=True),
                                 reads=[ktlB, qtlB], writes=[pb[3]])
                            S.op("dve", lambda e: e.tensor_tensor(out=at_[:], in0=ps[0:64, 3, 0:64], in1=tri[:], op=ALU.mult), reads=[pb[3], triB], writes=[atB_])
                            S.op("dve", lambda e: e.tensor_scalar(out=Sp[:], in0=Sst[:, hh, :], scalar1=ee[:, 0, ch:ch + 1], scalar2=None, op0=ALU.mult),
                                 reads=[SstB, eeB], writes=[SpB])
                            S.op("pe", lambda e: e.matmul(ps[:, ob, c0:c1], lhsT=vt[:, ch, hh * 128:(hh + 1) * 128], rhs=at_[:], start=True, stop=False),
                                 reads=[vtB, atB_], writes=[pb[ob]])
                            S.op("pe", lambda e: e.matmul(ps[:, ob, c0:c1], lhsT=Sp[:], rhs=qtl[:, c0:c1], start=False, stop=True),
                                 reads=[SpB, qtlB], writes=[pb[ob]])
                            S.op("pe", lambda e: e.matmul(ps[:, 4, 0:128], lhsT=kt_[:], rhs=vt[:, ch, hh * 128:(hh + 1) * 128], start=True, stop=True),
                                 reads=[ktB_, vtB], writes=[pb[4]])
                            S.op("dve", lambda e: e.tensor_scalar(out=Sst[:, hh, :], in0=Sst[:, hh, :], scalar1=ee[:, 1, ch:ch + 1], scalar2=None, op0=ALU.mult),
                                 reads=[SstB, eeB], writes=[SstB])
                            S.op("dve", lambda e: e.scalar_tensor_tensor(out=Sst[:, hh, :], in0=ps[:, 4, 0:128], scalar=ee[:, 2, ch:ch + 1], in1=Sst[:, hh, :],
                                                                         op0=ALU.mult, op1=ALU.add), reads=[pb[4], eeB, SstB], writes=[SstB])
                        S.op("act", lambda e: e.activation(out=osq[:, 0:N], in_=ps[:, ob, 0:N], func=AF.Square), reads=[pb[ob]], writes=[osqB])
                        S.op("pe", lambda e: e.matmul(ps[:, 5, 0:N], lhsT=onesb[:], rhs=osq[:, 0:N], start=True, stop=True), reads=[onesB, osqB], writes=[pb[5]])
                        S.op("act", lambda e: e.activation(out=rt[:, 0:N], in_=ps[:, 5, 0:N], func=AF.Sqrt, bias=epsc[:], scale=1.0 / 128),
                             reads=[pb[5], epsB], writes=[rtB])
                        S.op("dve", lambda e: e.reciprocal(out=rs[:, 0:N], in_=rt[:, 0:N]), reads=[rtB], writes=[rsB])
                        S.op("act", lambda e: e.activation(out=gs[:, 0:N], in_=fq[:, 2, hh, 0:N], func=AF.Silu), reads=[fqB], writes=[gsB])
                        S.op("dve", lambda e: e.scalar_tensor_tensor(out=on[:, 0:N], in0=ps[:, ob, 0:N], scalar=col(l, C_HGG), in1=rs[:, 0:N], op0=ALU.mult, op1=ALU.mult),
                             reads=[pb[ob], colsB, rsB], writes=[onB])
                        S.op("dve", lambda e: e.tensor_tensor(out=oT[:, hh, 0:N], in0=on[:, 0:N], in1=gs[:, 0:N], op=ALU.mult), reads=[onB, gsB], writes=[oTB])
                    S.op("act", lambda e: e.activation(out=sgb[:, :, 0:N], in_=u[:, 4:8, 0:N], func=AF.Sigmoid), reads=[uB], writes=[sgbB])
                    S.op("dve", lambda e: e.tensor_tensor(out=HB[:, :, 30:30 + N], in0=u[:, 0:4, 0:N], in1=sgb[:, :, 0:N], op=ALU.mult),
                         reads=[uB, sgbB], writes=[HBB])
                    for c in range(4):
                        S.op("dve", lambda e: e.tensor_scalar(out=acc[:, c, 0:N], in0=HB[:, c, 30:30 + N], scalar1=col(l, C_CVW + 30 * 4 + c), scalar2=col(l, C_CVB + c),
                                                              op0=ALU.mult, op1=ALU.add), reads=[HBB, colsB], writes=[accB])
                        for j in range(30):
                            S.op("dve", lambda e: e.scalar_tensor_tensor(out=acc[:, c, 0:N], in0=HB[:, c, j:j + N], scalar=col(l, C_CVW + j * 4 + c), in1=acc[:, c, 0:N],
                                                                         op0=ALU.mult, op1=ALU.add), reads=[HBB, colsB, accB], writes=[accB])
                    S.op("dve", lambda e: e.tensor_copy(out=HB[:, :, 0:30], in_=HB[:, :, N:N + 30]), reads=[HBB], writes=[HBB])
                    S.op("act", lambda e: e.activation(out=accb[:, :, 0:N], in_=acc[:, :, 0:N], func=AF.Copy), reads=[accB], writes=[accbB])
                    S.op("act", lambda e: e.activation(out=sqb[:, :, 0:N], in_=acc[:, :, 0:N], func=AF.Square), reads=[accB], writes=[sqbB])
                    for c in range(4):
                        S.op("pe", lambda e: e.matmul(ps[:, 6, 0:N], lhsT=onesb[:], rhs=accb[:, c, 0:N], start=(c == 0), stop=(c == 3)), reads=[onesB, accbB], writes=[pb[6]])
                    for c in range(4):
                        S.op("pe", lambda e: e.matmul(ps[:, 7, 0:N], lhsT=onesb[:], rhs=sqb[:, c, 0:N], start=(c == 0), stop=(c == 3)), reads=[onesB, sqbB], writes=[pb[7]])
                    S.op("act", lambda e: e.activation(out=mu[:, 0:N], in_=ps[:, 6, 0:N], func=AF.Copy, scale=1.0 / 512), reads=[pb[6]], writes=[muB])
                    S.op("dve", lambda e: e.tensor_tensor(out=var[:, 0:N], in0=mu[:, 0:N], in1=mu[:, 0:N], op=ALU.mult), reads=[muB], writes=[varB])
                    S.op("dve", lambda e: e.scalar_tensor_tensor(out=var[:, 0:N], in0=ps[:, 7, 0:N], scalar=1.0 / 512, in1=var[:, 0:N], op0=ALU.mult, op1=ALU.subtract),
                         reads=[pb[7], varB], writes=[varB])
                    S.op("act", lambda e: e.activation(out=rt[:, 0:N], in_=var[:, 0:N], func=AF.Sqrt, bias=epsc[:]), reads=[varB, epsB], writes=[rtB])
                    S.op("dve", lambda e: e.reciprocal(out=rs[:, 0:N], in_=rt[:, 0:N]), reads=[rtB], writes=[rsB])
                    for c in range(4):
                        S.op("dve", lambda e: e.tensor_tensor(out=acc[:, c, 0:N], in0=acc[:, c, 0:N], in1=mu[:, 0:N], op=ALU.subtract), reads=[accB, muB], writes=[accB])
                        S.op("dve", lambda e: e.tensor_tensor(out=acc[:, c, 0:N], in0=acc[:, c, 0:N], in1=rs[:, 0:N], op=ALU.mult), reads=[accB, rsB], writes=[accB])
                        S.op("act", lambda e: e.activation(out=cvo[:, c, 0:N], in_=acc[:, c, 0:N], func=AF.Silu, scale=col(l, C_LNG + c), bias=col(l, C_LNB + c)),
                             reads=[accB, colsB], writes=[cvoB])
                    for fo in range(8):
                        g_, gB_ = gl[gli % 2]
                        gli += 1
                        f0, f1 = fo * 128, (fo + 1) * 128
                        S.dma("sp", g_[:, :, 0:N], zgt[:, :, fo, t0:t0 + N], writes=[gB_])
                        S.op("act", lambda e: e.activation(out=g_[:, :, 0:N], in_=g_[:, :, 0:N], func=AF.Sigmoid), reads=[gB_], writes=[gB_])
                        for c in range(4):
                            S.op("pe", lambda e: e.matmul(ps[:, 0, 0:N], lhsT=whg[:, c, f0:f1], rhs=oT[:, c, 0:N], start=(c == 0), stop=(c == 3)), reads=[whgB, oTB], writes=[pb[0]])
                        for c in range(8):
                            S.op("pe", lambda e: e.matmul(ps[:, 1, 0:N], lhsT=wat[:, c, f0:f1], rhs=aot[:, c, 0:N], start=(c == 0), stop=(c == 7)), reads=[watB, aotB], writes=[pb[1]])
                        for c in range(4):
                            S.op("pe", lambda e: e.matmul(ps[:, 2, 0:N], lhsT=wcv[:, c, f0:f1], rhs=cvo[:, c, 0:N], start=(c == 0), stop=(c == 3)), reads=[wcvB, cvoB], writes=[pb[2]])
                        S.op("dve", lambda e: e.tensor_tensor(out=m1[:, 0:N], in0=ps[:, 0, 0:N], in1=g_[:, 0, 0:N], op=ALU.mult), reads=[pb[0], gB_], writes=[m1B])
                        S.op("dve", lambda e: e.tensor_tensor(out=m2[:, 0:N], in0=ps[:, 1, 0:N], in1=g_[:, 1, 0:N], op=ALU.mult), reads=[pb[1], gB_], writes=[m2B])
                        S.op("dve", lambda e: e.tensor_tensor(out=m3[:, 0:N], in0=ps[:, 2, 0:N], in1=g_[:, 2, 0:N], op=ALU.mult), reads=[pb[2], gB_], writes=[m3B])
                        S.op("pool", lambda e: e.tensor_tensor(out=m1[:, 0:N], in0=m1[:, 0:N], in1=m2[:, 0:N], op=ALU.add), reads=[m1B, m2B], writes=[m1B])
                        S.op("pool", lambda e: e.tensor_tensor(out=mT[:, fo, 0:N], in0=m1[:, 0:N], in1=m3[:, 0:N], op=ALU.add), reads=[m1B, m3B], writes=[mTB])
                    for fo in range(8):
                        b = 3 + fo % 2
                        for c in range(8):
                            S.op("pe", lambda e: e.matmul(ps[:, b, 0:N], lhsT=wmx[:, c, fo * 128:(fo + 1) * 128], rhs=mT[:, c, 0:N], start=(c == 0), stop=(c == 7)),
                                 reads=[wmxB, mTB], writes=[pb[b]])
                        S.op("dve", lambda e: e.tensor_tensor(out=h[:, fo, 0:N], in0=h[:, fo, 0:N], in1=ps[:, b, 0:N], op=ALU.add), reads=[hB, pb[b]], writes=[hB])
                    if ti == 0:
                        S.op("pool", lambda e: e.memset(h[:, :, 0:112], 0.0), reads=[hB], writes=[hB])
                    S.dma("pool", hT3[:, :, t0:t0 + N], h[:, :, 0:N], reads=[hB])
                S.barrier()
            chk(f"B1{l}")

            NC_ = 256
            with ExitStack() as pes:
                stg = [alloc(pes, f"stgC{i}", [128, 1408]) for i in range(2)]
                sT = [s[0] for s in stg]
                sB = [s[1] for s in stg]
                wup, wupB = load_weight(pes, "wup", w_ffn_up[l].rearrange("(c p) n -> p c n", p=128), 128, 8, 2 * FF, sT, sB, 1408)
                wdn, wdnB = load_weight(pes, "wdn", w_ffn_down[l].rearrange("(c p) n -> p c n", p=128), 128, 22, D, sT, sB, 1024)
                h, hB = alloc(pes, "hC", [128, 8, NC_])
                xn, xnB = alloc(pes, "xnC", [128, 8, NC_], BF16)
                rt, rtB = alloc(pes, "rtC", [128, NC_])
                rs, rsB = alloc(pes, "rsC", [128, NC_])
                xa = [alloc(pes, f"xa{i}", [128, 2, 2 + NC_]) for i in range(2)]
                ya = [alloc(pes, f"ya{i}", [128, 2, NC_]) for i in range(2)]
                sa, saB = alloc(pes, "sa", [128, NC_])
                gT, gTB = alloc(pes, "gT", [128, 22, NC_], BF16)
                hist, histB = alloc(pes, "hist", [128, 44, 2])
                ot = [alloc(pes, f"ot{i}", [128, D]) for i in range(2)]
                S.op("pool", lambda e: e.memset(hist[:], 0.0), writes=[histB])
                pi = 0
                oi = 0
                for ti, (t0, N) in enumerate(tiles(nblk, NC_)):
                    S.dma("sp", h[:, :, 0:N], hT3[:, :, t0:t0 + N], writes=[hB])
                    S.op("act", lambda e: e.activation(out=xn[:, :, 0:N], in_=h[:, :, 0:N], func=AF.Square), reads=[hB], writes=[xnB])
                    for c in range(8):
                        S.op("pe", lambda e: e.matmul(ps[:, 7, 0:N], lhsT=onesb[:], rhs=xn[:, c, 0:N], start=(c == 0), stop=(c == 7)), reads=[onesB, xnB], writes=[pb[7]])
                    S.op("act", lambda e: e.activation(out=rt[:, 0:N], in_=ps[:, 7, 0:N], func=AF.Sqrt, bias=epsc[:], scale=1.0 / D), reads=[pb[7], epsB], writes=[rtB])
                    S.op("dve", lambda e: e.reciprocal(out=rs[:, 0:N], in_=rt[:, 0:N]), reads=[rtB], writes=[rsB])
                    for c in range(8):
                        S.op("dve", lambda e: e.scalar_tensor_tensor(out=xn[:, c, 0:N], in0=h[:, c, 0:N], scalar=col(l, C_N2 + c), in1=rs[:, 0:N], op0=ALU.mult, op1=ALU.mult),
                             reads=[hB, colsB, rsB], writes=[xnB])
                    for j in range(22):
                        xa_, xaB_ = xa[pi % 2]
                        ya_, yaB_ = ya[pi % 2]
                        b0 = (pi % 2) * 2
                        pi += 1
                        for half in range(2):
                            wc = half * FF + j * 128
                            for c in range(8):
                                S.op("pe", lambda e: e.matmul(ps[:, b0 + half, 0:N], lhsT=wup[:, c, wc:wc + 128], rhs=xn[:, c, 0:N], start=(c == 0), stop=(c == 7)),
                                     reads=[wupB, xnB], writes=[pb[b0 + half]])
                        for half in range(2):
                            cj = half * 22 + j
                            S.op("act", lambda e: e.activation(out=xa_[:, half, 2:2 + N], in_=ps[:, b0 + half, 0:N], func=AF.Copy), reads=[pb[b0 + half]], writes=[xaB_])
                            S.op("pool", lambda e: e.tensor_copy(out=xa_[:, half, 0:2], in_=hist[:, cj, :]), reads=[histB], writes=[xaB_])
                            S.op("pool", lambda e: e.tensor_copy(out=hist[:, cj, :], in_=xa_[:, half, N:N + 2]), reads=[xaB_], writes=[histB])
                            S.op("dve", lambda e: e.tensor_scalar(out=ya_[:, half, 0:N], in0=xa_[:, half, 2:2 + N], scalar1=col(l, C_FW + 88 + cj), scalar2=col(l, C_FB + cj),
                                                                  op0=ALU.mult, op1=ALU.add), reads=[xaB_, colsB], writes=[yaB_])
                            S.op("dve", lambda e: e.scalar_tensor_tensor(out=ya_[:, half, 0:N], in0=xa_[:, half, 1:1 + N], scalar=col(l, C_FW + 44 + cj), in1=ya_[:, half, 0:N],
                                                                         op0=ALU.mult, op1=ALU.add), reads=[xaB_, colsB, yaB_], writes=[yaB_])
                            S.op("dve", lambda e: e.scalar_tensor_tensor(out=ya_[:, half, 0:N], in0=xa_[:, half, 0:N], scalar=col(l, C_FW + cj), in1=ya_[:, half, 0:N],
                                                                         op0=ALU.mult, op1=ALU.add), reads=[xaB_, colsB, yaB_], writes=[yaB_])
                        S.op("act", lambda e: e.activation(out=sa[:, 0:N], in_=ya_[:, 0, 0:N], func=AF.Silu), reads=[yaB_], writes=[saB])
                        S.op("pool", lambda e: e.tensor_tensor(out=gT[:, j, 0:N], in0=sa[:, 0:N], in1=ya_[:, 1, 0:N], op=ALU.mult), reads=[saB, yaB_], writes=[gTB])
                    for fo in range(8):
                        b = 4 + fo % 2
                        for c in range(22):
                            S.op("pe", lambda e: e.matmul(ps[:, b, 0:N], lhsT=wdn[:, c, fo * 128:(fo + 1) * 128], rhs=gT[:, c, 0:N], start=(c == 0), stop=(c == 21)),
                                 reads=[wdnB, gTB], writes=[pb[b]])
                        S.op("dve", lambda e: e.tensor_tensor(out=h[:, fo, 0:N], in0=h[:, fo, 0:N], in1=ps[:, b, 0:N], op=ALU.add), reads=[hB, pb[b]], writes=[hB])
                    if not last:
                        if ti == 0:
                            S.op("pool", lambda e: e.memset(h[:, :, 0:112], 0.0), reads=[hB], writes=[hB])
                        S.dma("pool", hT3[:, :, t0:t0 + N], h[:, :, 0:N], reads=[hB])
                    elif ti > 0:
                        for tbk in range(N // 128):
                            o_, oB_ = ot[oi % 2]
                            oi += 1
                            for fo in range(8):
                                S.op("pe", lambda e: e.transpose(ps[:, 6 + fo // 4, (fo % 4) * 128:(fo % 4 + 1) * 128], h[:, fo, tbk * 128:(tbk + 1) * 128], idf[:]),
                                     reads=[hB, idfB], writes=[pb[6 + fo // 4]])
                            S.op("act", lambda e: e.activation(out=o_[:].rearrange("p (a b) -> p a b", b=512), in_=ps[:, 6:8, :], func=AF.Copy),
                                 reads=[pb[6], pb[7]], writes=[oB_])
                            r0 = t0 - 128 + tbk * 128
                            S.dma("pool", y[r0:r0 + 128, :], o_[:], reads=[oB_])
                S.barrier()
            chk(f"C{l}")
        print("n_inst", S.n_inst, "nsem", S.nsem)
    except _Stop:
        pass
    return nc


_NAMES = ["meta_tokens", "hgrn_lb", "norm1_g", "w_in", "hg_norm_g", "w_hg_out", "cq_norm_g", "w_uq", "w_qi",
          "q_norm_g", "k_norm_g", "w_at_out", "cv_dw_w", "cv_dw_b", "cv_ln_g", "cv_ln_b", "w_cv_out", "w_mix_out",
          "norm2_g", "w_ffn_up", "ffn_dw_w", "ffn_dw_b", "w_ffn_down"]


def kernel(_nblk=65, _stop=None, _ncores=None, **inputs):
    x = np.asarray(inputs["x"], dtype=np.float32)
    B = x.shape[0] if _ncores is None else _ncores
    nx = (_nblk - 1) * 128
    nc = build(_nblk, stop=_stop)
    shared = {k: np.ascontiguousarray(np.asarray(inputs[k], dtype=np.float32)) for k in _NAMES}
    in_maps = []
    for b in range(B):
        m = dict(shared)
        m["x"] = np.ascontiguousarray(x[b, :nx])
        in_maps.append(m)
    res = run_bass_kernel_spmd(nc, in_maps, core_ids=list(range(B)))
    return np.stack([np.asarray(r["y"], dtype=np.float32) for r in res.results], axis=0)
```

```python
from contextlib import ExitStack

import numpy as np

import concourse.bass as bass
import concourse.mybir as mybir
from concourse.bass_utils import run_bass_kernel_spmd

F32 = mybir.dt.float32
BF16 = mybir.dt.bfloat16
AF = mybir.ActivationFunctionType
ALU = mybir.AluOpType
AX = mybir.AxisListType

SEM_LIMIT = 30000
D = 1024
NIN = 6596
FF = 2816
EPS = 1e-6
CQ_, CF_, CI_, CG_, CCQ_, CKA_, CVA_, CKI_, CWI_, CU_, CGT_ = 0, 512, 1024, 1536, 2048, 2304, 2368, 2432, 2496, 2500, 3524
ZQ, ZF, ZG, ZCQ, ZU, ZGT, ZROWS = 0, 512, 1024, 1536, 1792, 2816, 5888
NIT = 12
NEG = -30000.0
KSEL = 240


class Ev:
    __slots__ = ("sem", "val", "eng")

    def __init__(self, sem, val, eng):
        self.sem = sem
        self.val = val
        self.eng = eng


class Buf:
    def __init__(self, name):
        self.name = name
        self.w = None
        self.r = {}


class Sched:
    def __init__(self, nc, es):
        self.nc = nc
        self.es = es
        self.eng = {"pe": nc.tensor, "act": nc.scalar, "dve": nc.vector,
                    "pool": nc.gpsimd, "sp": nc.sync}
        self.sem = {}
        self.cnt = {}
        self.nsem = 0
        for e in self.eng:
            self._new_sem(e)
        self.waited = {e: {} for e in self.eng}
        self.dsems = {}
        self.all_dsems = []
        self.n_inst = 0

    def _alloc_sem(self, name):
        self.nsem += 1
        return self.es.enter_context(self.nc.semaphore(f"{name}_{self.nsem}"))

    def _new_sem(self, e):
        self.sem[e] = self._alloc_sem("s" + e)
        self.cnt[e] = 0

    def _wait(self, e, ev):
        if ev is None:
            return
        key = id(ev.sem)
        if self.waited[e].get(key, 0) >= ev.val:
            return
        self.eng[e].wait_ge(ev.sem, ev.val)
        self.waited[e][key] = ev.val
        self.n_inst += 1

    def _deps(self, e, reads, writes, is_dma=False):
        for b in reads:
            ev = b.w
            if ev is None:
                continue
            if ev.eng == e and e == "pe" and not is_dma:
                continue
            self._wait(e, ev)
        for b in writes:
            ev = b.w
            if ev is not None and (ev.eng != e or is_dma):
                self._wait(e, ev)
            for rev in b.r.values():
                if rev.eng != e or is_dma:
                    self._wait(e, rev)

    def op(self, e, fn, reads=(), writes=()):
        self._deps(e, reads, writes)
        inst = fn(self.eng[e])
        if self.cnt[e] >= SEM_LIMIT:
            self._new_sem(e)
        inst.then_inc(self.sem[e], 1)
        self.cnt[e] += 1
        self.n_inst += 1
        ev = Ev(self.sem[e], self.cnt[e], e)
        for b in reads:
            b.r[e] = ev
        for b in writes:
            b.w = ev
            b.r = {}
        return ev

    def dma(self, q, out, in_, reads=(), writes=(), semname=None, **kw):
        self._deps(q, reads, writes, is_dma=True)
        if semname is None:
            semname = (writes[0].name if writes else reads[0].name)
        ent = self.dsems.get(semname)
        if ent is None or ent[1] + 16 > SEM_LIMIT:
            ent = [self._alloc_sem("d"), 0]
            self.dsems[semname] = ent
            self.all_dsems.append(ent)
        inst = self.eng[q].dma_start(out=out, in_=in_, **kw)
        inst.then_inc(ent[0], 16)
        ent[1] += 16
        self.n_inst += 1
        ev = Ev(ent[0], ent[1], "dma")
        for b in reads:
            b.r["dma" + semname] = ev
        for b in writes:
            b.w = ev
            b.r = {}
        return ev

    def barrier(self):
        evs = [Ev(self.sem[e], self.cnt[e], e) for e in self.eng if self.cnt[e] > 0]
        for e in self.eng:
            for ev in evs:
                if ev.eng != e:
                    self._wait(e, ev)
            for ent in self.all_dsems:
                if ent[1] > 0:
                    self._wait(e, Ev(ent[0], ent[1], "dma"))


def tiles(nblk, n):
    out = [(0, 128)]
    t = 128
    tp = nblk * 128
    while t < tp:
        w = min(n, tp - t)
        out.append((t, w))
        t += w
    return out


class _Stop(Exception):
    pass


def build(nblk, depth=2, stop=None):
    TP = nblk * 128
    NX = (nblk - 1) * 128
    nc = bass.Bass("TRN2", target_bir_lowering=False)

    def din(name, shape):
        return nc.dram_tensor(name, shape, F32, kind="ExternalInput").ap()

    x = din("x", [NX, D])
    meta = din("meta_tokens", [16, D])
    hgrn_lb = din("hgrn_lb", [depth, 512])
    norm1_g = din("norm1_g", [depth, D])
    w_in = din("w_in", [depth, D, NIN])
    hg_norm_g = din("hg_norm_g", [depth, 128])
    w_hg_out = din("w_hg_out", [depth, 512, D])
    cq_norm_g = din("cq_norm_g", [depth, 256])
    w_uq = din("w_uq", [depth, 256, 512])
    w_qi = din("w_qi", [depth, 256, 256])
    q_norm_g = din("q_norm_g", [depth, 64])
    k_norm_g = din("k_norm_g", [depth, 64])
    w_at_out = din("w_at_out", [depth, 512, D])
    cv_dw_w = din("cv_dw_w", [depth, 31, 512])
    cv_dw_b = din("cv_dw_b", [depth, 512])
    cv_ln_g = din("cv_ln_g", [depth, 512])
    cv_ln_b = din("cv_ln_b", [depth, 512])
    w_cv_out = din("w_cv_out", [depth, 512, D])
    w_mix_out = din("w_mix_out", [depth, D, D])
    norm2_g = din("norm2_g", [depth, D])
    w_ffn_up = din("w_ffn_up", [depth, D, 2 * FF])
    ffn_dw_w = din("ffn_dw_w", [depth, 3, 2 * FF])
    ffn_dw_b = din("ffn_dw_b", [depth, 2 * FF])
    w_ffn_down = din("w_ffn_down", [depth, FF, D])
    y = nc.dram_tensor("y", [NX, D], F32, kind="ExternalOutput").ap()

    hT = nc.dram_tensor("hT_scr", [D, TP], F32).ap()
    zT = nc.dram_tensor("zT_scr", [ZROWS, TP], F32).ap()
    vhg = nc.dram_tensor("vhg_scr", [TP, 512], BF16).ap()
    aos = nc.dram_tensor("ao_scr", [64, 8, TP], BF16).ap()
    hT3 = hT.rearrange("(c p) t -> p c t", p=128)

    ges = ExitStack()
    try:
      with ges:
        S = Sched(nc, ges)

        uniq = [0]

        def alloc(es, name, shape, dt=F32):
            uniq[0] += 1
            return es.enter_context(nc.sbuf_tensor(f"{name}_{uniq[0]}", shape, dt)), Buf(name)

        ps = ges.enter_context(nc.psum_tensor("ps", [128, 8, 512], F32))
        pb = [Buf(f"psb{i}") for i in range(8)]

        idf, idfB = alloc(ges, "idf", [128, 128])
        idb, idbB = alloc(ges, "idb", [128, 128], BF16)
        id4, id4B = alloc(ges, "id4", [128, 4, 128], BF16)
        onesb, onesB = alloc(ges, "onesb", [128, 128], BF16)
        shf, shfB = alloc(ges, "shf", [128, 64])
        cmask, cmaskB = alloc(ges, "cmask", [128, 128])
        tri, triB = alloc(ges, "tri", [64, 64])
        mb0f, mb0fB = alloc(ges, "mb0f", [128, 128])
        mb0, mb0B = alloc(ges, "mb0", [128, 128], BF16)
        padb, padbB = alloc(ges, "padb", [128, 1])
        rm, rmB = alloc(ges, "rm", [128, 512])
        p2t, p2tB = alloc(ges, "p2t", [128, NIT])
        epsc, epsB = alloc(ges, "epsc", [128, 1])
        NCOL = 340
        cols, colsB = alloc(ges, "cols", [128, depth, NCOL])
        lbc, lbB = alloc(ges, "lbc", [128, depth, 4])
        omlc, omlB = alloc(ges, "omlc", [128, depth, 4])

        def asel(t, B, pattern, cmp, fill, base, cm):
            S.op("pool", lambda e: e.affine_select(out=t, in_=t, pattern=pattern, compare_op=cmp,
                                                   fill=fill, base=base, channel_multiplier=cm),
                 reads=[B], writes=[B])

        S.op("pool", lambda e: e.memset(idf[:], 0.0), writes=[idfB])
        asel(idf[:], idfB, [[-1, 128]], ALU.not_equal, 1.0, 0, 1)
        S.op("dve", lambda e: e.tensor_copy(out=idb[:], in_=idf[:]), reads=[idfB], writes=[idbB])
        for j in range(4):
            S.op("dve", lambda e: e.tensor_copy(out=id4[:, j, :], in_=idf[:]), reads=[idfB], writes=[id4B])
        S.op("pool", lambda e: e.memset(onesb[:], 1.0), writes=[onesB])
        S.op("pool", lambda e: e.memset(shf[:], 0.0), writes=[shfB])
        asel(shf[:], shfB, [[-1, 64]], ALU.not_equal, 1.0, -64, 1)
        S.op("pool", lambda e: e.memset(cmask[:], 0.0), writes=[cmaskB])
        asel(cmask[:], cmaskB, [[-1, 128]], ALU.is_ge, -1e30, 0, 1)
        S.op("pool", lambda e: e.memset(tri[:], 1.0), writes=[triB])
        asel(tri[:], triB, [[1, 64]], ALU.is_ge, 0.0, 0, -1)
        S.op("pool", lambda e: e.memset(mb0f[:], 0.0), writes=[mb0fB])
        asel(mb0f[:], mb0fB, [[-1, 128]], ALU.is_ge, NEG, 0, 1)
        asel(mb0f[:], mb0fB, [[1, 128]], ALU.is_ge, NEG, -112, 0)
        asel(mb0f[:], mb0fB, [[-1, 128]], ALU.not_equal, 0.0, 0, 1)
        S.op("dve", lambda e: e.tensor_copy(out=mb0[:], in_=mb0f[:]), reads=[mb0fB], writes=[mb0B])
        S.op("pool", lambda e: e.memset(padb[:], 0.0), writes=[padbB])
        asel(padb[:], padbB, [[0, 1]], ALU.is_ge, NEG, -112, 1)
        S.op("pool", lambda e: e.memset(rm[:], 1.0), writes=[rmB])
        S.op("pool", lambda e: e.memset(rm[:].rearrange("p (c k) -> p c k", k=64)[:, :, 0:1], 0.0),
             reads=[rmB], writes=[rmB])
        for i in range(NIT):
            S.op("pool", lambda e: e.memset(p2t[:, i:i + 1], 2.0 ** -(i + 1)), writes=[p2tB])
        S.op("pool", lambda e: e.memset(epsc[:], EPS), writes=[epsB])
        S.op("pool", lambda e: e.memset(cols[:], 0.0), writes=[colsB])

        C_N1, C_N2, C_HGG, C_CQG, C_QG, C_KG = 0, 8, 16, 17, 19, 20
        C_CVW, C_CVB, C_LNG, C_LNB, C_FW, C_FB = 21, 145, 149, 153, 157, 289
        C_LB = 333
        pst, pstB = alloc(ges, "pstage", [128, 128])
        bank_rr = [0]

        def load_cols(l, src2d, n, off, npart=128):
            S.dma("sp", pst[0:n, 0:npart], src2d, writes=[pstB])
            b = bank_rr[0] % 4
            bank_rr[0] += 1
            S.op("pe", lambda e: e.transpose(ps[0:npart, b, 0:n], pst[0:n, 0:npart], idf[0:n, 0:n]),
                 reads=[pstB, idfB], writes=[pb[b]])
            S.op("dve", lambda e: e.tensor_copy(out=cols[0:npart, l, off:off + n], in_=ps[0:npart, b, 0:n]),
                 reads=[pb[b]], writes=[colsB])

        for l in range(depth):
            load_cols(l, norm1_g[l].rearrange("(c p) -> c p", p=128), 8, C_N1)
            load_cols(l, norm2_g[l].rearrange("(c p) -> c p", p=128), 8, C_N2)
            load_cols(l, hg_norm_g[l].rearrange("(c p) -> c p", p=128), 1, C_HGG)
            load_cols(l, cq_norm_g[l].rearrange("(c p) -> c p", p=128), 2, C_CQG)
            load_cols(l, q_norm_g[l].rearrange("(c p) -> c p", p=64), 1, C_QG, npart=64)
            load_cols(l, k_norm_g[l].rearrange("(c p) -> c p", p=64), 1, C_KG, npart=64)
            load_cols(l, cv_dw_w[l].rearrange("j (c p) -> (j c) p", p=128), 124, C_CVW)
            load_cols(l, cv_dw_b[l].rearrange("(c p) -> c p", p=128), 4, C_CVB)
            load_cols(l, cv_ln_g[l].rearrange("(c p) -> c p", p=128), 4, C_LNG)
            load_cols(l, cv_ln_b[l].rearrange("(c p) -> c p", p=128), 4, C_LNB)
            for j in range(3):
                load_cols(l, ffn_dw_w[l, j].rearrange("(c p) -> c p", p=128), 44, C_FW + 44 * j)
            load_cols(l, ffn_dw_b[l].rearrange("(c p) -> c p", p=128), 44, C_FB)
            load_cols(l, hgrn_lb[l].rearrange("(c p) -> c p", p=128), 4, C_LB)
            S.op("dve", lambda e: e.tensor_scalar(out=cols[0:64, l, C_QG:C_QG + 1], in0=cols[0:64, l, C_QG:C_QG + 1],
                                                  scalar1=0.125, scalar2=None, op0=ALU.mult),
                 reads=[colsB], writes=[colsB])
        S.op("pool", lambda e: e.memset(lbc[:], 0.0), writes=[lbB])
        S.op("pool", lambda e: e.memset(omlc[:], 1.0), writes=[omlB])
        if depth == 2:
            S.op("dve", lambda e: e.tensor_tensor(out=lbc[:, 1, :], in0=cols[:, 1, C_LB:C_LB + 4],
                                                  in1=cols[:, 0, C_LB:C_LB + 4], op=ALU.subtract),
                 reads=[colsB], writes=[lbB])
            S.op("act", lambda e: e.activation(out=lbc[:, 1, :], in_=lbc[:, 1, :], func=AF.Sigmoid),
                 reads=[lbB], writes=[lbB])
            S.op("dve", lambda e: e.tensor_scalar(out=omlc[:, 1, :], in0=lbc[:, 1, :], scalar1=-1.0, scalar2=1.0,
                                                  op0=ALU.mult, op1=ALU.add), reads=[lbB], writes=[omlB])

        def chk(tag):
            if stop == tag:
                S.barrier()
                raise _Stop()

        chk("prologue")

        def col(l, c, n=1, npart=128):
            return cols[0:npart, l, c:c + n]

        rr = {"ev": 0}

        def evac_eng():
            rr["ev"] += 1
            return "act" if rr["ev"] % 2 == 0 else "dve"

        def copy_op(e_name, out, in_, reads, writes):
            if e_name == "act":
                S.op("act", lambda e: e.activation(out=out, in_=in_, func=AF.Copy), reads=reads, writes=writes)
            else:
                S.op(e_name, lambda e: e.tensor_copy(out=out, in_=in_), reads=reads, writes=writes)

        def load_weight(es, name, src3, kp, kc, ncols, stage, stageB, piece):
            wt, wB = alloc(es, name, [kp, kc, ncols], BF16)
            i = 0
            for c in range(kc):
                for c0 in range(0, ncols, piece):
                    w = min(piece, ncols - c0)
                    sl = i % 2
                    S.dma("sp", stage[sl][0:kp, 0:w], src3[:, c, c0:c0 + w], writes=[stageB[sl]])
                    eng = "pool" if i % 2 == 0 else "dve"
                    S.op(eng, lambda e: e.tensor_copy(out=wt[:, c, c0:c0 + w], in_=stage[sl][0:kp, 0:w]),
                         reads=[stageB[sl]], writes=[wB])
                    i += 1
            return wt, wB

        with ExitStack() as pes:
            xs, xsB = alloc(pes, "xs", [128, 4, D])
            hb = [alloc(pes, f"hbI{i}", [128, 8, 512]) for i in range(2)]
            for ti, (t0, N) in enumerate(tiles(nblk, 512)):
                h, hB = hb[ti % 2]
                nb = N // 128
                if ti == 0:
                    S.op("pool", lambda e: e.memset(h[:, :, 0:128], 0.0), writes=[hB])
                    S.dma("sp", xs[0:16, 0, :], meta[:, :], writes=[xsB])
                    for c in range(8):
                        b = c % 4
                        S.op("pe", lambda e: e.transpose(ps[:, b, 0:16], xs[0:16, 0, c * 128:(c + 1) * 128], idf[0:16, 0:16]),
                             reads=[xsB, idfB], writes=[pb[b]])
                        copy_op(evac_eng(), h[:, c, 112:128], ps[:, b, 0:16], [pb[b]], [hB])
                else:
                    r0 = t0 - 128
                    S.dma("sp", xs[:, 0:nb, :], x[r0:r0 + N, :].rearrange("(j p) d -> p j d", p=128), writes=[xsB])
                    for c in range(8):
                        b = c % 4
                        for j in range(nb):
                            S.op("pe", lambda e: e.transpose(ps[:, b, j * 128:(j + 1) * 128], xs[:, j, c * 128:(c + 1) * 128], idf[:]),
                                 reads=[xsB, idfB], writes=[pb[b]])
                        copy_op(evac_eng(), h[:, c, 0:N], ps[:, b, 0:N], [pb[b]], [hB])
                S.dma("pool", hT3[:, :, t0:t0 + N], h[:, :, 0:N], reads=[hB])
            S.barrier()
        chk("I")

        for l in range(depth):
            last = (l == depth - 1)
            with ExitStack() as les:
                kT, kTB = alloc(les, "kT", [64, TP], BF16)
                kiT, kiTB = alloc(les, "kiT", [64, TP], BF16)
                Va, VaB = alloc(les, "Va", [128, nblk, 128], BF16)
                Wx, WxB = alloc(les, "Wx", [128, nblk, 4])
                S.op("pool", lambda e: e.memset(Va[:], 1.0), writes=[VaB])

                with ExitStack() as pes:
                    h, hB = alloc(pes, "hA", [128, 8, 512])
                    hflat = h[:].rearrange("p c n -> p (c n)")
                    stage = [hflat[:, 0:1649], hflat[:, 2048:2048 + 1649]]
                    wb, wbB = load_weight(pes, "w_in_bf", w_in[l].rearrange("(c p) n -> p c n", p=128), 128, 8, NIN,
                                          stage, [hB, hB], 1649)
                    xn, xnB = alloc(pes, "xnA", [128, 8, 512], BF16)
                    rt, rtB = alloc(pes, "rtA", [128, 512])
                    rs, rsB = alloc(pes, "rsA", [128, 512])
                    zs = [alloc(pes, f"zsA{i}", [128, 512]) for i in range(4)]
                    vst = [alloc(pes, f"vstA{i}", [128, 512], BF16) for i in range(2)]
                    kraw, krawB = alloc(pes, "krawA", [64, 512])
                    ksq, ksqB = alloc(pes, "ksqA", [64, 512], BF16)
                    groups = []
                    for (wc, zr, n) in ((CQ_, ZQ, 4), (CF_, ZF, 4), (CG_, ZG, 4), (CCQ_, ZCQ, 2), (CU_, ZU, 8), (CGT_, ZGT, 24)):
                        for i in range(n):
                            groups.append((wc + i * 128, zr + i * 128))
                    gi = 0
                    for ti, (t0, N) in enumerate(tiles(nblk, 512)):
                        nb = N // 128
                        S.dma("sp", h[:, :, 0:N], hT3[:, :, t0:t0 + N], writes=[hB])
                        S.op("act", lambda e: e.activation(out=xn[:, :, 0:N], in_=h[:, :, 0:N], func=AF.Square),
                             reads=[hB], writes=[xnB])
                        for c in range(8):
                            S.op("pe", lambda e: e.matmul(ps[:, 7, 0:N], lhsT=onesb[:], rhs=xn[:, c, 0:N], start=(c == 0), stop=(c == 7)),
                                 reads=[onesB, xnB], writes=[pb[7]])
                        S.op("act", lambda e: e.activation(out=rt[:, 0:N], in_=ps[:, 7, 0:N], func=AF.Sqrt, bias=epsc[:], scale=1.0 / D),
                             reads=[pb[7], epsB], writes=[rtB])
                        S.op("dve", lambda e: e.reciprocal(out=rs[:, 0:N], in_=rt[:, 0:N]), reads=[rtB], writes=[rsB])
                        for c in range(8):
                            S.op("dve", lambda e: e.scalar_tensor_tensor(out=xn[:, c, 0:N], in0=h[:, c, 0:N], scalar=col(l, C_N1 + c),
                                                                         in1=rs[:, 0:N], op0=ALU.mult, op1=ALU.mult),
                                 reads=[hB, colsB, rsB], writes=[xnB])
                        for (wc, zr) in groups:
                            b = gi % 4
                            z, zB = zs[gi % 4]
                            gi += 1
                            for c in range(8):
                                S.op("pe", lambda e: e.matmul(ps[:, b, 0:N], lhsT=wb[:, c, wc:wc + 128], rhs=xn[:, c, 0:N], start=(c == 0), stop=(c == 7)),
                                     reads=[wbB, xnB], writes=[pb[b]])
                            copy_op(evac_eng(), z[:, 0:N], ps[:, b, 0:N], [pb[b]], [zB])
                            S.dma("pool", zT[zr:zr + 128, t0:t0 + N], z[:, 0:N], reads=[zB])
                        for c in range(8):
                            S.op("pe", lambda e: e.matmul(ps[0:64, 4, 0:N], lhsT=wb[:, c, CKA_:CKA_ + 64], rhs=xn[:, c, 0:N], start=(c == 0), stop=(c == 7)),
                                 reads=[wbB, xnB], writes=[pb[4]])
                        S.op("act", lambda e: e.activation(out=kraw[:, 0:N], in_=ps[0:64, 4, 0:N], func=AF.Copy), reads=[pb[4]], writes=[krawB])
                        S.op("act", lambda e: e.activation(out=ksq[:, 0:N], in_=kraw[:, 0:N], func=AF.Square), reads=[krawB], writes=[ksqB])
                        S.op("pe", lambda e: e.matmul(ps[0:64, 5, 0:N], lhsT=onesb[0:64, 0:64], rhs=ksq[:, 0:N], start=True, stop=True),
                             reads=[onesB, ksqB], writes=[pb[5]])
                        S.op("act", lambda e: e.activation(out=rt[0:64, 0:N], in_=ps[0:64, 5, 0:N], func=AF.Sqrt, bias=epsc[0:64, :], scale=1.0 / 64),
                             reads=[pb[5], epsB], writes=[rtB])
                        S.op("dve", lambda e: e.reciprocal(out=rs[0:64, 0:N], in_=rt[0:64, 0:N]), reads=[rtB], writes=[rsB])
                        S.op("dve", lambda e: e.scalar_tensor_tensor(out=kT[:, t0:t0 + N], in0=kraw[:, 0:N], scalar=col(l, C_KG, 1, 64),
                                                                     in1=rs[0:64, 0:N], op0=ALU.mult, op1=ALU.mult),
                             reads=[krawB, colsB, rsB], writes=[kTB])
                        for c in range(8):
                            S.op("pe", lambda e: e.matmul(ps[0:64, 6, 0:N], lhsT=wb[:, c, CKI_:CKI_ + 64], rhs=xn[:, c, 0:N], start=(c == 0), stop=(c == 7)),
                                 reads=[wbB, xnB], writes=[pb[6]])
                        S.op("act", lambda e: e.activation(out=kiT[:, t0:t0 + N], in_=ps[0:64, 6, 0:N], func=AF.Copy), reads=[pb[6]], writes=[kiTB])
                        for j in range(nb):
                            blk = t0 // 128 + j
                            b = gi % 4
                            vs, vsB = vst[gi % 2]
                            gi += 1
                            for c in range(8):
                                S.op("pe", lambda e: e.matmul(ps[:, b, :], lhsT=xn[:, c, j * 128:(j + 1) * 128], rhs=wb[:, c, CI_:CI_ + 512], start=(c == 0), stop=(c == 7)),
                                     reads=[wbB, xnB], writes=[pb[b]])
                            copy_op(evac_eng(), vs[:], ps[:, b, :], [pb[b]], [vsB])
                            S.dma("pool", vhg[t0 + j * 128:t0 + (j + 1) * 128, :], vs[:], reads=[vsB])
                            b = gi % 4
                            gi += 1
                            for c in range(8):
                                S.op("pe", lambda e: e.matmul(ps[:, b, 0:64], lhsT=xn[:, c, j * 128:(j + 1) * 128], rhs=wb[:, c, CVA_:CVA_ + 64], start=(c == 0), stop=(c == 7)),
                                     reads=[wbB, xnB], writes=[pb[b]])
                            for c in range(8):
                                S.op("pe", lambda e: e.matmul(ps[:, b, 64:68], lhsT=xn[:, c, j * 128:(j + 1) * 128], rhs=wb[:, c, CWI_:CWI_ + 4], start=(c == 0), stop=(c == 7)),
                                     reads=[wbB, xnB], writes=[pb[b]])
                            S.op("act", lambda e: e.activation(out=Va[:, blk, 0:64], in_=ps[:, b, 0:64], func=AF.Copy), reads=[pb[b]], writes=[VaB])
                            S.op("dve", lambda e: e.tensor_scalar(out=Wx[:, blk, :], in0=ps[:, b, 64:68], scalar1=1.0 / 16, scalar2=None, op0=ALU.mult),
                                 reads=[pb[b]], writes=[WxB])
                    S.barrier()
                chk(f"A{l}")

                with ExitStack() as pes:
                    stg = [alloc(pes, f"stgB2{i}", [128, 512]) for i in range(2)]
                    wuq, wuqB = load_weight(pes, "wuq", w_uq[l].rearrange("(c p) n -> p c n", p=128), 128, 2, 512,
                                            [s[0] for s in stg], [s[1] for s in stg], 512)
                    wqi, wqiB = load_weight(pes, "wqi", w_qi[l].rearrange("(c p) n -> p c n", p=128), 128, 2, 256,
                                            [s[0] for s in stg], [s[1] for s in stg], 512)
                    LMAX = max(128, (nblk - 1) * 128)
                    I, IB = alloc(pes, "Isc", [128, LMAX])
                    mbs = [alloc(pes, f"mb{i}", [128, LMAX], BF16) for i in range(2)]
                    cq, cqB = alloc(pes, "cq", [128, 2, 512])
                    cqn, cqnB = alloc(pes, "cqn", [128, 2, 512], BF16)
                    rt, rtB = alloc(pes, "rtB2", [128, 512])
                    rs, rsB = alloc(pes, "rsB2", [128, 512])
                    qraw, qrawB = alloc(pes, "qraw", [64, 512])
                    qsq, qsqB = alloc(pes, "qsq", [64, 512], BF16)
                    qd, qdB = alloc(pes, "qd", [64, 8, 512], BF16)
                    qid, qidB = alloc(pes, "qid", [64, 4, 512], BF16)
                    rb = [alloc(pes, f"rb{i}", [128, 512]) for i in range(2)]
                    pt = [alloc(pes, f"pt{i}", [128, 1024], BF16) for i in range(2)]
                    rden, rdenB = alloc(pes, "rden", [128, 1024])
                    rsh, rshB = alloc(pes, "rsh", [64, 1024])
                    aot, aotB = alloc(pes, "aot", [64, 8, 512], BF16)
                    sm, smB = alloc(pes, "smB2", [128, 8])
                    steps, stepsB = alloc(pes, "steps", [128, NIT])
                    S.op("pool", lambda e: e.memset(rden[:], 0.0), writes=[rdenB])
                    bi = 0
                    pti = 0
                    lgi = [0]
                    for ti, (t0, N) in enumerate(tiles(nblk, 512)):
                        nb = N // 128
                        S.dma("sp", cq[:, :, 0:N], zT[ZCQ:ZCQ + 256, t0:t0 + N].rearrange("(c p) t -> p c t", p=128), writes=[cqB])
                        S.op("act", lambda e: e.activation(out=cqn[:, :, 0:N], in_=cq[:, :, 0:N], func=AF.Square), reads=[cqB], writes=[cqnB])
                        for c in range(2):
                            S.op("pe", lambda e: e.matmul(ps[:, 0, 0:N], lhsT=onesb[:], rhs=cqn[:, c, 0:N], start=(c == 0), stop=(c == 1)),
                                 reads=[onesB, cqnB], writes=[pb[0]])
                        S.op("act", lambda e: e.activation(out=rt[:, 0:N], in_=ps[:, 0, 0:N], func=AF.Sqrt, bias=epsc[:], scale=1.0 / 256),
                             reads=[pb[0], epsB], writes=[rtB])
                        S.op("dve", lambda e: e.reciprocal(out=rs[:, 0:N], in_=rt[:, 0:N]), reads=[rtB], writes=[rsB])
                        for c in range(2):
                            S.op("dve", lambda e: e.scalar_tensor_tensor(out=cqn[:, c, 0:N], in0=cq[:, c, 0:N], scalar=col(l, C_CQG + c),
                                                                         in1=rs[:, 0:N], op0=ALU.mult, op1=ALU.mult),
                                 reads=[cqB, colsB, rsB], writes=[cqnB])
                        for hh in range(8):
                            b = bi % 2
                            bi += 1
                            for c in range(2):
                                S.op("pe", lambda e: e.matmul(ps[0:64, b, 0:N], lhsT=wuq[:, c, hh * 64:(hh + 1) * 64], rhs=cqn[:, c, 0:N], start=(c == 0), stop=(c == 1)),
                                     reads=[wuqB, cqnB], writes=[pb[b]])
                            S.op("act", lambda e: e.activation(out=qraw[:, 0:N], in_=ps[0:64, b, 0:N], func=AF.Copy), reads=[pb[b]], writes=[qrawB])
                            S.op("act", lambda e: e.activation(out=qsq[:, 0:N], in_=qraw[:, 0:N], func=AF.Square), reads=[qrawB], writes=[qsqB])
                            S.op("pe", lambda e: e.matmul(ps[0:64, 2, 0:N], lhsT=onesb[0:64, 0:64], rhs=qsq[:, 0:N], start=True, stop=True),
                                 reads=[onesB, qsqB], writes=[pb[2]])
                            S.op("act", lambda e: e.activation(out=rt[0:64, 0:N], in_=ps[0:64, 2, 0:N], func=AF.Sqrt, bias=epsc[0:64, :], scale=1.0 / 64),
                                 reads=[pb[2], epsB], writes=[rtB])
                            S.op("dve", lambda e: e.reciprocal(out=rs[0:64, 0:N], in_=rt[0:64, 0:N]), reads=[rtB], writes=[rsB])
                            S.op("dve", lambda e: e.scalar_tensor_tensor(out=qd[:, hh, 0:N], in0=qraw[:, 0:N], scalar=col(l, C_QG, 1, 64),
                                                                         in1=rs[0:64, 0:N], op0=ALU.mult, op1=ALU.mult),
                                 reads=[qrawB, colsB, rsB], writes=[qdB])
                        for hh in range(4):
                            b = bi % 2
                            bi += 1
                            for c in range(2):
                                S.op("pe", lambda e: e.matmul(ps[0:64, b, 0:N], lhsT=wqi[:, c, hh * 64:(hh + 1) * 64], rhs=cqn[:, c, 0:N], start=(c == 0), stop=(c == 1)),
                                     reads=[wqiB, cqnB], writes=[pb[b]])
                            copy_op(evac_eng(), qid[:, hh, 0:N], ps[0:64, b, 0:N], [pb[b]], [qidB])
                        for j in range(nb):
                            bq = t0 // 128 + j
                            qc0, qc1 = j * 128, (j + 1) * 128
                            L = 128 * bq
                            mb, mbB = mbs[bq % 2]
                            if bq >= 1:
                                for st in range(0, L, 512):
                                    w = min(512, L - st)
                                    for hh in range(4):
                                        b = bi % 2
                                        bi += 1
                                        r, rB = rb[bi % 2]
                                        S.op("pe", lambda e: e.matmul(ps[:, b, 0:w], lhsT=qid[:, hh, qc0:qc1], rhs=kiT[:, 128 + st:128 + st + w], start=True, stop=True),
                                             reads=[qidB, kiTB], writes=[pb[b]])
                                        S.op("act", lambda e: e.activation(out=r[:, 0:w], in_=ps[:, b, 0:w], func=AF.Relu), reads=[pb[b]], writes=[rB])
                                        if hh == 0:
                                            S.op("dve", lambda e: e.tensor_scalar(out=I[:, st:st + w], in0=r[:, 0:w], scalar1=Wx[:, bq, 0:1], scalar2=None, op0=ALU.mult),
                                                 reads=[rB, WxB], writes=[IB])
                                        else:
                                            S.op("dve", lambda e: e.scalar_tensor_tensor(out=I[:, st:st + w], in0=r[:, 0:w], scalar=Wx[:, bq, hh:hh + 1],
                                                                                         in1=I[:, st:st + w], op0=ALU.mult, op1=ALU.add),
                                                 reads=[rB, WxB, IB], writes=[IB])
                                S.op("dve", lambda e: e.tensor_reduce(out=sm[:, 0:1], in_=I[:, 0:L], axis=AX.X, op=ALU.max, apply_absolute_value=True),
                                     reads=[IB], writes=[smB])
                                S.op("dve", lambda e: e.tensor_tensor(out=I[:, L - 128:L], in0=I[:, L - 128:L], in1=cmask[:], op=ALU.add),
                                     reads=[IB, cmaskB], writes=[IB])
                                S.op("dve", lambda e: e.tensor_scalar(out=sm[:, 1:2], in0=sm[:, 0:1], scalar1=-1.001, scalar2=-1e-6, op0=ALU.mult, op1=ALU.add),
                                     reads=[smB], writes=[smB])
                                S.op("dve", lambda e: e.tensor_scalar(out=sm[:, 2:3], in0=sm[:, 0:1], scalar1=2.002, scalar2=2e-6, op0=ALU.mult, op1=ALU.add),
                                     reads=[smB], writes=[smB])
                                S.op("dve", lambda e: e.tensor_scalar(out=steps[:], in0=p2t[:], scalar1=sm[:, 2:3], scalar2=None, op0=ALU.mult),
                                     reads=[p2tB, smB], writes=[stepsB])
                                for it in range(NIT):
                                    S.op("dve", lambda e: e.tensor_tensor(out=sm[:, 3:4], in0=sm[:, 1:2], in1=steps[:, it:it + 1], op=ALU.add),
                                         reads=[smB, stepsB], writes=[smB])
                                    S.op("dve", lambda e: e.tensor_scalar(out=mb[:, 0:L], in0=I[:, 0:L], scalar1=sm[:, 3:4], scalar2=None,
                                                                          op0=ALU.is_ge, op1=ALU.add, accum_out=sm[:, 4:5]),
                                         reads=[IB, smB], writes=[mbB, smB])
                                    S.op("dve", lambda e: e.tensor_scalar(out=sm[:, 5:6], in0=sm[:, 4:5], scalar1=KSEL - 0.5, scalar2=steps[:, it:it + 1],
                                                                          op0=ALU.is_ge, op1=ALU.mult),
                                         reads=[smB, stepsB], writes=[smB])
                                    S.op("dve", lambda e: e.tensor_tensor(out=sm[:, 1:2], in0=sm[:, 1:2], in1=sm[:, 5:6], op=ALU.add),
                                         reads=[smB], writes=[smB])
                                S.op("dve", lambda e: e.tensor_scalar(out=mb[:, 0:L], in0=I[:, 0:L], scalar1=sm[:, 1:2], scalar2=NEG,
                                                                      op0=ALU.is_lt, op1=ALU.mult),
                                     reads=[IB, smB], writes=[mbB])
                            def emit_qk(c):
                                lp = 2 + 2 * (lgi[0] % 2)
                                lgi[0] += 1
                                has_mask = (bq == 0) or (c >= 1)
                                for half in range(2):
                                    S.op("pe", lambda e: e.matmul(ps[:, lp + half, :].rearrange("p (a b) -> p a b", b=128),
                                                                  lhsT=kT[:, c * 128:(c + 1) * 128], rhs=qd[:, half * 4:half * 4 + 4, qc0:qc1],
                                                                  start=True, stop=(not has_mask)),
                                         reads=[kTB, qdB], writes=[pb[lp + half]])
                                    if has_mask:
                                        if bq == 0:
                                            S.op("pe", lambda e: e.matmul(ps[:, lp + half, :].rearrange("p (a b) -> p a b", b=128),
                                                                          lhsT=mb0[:], rhs=id4[:], start=False, stop=True),
                                                 reads=[mb0B, id4B], writes=[pb[lp + half]])
                                        else:
                                            S.op("pe", lambda e: e.matmul(ps[:, lp + half, :].rearrange("p (a b) -> p a b", b=128),
                                                                          lhsT=mb[:, (c - 1) * 128:c * 128], rhs=id4[:], start=False, stop=True),
                                                 reads=[mbB, id4B], writes=[pb[lp + half]])
                                return lp

                            lp_next = emit_qk(0)
                            for c in range(bq + 1):
                                lp = lp_next
                                if c + 1 <= bq:
                                    lp_next = emit_qk(c + 1)
                                p, pB = pt[pti % 2]
                                pti += 1
                                if c == 0 and bq >= 1:
                                    S.op("act", lambda e: e.activation(out=p[:].rearrange("p (a b) -> p a b", b=512), in_=ps[:, lp:lp + 2, :], func=AF.Exp, bias=padb[:]),
                                         reads=[pb[lp], pb[lp + 1], padbB], writes=[pB])
                                else:
                                    S.op("act", lambda e: e.activation(out=p[:].rearrange("p (a b) -> p a b", b=512), in_=ps[:, lp:lp + 2, :], func=AF.Exp),
                                         reads=[pb[lp], pb[lp + 1]], writes=[pB])
                                for half in range(2):
                                    S.op("pe", lambda e: e.matmul(ps[:, 6 + half, :], lhsT=Va[:, c, :], rhs=p[:, half * 512:(half + 1) * 512],
                                                                  start=(c == 0), stop=(c == bq)),
                                         reads=[VaB, pB], writes=[pb[6 + half]])
                            lp = 2 + 2 * (lgi[0] % 2)
                            lgi[0] += 1
                            S.op("dve", lambda e: e.reciprocal(out=rden[64:128, :].rearrange("p (a b) -> p a b", b=512), in_=ps[64:128, 6:8, :]),
                                 reads=[pb[6], pb[7]], writes=[rdenB])
                            for half in range(2):
                                S.op("pe", lambda e: e.matmul(ps[0:64, lp + half, :], lhsT=shf[:], rhs=rden[:, half * 512:(half + 1) * 512], start=True, stop=True),
                                     reads=[shfB, rdenB], writes=[pb[lp + half]])
                            S.op("act", lambda e: e.activation(out=rsh[:].rearrange("p (a b) -> p a b", b=512), in_=ps[0:64, lp:lp + 2, :], func=AF.Copy),
                                 reads=[pb[lp], pb[lp + 1]], writes=[rshB])
                            for half in range(2):
                                S.op("dve", lambda e: e.tensor_tensor(out=aot[:, half * 4:half * 4 + 4, qc0:qc1],
                                                                      in0=ps[0:64, 6 + half, :].rearrange("p (a b) -> p a b", b=128),
                                                                      in1=rsh[:, half * 512:(half + 1) * 512].rearrange("p (a b) -> p a b", b=128), op=ALU.mult),
                                     reads=[pb[6 + half], rshB], writes=[aotB])
                        S.dma("pool", aos[:, :, t0:t0 + N], aot[:, :, 0:N], reads=[aotB])
                    S.barrier()
                chk(f"B2{l}")

            NB1 = 256
            with ExitStack() as pes:
                stg = [alloc(pes, f"stgB1{i}", [128, 1024]) for i in range(2)]
                sT = [s[0] for s in stg]
                sB = [s[1] for s in stg]
                whg, whgB = load_weight(pes, "whg", w_hg_out[l].rearrange("(c p) n -> p c n", p=128), 128, 4, D, sT, sB, 1024)
                wat, watB = load_weight(pes, "wat", w_at_out[l].rearrange("(c p) n -> p c n", p=64), 64, 8, D, sT, sB, 1024)
                wcv, wcvB = load_weight(pes, "wcv", w_cv_out[l].rearrange("(c p) n -> p c n", p=128), 128, 4, D, sT, sB, 1024)
                wmx, wmxB = load_weight(pes, "wmx", w_mix_out[l].rearrange("(c p) n -> p c n", p=128), 128, 8, D, sT, sB, 1024)
                Sst, SstB = alloc(pes, "Sst", [128, 4, 128])
                Sp, SpB = alloc(pes, "Sp", [128, 128], BF16)
                HB, HBB = alloc(pes, "HB", [128, 4, 30 + NB1], BF16)
                dg, dgB = alloc(pes, "dg", [128, 124, 128], BF16)
                S.op("pool", lambda e: e.memset(Sst[:], 0.0), writes=[SstB])
                S.op("pool", lambda e: e.memset(HB[:], 0.0), writes=[HBB])
                for idx in range(124):
                    S.op("dve" if idx % 2 == 0 else "pool",
                         lambda e: e.tensor_scalar(out=dg[:, idx, :], in0=idf[:], scalar1=col(l, C_CVW + idx), scalar2=None, op0=ALU.mult),
                         reads=[idfB, colsB], writes=[dgB])
                fq, fqB = alloc(pes, "fq", [128, 3, 4, NB1])
                vt, vtB = alloc(pes, "vt", [64, NB1 // 64, 512], BF16)
                tA, tAB = alloc(pes, "tA", [128, NB1])
                tB_, tBB = alloc(pes, "tB", [128, NB1])
                tb, tbB = alloc(pes, "tb", [128, NB1])
                tbm, tbmB = alloc(pes, "tbm", [128, NB1])
                teq, teqB = alloc(pes, "teq", [128, NB1])
                tek, tekB = alloc(pes, "tek", [128, NB1])
                tqs, tqsB = alloc(pes, "tqs", [128, NB1])
                qtl, qtlB = alloc(pes, "qtl", [128, NB1], BF16)
                ktl, ktlB = alloc(pes, "ktl", [128, NB1], BF16)
                ee, eeB = alloc(pes, "ee", [128, 3, NB1 // 64])
                ktok = [alloc(pes, f"ktok{i}", [64, 128], BF16) for i in range(2)]
                att = [alloc(pes, f"att{i}", [64, 64], BF16) for i in range(2)]
                osq, osqB = alloc(pes, "osq", [128, NB1], BF16)
                rt, rtB = alloc(pes, "rtB1", [128, NB1])
                rs, rsB = alloc(pes, "rsB1", [128, NB1])
                gs, gsB = alloc(pes, "gs", [128, NB1])
                on, onB = alloc(pes, "on", [128, NB1])
                oT, oTB = alloc(pes, "oT", [128, 4, NB1], BF16)
                u, uB = alloc(pes, "u", [128, 8, NB1])
                sgb, sgbB = alloc(pes, "sgb", [128, 4, NB1])
                acc, accB = alloc(pes, "acc", [128, 4, NB1])
                accb, accbB = alloc(pes, "accb", [128, 4, NB1], BF16)
                sqb, sqbB = alloc(pes, "sqb", [128, 4, NB1], BF16)
                mu, muB = alloc(pes, "mu", [128, NB1])
                var, varB = alloc(pes, "var", [128, NB1])
                cvo, cvoB = alloc(pes, "cvo", [128, 4, NB1], BF16)
                aot, aotB = alloc(pes, "aotB1", [64, 8, NB1], BF16)
                gl = [alloc(pes, f"gl{i}", [128, 3, NB1]) for i in range(2)]
                m1, m1B = alloc(pes, "m1", [128, NB1])
                m2, m2B = alloc(pes, "m2", [128, NB1])
                m3, m3B = alloc(pes, "m3", [128, NB1])
                mT, mTB = alloc(pes, "mT", [128, 8, NB1], BF16)
                h, hB = alloc(pes, "hB1", [128, 8, NB1])
                zgt = zT[ZGT:ZGT + 3072, :].rearrange("(br fo p) t -> p br fo t", br=3, fo=8, p=128)
                ki = 0
                gli = 0
                for ti, (t0, N) in enumerate(tiles(nblk, NB1)):
                    nch = N // 64
                    S.dma("sp", fq[:, 0, :, 0:N], zT[ZF:ZF + 512, t0:t0 + N].rearrange("(c p) t -> p c t", p=128), writes=[fqB])
                    S.dma("sp", fq[:, 1, :, 0:N], zT[ZQ:ZQ + 512, t0:t0 + N].rearrange("(c p) t -> p c t", p=128), writes=[fqB])
                    S.dma("sp", fq[:, 2, :, 0:N], zT[ZG:ZG + 512, t0:t0 + N].rearrange("(c p) t -> p c t", p=128), writes=[fqB])
                    S.dma("sp", vt[:, 0:nch, :], vhg[t0:t0 + N, :].rearrange("(c p) v -> p c v", p=64), writes=[vtB])
                    S.dma("sp", u[:, :, 0:N], zT[ZU:ZU + 1024, t0:t0 + N].rearrange("(c p) t -> p c t", p=128), writes=[uB])
                    S.dma("sp", aot[:, :, 0:N], aos[:, :, t0:t0 + N], writes=[aotB])
                    S.dma("sp", h[:, :, 0:N], hT3[:, :, t0:t0 + N], writes=[hB])
                    for hh in range(4):
                        S.op("act", lambda e: e.activation(out=tA[:, 0:N], in_=fq[:, 0, hh, 0:N], func=AF.Sigmoid), reads=[fqB], writes=[tAB])
                        S.op("dve", lambda e: e.tensor_scalar(out=tA[:, 0:N], in0=tA[:, 0:N], scalar1=omlc[:, l, hh:hh + 1], scalar2=lbc[:, l, hh:hh + 1],
                                                              op0=ALU.mult, op1=ALU.add), reads=[tAB, omlB, lbB], writes=[tAB])
                        S.op("act", lambda e: e.activation(out=tB_[:, 0:N], in_=tA[:, 0:N], func=AF.Ln), reads=[tAB], writes=[tBB])
                        S.op("dve", lambda e: e.tensor_scalar(out=tA[:, 0:N], in0=tA[:, 0:N], scalar1=-1.0, scalar2=1.0, op0=ALU.mult, op1=ALU.add),
                             reads=[tAB], writes=[tAB])
                        S.op("dve", lambda e: e.tensor_tensor_scan(out=tb[:, 0:N], data0=rm[:, 0:N], data1=tB_[:, 0:N], initial=0.0, op0=ALU.mult, op1=ALU.add),
                             reads=[rmB, tBB], writes=[tbB])
                        b3 = tb[:, 0:N].rearrange("p (c k) -> p c k", k=64)
                        bmid = bass.AP(tb, 31, [[NB1, 128], [64, nch], [0, 64]])
                        S.op("dve", lambda e: e.tensor_tensor(out=tbm[:, 0:N].rearrange("p (c k) -> p c k", k=64), in0=b3, in1=bmid, op=ALU.subtract),
                             reads=[tbB], writes=[tbmB])
                        S.op("act", lambda e: e.activation(out=teq[:, 0:N], in_=tbm[:, 0:N], func=AF.Exp), reads=[tbmB], writes=[teqB])
                        S.op("act", lambda e: e.activation(out=tek[:, 0:N], in_=tbm[:, 0:N], func=AF.Exp, scale=-1.0), reads=[tbmB], writes=[tekB])
                        S.op("act", lambda e: e.activation(out=tqs[:, 0:N], in_=fq[:, 1, hh, 0:N], func=AF.Silu), reads=[fqB], writes=[tqsB])
                        S.op("dve", lambda e: e.scalar_tensor_tensor(out=qtl[:, 0:N], in0=tqs[:, 0:N], scalar=128.0 ** -0.5, in1=teq[:, 0:N], op0=ALU.mult, op1=ALU.mult),
                             reads=[tqsB, teqB], writes=[qtlB])
                        S.op("dve", lambda e: e.tensor_tensor(out=ktl[:, 0:N], in0=tA[:, 0:N], in1=tek[:, 0:N], op=ALU.mult), reads=[tAB, tekB], writes=[ktlB])
                        bm3 = tbm[:, 0:N].rearrange("p (c k) -> p c k", k=64)
                        S.op("act", lambda e: e.activation(out=ee[:, 0, 0:nch], in_=b3[:, :, 31], func=AF.Exp), reads=[tbB], writes=[eeB])
                        S.op("act", lambda e: e.activation(out=ee[:, 1, 0:nch], in_=b3[:, :, 63], func=AF.Exp), reads=[tbB], writes=[eeB])
                        S.op("act", lambda e: e.activation(out=ee[:, 2, 0:nch], in_=bm3[:, :, 63], func=AF.Exp), reads=[tbmB], writes=[eeB])
                        ob = hh % 2
                        for ch in range(nch):
                            c0, c1 = ch * 64, (ch + 1) * 64
                            kt_, ktB_ = ktok[ki % 2]
                            at_, atB_ = att[ki % 2]
                            ki += 1
                            psbf = ps[:, 2, :].bitcast(BF16)
                            S.op("pe", lambda e: e.transpose(psbf[0:64, 0:128], ktl[:, c0:c1], idb[:]), reads=[ktlB, idbB], writes=[pb[2]])
                            S.op("act", lambda e: e.activation(out=kt_[:], in_=psbf[0:64, 0:128], func=AF.Copy), reads=[pb[2]], writes=[ktB_])
                            S.op("pe", lambda e: e.matmul(ps[0:64, 3, 0:64], lhsT=ktl[:, c0:c1], rhs=qtl[:, c0:c1], start=True, stop=True),
                                 reads=[ktlB, qtlB], writes=[pb[3]])
                            S.op("dve", lambda e: e.tensor_tensor(out=at_[:], in0=ps[0:64, 3, 0:64], in1=tri[:], op=ALU.mult), reads=[pb[3], triB], writes=[atB_])
                            S.op("dve", lambda e: e.tensor_scalar(out=Sp[:], in0=Sst[:, hh, :], scalar1=ee[:, 0, ch:ch + 1], scalar2=None, op0=ALU.mult),
                                 reads=[SstB, eeB], writes=[SpB])
                            S.op("pe", lambda e: e.matmul(ps[:, ob, c0:c1], lhsT=vt[:, ch, hh * 128:(hh + 1) * 128], rhs=at_[:], start=True, stop=False),
                                 reads=[vtB, atB_], writes=[pb[ob]])
                            S.op("pe", lambda e: e.matmul(ps[:, ob, c0:c1], lhsT=Sp[:], rhs=qtl[:, c0:c1], start=False, stop=True),
                                 reads=[SpB, qtlB], writes=[pb[ob]])
                            S.op("pe", lambda e: e.matmul(ps[:, 4, 0:128], lhsT=kt_[:], rhs=vt[:, ch, hh * 128:(hh + 1) * 128], start=True, stop=True),
                                 reads=[ktB_, vtB], writes=[pb[4]])
                            S.op("dve", lambda e: e.tensor_scalar(out=Sst[:, hh, :], in0=Sst[:, hh, :], scalar1=ee[:, 1, ch:ch + 1], scalar2=None, op0=ALU.mult),
                                 reads=[SstB, eeB], writes=[SstB])
                            S.op("dve", lambda e: e.scalar_tensor_tensor(out=Sst[:, hh, :], in0=ps[:, 4, 0:128], scalar=ee[:, 2, ch:ch + 1], in1=Sst[:, hh, :],
                                                                         op0=ALU.mult, op1=ALU.add), reads=[pb[4], eeB, SstB], writes=[SstB])
                        S.op("act", lambda e: e.activation(out=osq[:, 0:N], in_=ps[:, ob, 0:N], func=AF.Square), reads=[pb[ob]], writes=[osqB])
                        S.op("pe", lambda e: e.matmul(ps[:, 5, 0:N], lhsT=onesb[:], rhs=osq[:, 0:N], start=True, stop=True), reads=[onesB, osqB], writes=[pb[5]])
                        S.op("act", lambda e: e.activation(out=rt[:, 0:N], in_=ps[:, 5, 0:N], func=AF.Sqrt, bias=epsc[:], scale=1.0 / 128),
                             reads=[pb[5], epsB], writes=[rtB])
                        S.op("dve", lambda e: e.reciprocal(out=rs[:, 0:N], in_=rt[:, 0:N]), reads=[rtB], writes=[rsB])
                        S.op("act", lambda e: e.activation(out=gs[:, 0:N], in_=fq[:, 2, hh, 0:N], func=AF.Silu), reads=[fqB], writes=[gsB])
                        S.op("dve", lambda e: e.scalar_tensor_tensor(out=on[:, 0:N], in0=ps[:, ob, 0:N], scalar=col(l, C_HGG), in1=rs[:, 0:N], op0=ALU.mult, op1=ALU.mult),
                             reads=[pb[ob], colsB, rsB], writes=[onB])
                        S.op("dve", lambda e: e.tensor_tensor(out=oT[:, hh, 0:N], in0=on[:, 0:N], in1=gs[:, 0:N], op=ALU.mult), reads=[onB, gsB], writes=[oTB])
                    S.op("act", lambda e: e.activation(out=sgb[:, :, 0:N], in_=u[:, 4:8, 0:N], func=AF.Sigmoid), reads=[uB], writes=[sgbB])
                    S.op("dve", lambda e: e.tensor_tensor(out=HB[:, :, 30:30 + N], in0=u[:, 0:4, 0:N], in1=sgb[:, :, 0:N], op=ALU.mult),
                         reads=[uB, sgbB], writes=[HBB])
                    for c in range(4):
                        cbk = 6 + c % 2
                        for j in range(31):
                            S.op("pe", lambda e: e.matmul(ps[:, cbk, 0:N], lhsT=dg[:, j * 4 + c, :], rhs=HB[:, c, j:j + N], start=(j == 0), stop=(j == 30)),
                                 reads=[dgB, HBB], writes=[pb[cbk]])
                        S.op("act", lambda e: e.activation(out=acc[:, c, 0:N], in_=ps[:, cbk, 0:N], func=AF.Identity, bias=col(l, C_CVB + c)),
                             reads=[pb[cbk], colsB], writes=[accB])
                    S.op("dve", lambda e: e.tensor_copy(out=HB[:, :, 0:30], in_=HB[:, :, N:N + 30]), reads=[HBB], writes=[HBB])
                    S.op("act", lambda e: e.activation(out=accb[:, :, 0:N], in_=acc[:, :, 0:N], func=AF.Copy), reads=[accB], writes=[accbB])
                    S.op("act", lambda e: e.activation(out=sqb[:, :, 0:N], in_=acc[:, :, 0:N], func=AF.Square), reads=[accB], writes=[sqbB])
                    for c in range(4):
                        S.op("pe", lambda e: e.matmul(ps[:, 6, 0:N], lhsT=onesb[:], rhs=accb[:, c, 0:N], start=(c == 0), stop=(c == 3)), reads=[onesB, accbB], writes=[pb[6]])
                    for c in range(4):
                        S.op("pe", lambda e: e.matmul(ps[:, 7, 0:N], lhsT=onesb[:], rhs=sqb[:, c, 0:N], start=(c == 0), stop=(c == 3)), reads=[onesB, sqbB], writes=[pb[7]])
                    S.op("act", lambda e: e.activation(out=mu[:, 0:N], in_=ps[:, 6, 0:N], func=AF.Copy, scale=1.0 / 512), reads=[pb[6]], writes=[muB])
                    S.op("dve", lambda e: e.tensor_tensor(out=var[:, 0:N], in0=mu[:, 0:N], in1=mu[:, 0:N], op=ALU.mult), reads=[muB], writes=[varB])
                    S.op("dve", lambda e: e.scalar_tensor_tensor(out=var[:, 0:N], in0=ps[:, 7, 0:N], scalar=1.0 / 512, in1=var[:, 0:N], op0=ALU.mult, op1=ALU.subtract),
                         reads=[pb[7], varB], writes=[varB])
                    S.op("act", lambda e: e.activation(out=rt[:, 0:N], in_=var[:, 0:N], func=AF.Sqrt, bias=epsc[:]), reads=[varB, epsB], writes=[rtB])
                    S.op("dve", lambda e: e.reciprocal(out=rs[:, 0:N], in_=rt[:, 0:N]), reads=[rtB], writes=[rsB])
                    for c in range(4):
                        S.op("dve", lambda e: e.tensor_tensor(out=acc[:, c, 0:N], in0=acc[:, c, 0:N], in1=mu[:, 0:N], op=ALU.subtract), reads=[accB, muB], writes=[accB])
                        S.op("dve", lambda e: e.tensor_tensor(out=acc[:, c, 0:N], in0=acc[:, c, 0:N], in1=rs[:, 0:N], op=ALU.mult), reads=[accB, rsB], writes=[accB])
                        S.op("act", lambda e: e.activation(out=cvo[:, c, 0:N], in_=acc[:, c, 0:N], func=AF.Silu, scale=col(l, C_LNG + c), bias=col(l, C_LNB + c)),
                             reads=[accB, colsB], writes=[cvoB])
                    for fo in range(8):
                        g_, gB_ = gl[gli % 2]
                        gli += 1
                        f0, f1 = fo * 128, (fo + 1) * 128
                        S.dma("sp", g_[:, :, 0:N], zgt[:, :, fo, t0:t0 + N], writes=[gB_])
                        S.op("act", lambda e: e.activation(out=g_[:, :, 0:N], in_=g_[:, :, 0:N], func=AF.Sigmoid), reads=[gB_], writes=[gB_])
                        for c in range(4):
                            S.op("pe", lambda e: e.matmul(ps[:, 0, 0:N], lhsT=whg[:, c, f0:f1], rhs=oT[:, c, 0:N], start=(c == 0), stop=(c == 3)), reads=[whgB, oTB], writes=[pb[0]])
                        for c in range(8):
                            S.op("pe", lambda e: e.matmul(ps[:, 1, 0:N], lhsT=wat[:, c, f0:f1], rhs=aot[:, c, 0:N], start=(c == 0), stop=(c == 7)), reads=[watB, aotB], writes=[pb[1]])
                        for c in range(4):
                            S.op("pe", lambda e: e.matmul(ps[:, 2, 0:N], lhsT=wcv[:, c, f0:f1], rhs=cvo[:, c, 0:N], start=(c == 0), stop=(c == 3)), reads=[wcvB, cvoB], writes=[pb[2]])
                        S.op("dve", lambda e: e.tensor_tensor(out=m1[:, 0:N], in0=ps[:, 0, 0:N], in1=g_[:, 0, 0:N], op=ALU.mult), reads=[pb[0], gB_], writes=[m1B])
                        S.op("dve", lambda e: e.tensor_tensor(out=m2[:, 0:N], in0=ps[:, 1, 0:N], in1=g_[:, 1, 0:N], op=ALU.mult), reads=[pb[1], gB_], writes=[m2B])
                        S.op("dve", lambda e: e.tensor_tensor(out=m3[:, 0:N], in0=ps[:, 2, 0:N], in1=g_[:, 2, 0:N], op=ALU.mult), reads=[pb[2], gB_], writes=[m3B])
                        S.op("pool", lambda e: e.tensor_tensor(out=m1[:, 0:N], in0=m1[:, 0:N], in1=m2[:, 0:N], op=ALU.add), reads=[m1B, m2B], writes=[m1B])
                        S.op("pool", lambda e: e.tensor_tensor(out=mT[:, fo, 0:N], in0=m1[:, 0:N], in1=m3[:, 0:N], op=ALU.add), reads=[m1B, m3B], writes=[mTB])
                    for fo in range(8):
                        b = 3 + fo % 2
                        for c in range(8):
                            S.op("pe", lambda e: e.matmul(ps[:, b, 0:N], lhsT=wmx[:, c, fo * 128:(fo + 1) * 128], rhs=mT[:, c, 0:N], start=(c == 0), stop=(c == 7)),
                                 reads=[wmxB, mTB], writes=[pb[b]])
                        S.op("dve", lambda e: e.tensor_tensor(out=h[:, fo, 0:N], in0=h[:, fo, 0:N], in1=ps[:, b, 0:N], op=ALU.add), reads=[hB, pb[b]], writes=[hB])
                    if ti == 0:
                        S.op("pool", lambda e: e.memset(h[:, :, 0:112], 0.0), reads=[hB], writes=[hB])
                    S.dma("pool", hT3[:, :, t0:t0 + N], h[:, :, 0:N], reads=[hB])
                S.barrier()
            chk(f"B1{l}")

            NC_ = 256
            with ExitStack() as pes:
                stg = [alloc(pes, f"stgC{i}", [128, 1408]) for i in range(2)]
                sT = [s[0] for s in stg]
                sB = [s[1] for s in stg]
                wup, wupB = load_weight(pes, "wup", w_ffn_up[l].rearrange("(c p) n -> p c n", p=128), 128, 8, 2 * FF, sT, sB, 1408)
                wdn, wdnB = load_weight(pes, "wdn", w_ffn_down[l].rearrange("(c p) n -> p c n", p=128), 128, 22, D, sT, sB, 1024)
                h, hB = alloc(pes, "hC", [128, 8, NC_])
                xn, xnB = alloc(pes, "xnC", [128, 8, NC_], BF16)
                rt, rtB = alloc(pes, "rtC", [128, NC_])
                rs, rsB = alloc(pes, "rsC", [128, NC_])
                xa = [alloc(pes, f"xa{i}", [128, 2, 2 + NC_]) for i in range(3)]
                ya = [alloc(pes, f"ya{i}", [128, 2, NC_]) for i in range(3)]
                sas = [alloc(pes, f"sa{i}", [128, NC_]) for i in range(2)]
                gT, gTB = alloc(pes, "gT", [128, 22, NC_], BF16)
                hist, histB = alloc(pes, "hist", [128, 44, 2])
                ot = [alloc(pes, f"ot{i}", [128, D]) for i in range(2)]
                S.op("pool", lambda e: e.memset(hist[:], 0.0), writes=[histB])
                pi = 0
                oi = 0
                for ti, (t0, N) in enumerate(tiles(nblk, NC_)):
                    S.dma("sp", h[:, :, 0:N], hT3[:, :, t0:t0 + N], writes=[hB])
                    S.op("act", lambda e: e.activation(out=xn[:, :, 0:N], in_=h[:, :, 0:N], func=AF.Square), reads=[hB], writes=[xnB])
                    for c in range(8):
                        S.op("pe", lambda e: e.matmul(ps[:, 7, 0:N], lhsT=onesb[:], rhs=xn[:, c, 0:N], start=(c == 0), stop=(c == 7)), reads=[onesB, xnB], writes=[pb[7]])
                    S.op("act", lambda e: e.activation(out=rt[:, 0:N], in_=ps[:, 7, 0:N], func=AF.Sqrt, bias=epsc[:], scale=1.0 / D), reads=[pb[7], epsB], writes=[rtB])
                    S.op("dve", lambda e: e.reciprocal(out=rs[:, 0:N], in_=rt[:, 0:N]), reads=[rtB], writes=[rsB])
                    for c in range(8):
                        S.op("dve", lambda e: e.scalar_tensor_tensor(out=xn[:, c, 0:N], in0=h[:, c, 0:N], scalar=col(l, C_N2 + c), in1=rs[:, 0:N], op0=ALU.mult, op1=ALU.mult),
                             reads=[hB, colsB, rsB], writes=[xnB])
                    pend = None
                    for j in range(22):
                        xa_, xaB_ = xa[pi % 3]
                        ya_, yaB_ = ya[pi % 3]
                        b0 = (pi % 2) * 2
                        pi += 1
                        for half in range(2):
                            wc = half * FF + j * 128
                            for c in range(8):
                                S.op("pe", lambda e: e.matmul(ps[:, b0 + half, 0:N], lhsT=wup[:, c, wc:wc + 128], rhs=xn[:, c, 0:N], start=(c == 0), stop=(c == 7)),
                                     reads=[wupB, xnB], writes=[pb[b0 + half]])
                        for half in range(2):
                            cj = half * 22 + j
                            S.op("act", lambda e: e.activation(out=xa_[:, half, 2:2 + N], in_=ps[:, b0 + half, 0:N], func=AF.Copy), reads=[pb[b0 + half]], writes=[xaB_])
                            S.op("act", lambda e: e.activation(out=ya_[:, half, 0:N], in_=ps[:, b0 + half, 0:N], func=AF.Identity,
                                                               scale=col(l, C_FW + 88 + cj), bias=col(l, C_FB + cj)), reads=[pb[b0 + half], colsB], writes=[yaB_])
                            S.op("pool", lambda e: e.tensor_copy(out=xa_[:, half, 0:2], in_=hist[:, cj, :]), reads=[histB], writes=[xaB_])
                            S.op("pool", lambda e: e.tensor_copy(out=hist[:, cj, :], in_=xa_[:, half, N:N + 2]), reads=[xaB_], writes=[histB])
                        if pend is not None:
                            pend()
                        for half in range(2):
                            cj = half * 22 + j
                            S.op("dve", lambda e: e.scalar_tensor_tensor(out=ya_[:, half, 0:N], in0=xa_[:, half, 1:1 + N], scalar=col(l, C_FW + 44 + cj), in1=ya_[:, half, 0:N],
                                                                         op0=ALU.mult, op1=ALU.add), reads=[xaB_, colsB, yaB_], writes=[yaB_])
                            S.op("dve", lambda e: e.scalar_tensor_tensor(out=ya_[:, half, 0:N], in0=xa_[:, half, 0:N], scalar=col(l, C_FW + cj), in1=ya_[:, half, 0:N],
                                                                         op0=ALU.mult, op1=ALU.add), reads=[xaB_, colsB, yaB_], writes=[yaB_])

                        def pend(j=j, ya_=ya_, yaB_=yaB_):
                            sa_, saB_ = sas[j % 2]
                            S.op("act", lambda e: e.activation(out=sa_[:, 0:N], in_=ya_[:, 0, 0:N], func=AF.Silu), reads=[yaB_], writes=[saB_])
                            S.op("pool", lambda e: e.tensor_tensor(out=gT[:, j, 0:N], in0=sa_[:, 0:N], in1=ya_[:, 1, 0:N], op=ALU.mult), reads=[saB_, yaB_], writes=[gTB])
                    pend()
                    for fo in range(8):
                        b = 4 + fo % 2
                        for c in range(22):
                            S.op("pe", lambda e: e.matmul(ps[:, b, 0:N], lhsT=wdn[:, c, fo * 128:(fo + 1) * 128], rhs=gT[:, c, 0:N], start=(c == 0), stop=(c == 21)),
                                 reads=[wdnB, gTB], writes=[pb[b]])
                        S.op("dve", lambda e: e.tensor_tensor(out=h[:, fo, 0:N], in0=h[:, fo, 0:N], in1=ps[:, b, 0:N], op=ALU.add), reads=[hB, pb[b]], writes=[hB])
                    if not last:
                        if ti == 0:
                            S.op("pool", lambda e: e.memset(h[:, :, 0:112], 0.0), reads=[hB], writes=[hB])
                        S.dma("pool", hT3[:, :, t0:t0 + N], h[:, :, 0:N], reads=[hB])
                    elif ti > 0:
                        for tbk in range(N // 128):
                            o_, oB_ = ot[oi % 2]
                            oi += 1
                            for fo in range(8):
                                S.op("pe", lambda e: e.transpose(ps[:, 6 + fo // 4, (fo % 4) * 128:(fo % 4 + 1) * 128], h[:, fo, tbk * 128:(tbk + 1) * 128], idf[:]),
                                     reads=[hB, idfB], writes=[pb[6 + fo // 4]])
                            S.op("act", lambda e: e.activation(out=o_[:].rearrange("p (a b) -> p a b", b=512), in_=ps[:, 6:8, :], func=AF.Copy),
                                 reads=[pb[6], pb[7]], writes=[oB_])
                            r0 = t0 - 128 + tbk * 128
                            S.dma("pool", y[r0:r0 + 128, :], o_[:], reads=[oB_])
                S.barrier()
            chk(f"C{l}")
        print("n_inst", S.n_inst, "nsem", S.nsem)
    except _Stop:
        pass
    return nc


_NAMES = ["meta_tokens", "hgrn_lb", "norm1_g", "w_in", "hg_norm_g", "w_hg_out", "cq_norm_g", "w_uq", "w_qi",
          "q_norm_g", "k_norm_g", "w_at_out", "cv_dw_w", "cv_dw_b", "cv_ln_g", "cv_ln_b", "w_cv_out", "w_mix_out",
          "norm2_g", "w_ffn_up", "ffn_dw_w", "ffn_dw_b", "w_ffn_down"]


def kernel(_nblk=65, _stop=None, _ncores=None, **inputs):
    x = np.asarray(inputs["x"], dtype=np.float32)
    B = x.shape[0] if _ncores is None else _ncores
    nx = (_nblk - 1) * 128
    nc = build(_nblk, stop=_stop)
    shared = {k: np.ascontiguousarray(np.asarray(inputs[k], dtype=np.float32)) for k in _NAMES}
    in_maps = []
    for b in range(B):
        m = dict(shared)
        m["x"] = np.ascontiguousarray(x[b, :nx])
        in_maps.append(m)
    res = run_bass_kernel_spmd(nc, in_maps, core_ids=list(range(B)))
    return np.stack([np.asarray(r["y"], dtype=np.float32) for r in res.results], axis=0)
```

```python
from contextlib import ExitStack

import numpy as np

import concourse.bass as bass
import concourse.mybir as mybir
from concourse.bass_utils import run_bass_kernel_spmd

F32 = mybir.dt.float32
BF16 = mybir.dt.bfloat16
AF = mybir.ActivationFunctionType
ALU = mybir.AluOpType
AX = mybir.AxisListType

SEM_LIMIT = 30000
D = 1024
NIN = 6596
FF = 2816
EPS = 1e-6
CQ_, CF_, CI_, CG_, CCQ_, CKA_, CVA_, CKI_, CWI_, CU_, CGT_ = 0, 512, 1024, 1536, 2048, 2304, 2368, 2432, 2496, 2500, 3524
ZQ, ZF, ZG, ZCQ, ZU, ZGT, ZROWS = 0, 512, 1024, 1536, 1792, 2816, 5888
NIT = 12
NEG = -30000.0
KSEL = 240


class Ev:
    __slots__ = ("sem", "val", "eng")

    def __init__(self, sem, val, eng):
        self.sem = sem
        self.val = val
        self.eng = eng


class Buf:
    def __init__(self, name):
        self.name = name
        self.w = None
        self.r = {}


class Sched:
    def __init__(self, nc, es):
        self.nc = nc
        self.es = es
        self.eng = {"pe": nc.tensor, "act": nc.scalar, "dve": nc.vector,
                    "pool": nc.gpsimd, "sp": nc.sync}
        self.sem = {}
        self.cnt = {}
        self.nsem = 0
        for e in self.eng:
            self._new_sem(e)
        self.waited = {e: {} for e in self.eng}
        self.dsems = {}
        self.all_dsems = []
        self.n_inst = 0

    def _alloc_sem(self, name):
        self.nsem += 1
        return self.es.enter_context(self.nc.semaphore(f"{name}_{self.nsem}"))

    def _new_sem(self, e):
        self.sem[e] = self._alloc_sem("s" + e)
        self.cnt[e] = 0

    def _wait(self, e, ev):
        if ev is None:
            return
        key = id(ev.sem)
        if self.waited[e].get(key, 0) >= ev.val:
            return
        self.eng[e].wait_ge(ev.sem, ev.val)
        self.waited[e][key] = ev.val
        self.n_inst += 1

    def _deps(self, e, reads, writes, is_dma=False):
        for b in reads:
            ev = b.w
            if ev is None:
                continue
            if ev.eng == e and e == "pe" and not is_dma:
                continue
            self._wait(e, ev)
        for b in writes:
            ev = b.w
            if ev is not None and (ev.eng != e or is_dma):
                self._wait(e, ev)
            for rev in b.r.values():
                if rev.eng != e or is_dma:
                    self._wait(e, rev)

    def op(self, e, fn, reads=(), writes=()):
        self._deps(e, reads, writes)
        inst = fn(self.eng[e])
        if self.cnt[e] >= SEM_LIMIT:
            self._new_sem(e)
        inst.then_inc(self.sem[e], 1)
        self.cnt[e] += 1
        self.n_inst += 1
        ev = Ev(self.sem[e], self.cnt[e], e)
        for b in reads:
            b.r[e] = ev
        for b in writes:
            b.w = ev
            b.r = {}
        return ev

    def dma(self, q, out, in_, reads=(), writes=(), semname=None, **kw):
        self._deps(q, reads, writes, is_dma=True)
        if semname is None:
            semname = (writes[0].name if writes else reads[0].name)
        ent = self.dsems.get(semname)
        if ent is None or ent[1] + 16 > SEM_LIMIT:
            ent = [self._alloc_sem("d"), 0]
            self.dsems[semname] = ent
            self.all_dsems.append(ent)
        inst = self.eng[q].dma_start(out=out, in_=in_, **kw)
        inst.then_inc(ent[0], 16)
        ent[1] += 16
        self.n_inst += 1
        ev = Ev(ent[0], ent[1], "dma")
        for b in reads:
            b.r["dma" + semname] = ev
        for b in writes:
            b.w = ev
            b.r = {}
        return ev

    def barrier(self):
        evs = [Ev(self.sem[e], self.cnt[e], e) for e in self.eng if self.cnt[e] > 0]
        for e in self.eng:
            for ev in evs:
                if ev.eng != e:
                    self._wait(e, ev)
            for ent in self.all_dsems:
                if ent[1] > 0:
                    self._wait(e, Ev(ent[0], ent[1], "dma"))


def tiles(nblk, n):
    out = [(0, 128)]
    t = 128
    tp = nblk * 128
    while t < tp:
        w = min(n, tp - t)
        out.append((t, w))
        t += w
    return out


class _Stop(Exception):
    pass


def build(nblk, depth=2, stop=None):
    TP = nblk * 128
    NX = (nblk - 1) * 128
    nc = bass.Bass("TRN2", target_bir_lowering=False)

    def din(name, shape):
        return nc.dram_tensor(name, shape, F32, kind="ExternalInput").ap()

    x = din("x", [NX, D])
    meta = din("meta_tokens", [16, D])
    hgrn_lb = din("hgrn_lb", [depth, 512])
    norm1_g = din("norm1_g", [depth, D])
    w_in = din("w_in", [depth, D, NIN])
    hg_norm_g = din("hg_norm_g", [depth, 128])
    w_hg_out = din("w_hg_out", [depth, 512, D])
    cq_norm_g = din("cq_norm_g", [depth, 256])
    w_uq = din("w_uq", [depth, 256, 512])
    w_qi = din("w_qi", [depth, 256, 256])
    q_norm_g = din("q_norm_g", [depth, 64])
    k_norm_g = din("k_norm_g", [depth, 64])
    w_at_out = din("w_at_out", [depth, 512, D])
    cv_dw_w = din("cv_dw_w", [depth, 31, 512])
    cv_dw_b = din("cv_dw_b", [depth, 512])
    cv_ln_g = din("cv_ln_g", [depth, 512])
    cv_ln_b = din("cv_ln_b", [depth, 512])
    w_cv_out = din("w_cv_out", [depth, 512, D])
    w_mix_out = din("w_mix_out", [depth, D, D])
    norm2_g = din("norm2_g", [depth, D])
    w_ffn_up = din("w_ffn_up", [depth, D, 2 * FF])
    ffn_dw_w = din("ffn_dw_w", [depth, 3, 2 * FF])
    ffn_dw_b = din("ffn_dw_b", [depth, 2 * FF])
    w_ffn_down = din("w_ffn_down", [depth, FF, D])
    y = nc.dram_tensor("y", [NX, D], F32, kind="ExternalOutput").ap()

    hT = nc.dram_tensor("hT_scr", [D, TP], F32).ap()
    zT = nc.dram_tensor("zT_scr", [ZROWS, TP], F32).ap()
    vhg = nc.dram_tensor("vhg_scr", [TP, 512], BF16).ap()
    aos = nc.dram_tensor("ao_scr", [64, 8, TP], BF16).ap()
    hT3 = hT.rearrange("(c p) t -> p c t", p=128)

    ges = ExitStack()
    try:
      with ges:
        S = Sched(nc, ges)

        uniq = [0]

        def alloc(es, name, shape, dt=F32):
            uniq[0] += 1
            return es.enter_context(nc.sbuf_tensor(f"{name}_{uniq[0]}", shape, dt)), Buf(name)

        ps = ges.enter_context(nc.psum_tensor("ps", [128, 8, 512], F32))
        pb = [Buf(f"psb{i}") for i in range(8)]

        idf, idfB = alloc(ges, "idf", [128, 128])
        idb, idbB = alloc(ges, "idb", [128, 128], BF16)
        id4, id4B = alloc(ges, "id4", [128, 4, 128], BF16)
        onesb, onesB = alloc(ges, "onesb", [128, 128], BF16)
        shf, shfB = alloc(ges, "shf", [128, 64])
        cmask, cmaskB = alloc(ges, "cmask", [128, 128])
        tri, triB = alloc(ges, "tri", [64, 64])
        mb0f, mb0fB = alloc(ges, "mb0f", [128, 128])
        mb0, mb0B = alloc(ges, "mb0", [128, 128], BF16)
        padb, padbB = alloc(ges, "padb", [128, 1])
        rm, rmB = alloc(ges, "rm", [128, 512])
        p2t, p2tB = alloc(ges, "p2t", [128, NIT])
        epsc, epsB = alloc(ges, "epsc", [128, 1])
        NCOL = 340
        cols, colsB = alloc(ges, "cols", [128, depth, NCOL])
        lbc, lbB = alloc(ges, "lbc", [128, depth, 4])
        omlc, omlB = alloc(ges, "omlc", [128, depth, 4])

        def asel(t, B, pattern, cmp, fill, base, cm):
            S.op("pool", lambda e: e.affine_select(out=t, in_=t, pattern=pattern, compare_op=cmp,
                                                   fill=fill, base=base, channel_multiplier=cm),
                 reads=[B], writes=[B])

        S.op("pool", lambda e: e.memset(idf[:], 0.0), writes=[idfB])
        asel(idf[:], idfB, [[-1, 128]], ALU.not_equal, 1.0, 0, 1)
        S.op("dve", lambda e: e.tensor_copy(out=idb[:], in_=idf[:]), reads=[idfB], writes=[idbB])
        for j in range(4):
            S.op("dve", lambda e: e.tensor_copy(out=id4[:, j, :], in_=idf[:]), reads=[idfB], writes=[id4B])
        S.op("pool", lambda e: e.memset(onesb[:], 1.0), writes=[onesB])
        S.op("pool", lambda e: e.memset(shf[:], 0.0), writes=[shfB])
        asel(shf[:], shfB, [[-1, 64]], ALU.not_equal, 1.0, -64, 1)
        S.op("pool", lambda e: e.memset(cmask[:], 0.0), writes=[cmaskB])
        asel(cmask[:], cmaskB, [[-1, 128]], ALU.is_ge, -1e30, 0, 1)
        S.op("pool", lambda e: e.memset(tri[:], 1.0), writes=[triB])
        asel(tri[:], triB, [[1, 64]], ALU.is_ge, 0.0, 0, -1)
        S.op("pool", lambda e: e.memset(mb0f[:], 0.0), writes=[mb0fB])
        asel(mb0f[:], mb0fB, [[-1, 128]], ALU.is_ge, NEG, 0, 1)
        asel(mb0f[:], mb0fB, [[1, 128]], ALU.is_ge, NEG, -112, 0)
        asel(mb0f[:], mb0fB, [[-1, 128]], ALU.not_equal, 0.0, 0, 1)
        S.op("dve", lambda e: e.tensor_copy(out=mb0[:], in_=mb0f[:]), reads=[mb0fB], writes=[mb0B])
        S.op("pool", lambda e: e.memset(padb[:], 0.0), writes=[padbB])
        asel(padb[:], padbB, [[0, 1]], ALU.is_ge, NEG, -112, 1)
        S.op("pool", lambda e: e.memset(rm[:], 1.0), writes=[rmB])
        S.op("pool", lambda e: e.memset(rm[:].rearrange("p (c k) -> p c k", k=64)[:, :, 0:1], 0.0),
             reads=[rmB], writes=[rmB])
        for i in range(NIT):
            S.op("pool", lambda e: e.memset(p2t[:, i:i + 1], 2.0 ** -(i + 1)), writes=[p2tB])
        S.op("pool", lambda e: e.memset(epsc[:], EPS), writes=[epsB])
        S.op("pool", lambda e: e.memset(cols[:], 0.0), writes=[colsB])

        C_N1, C_N2, C_HGG, C_CQG, C_QG, C_KG = 0, 8, 16, 17, 19, 20
        C_CVW, C_CVB, C_LNG, C_LNB, C_FW, C_FB = 21, 145, 149, 153, 157, 289
        C_LB = 333
        pst, pstB = alloc(ges, "pstage", [128, 128])
        bank_rr = [0]

        def load_cols(l, src2d, n, off, npart=128):
            S.dma("sp", pst[0:n, 0:npart], src2d, writes=[pstB])
            b = bank_rr[0] % 4
            bank_rr[0] += 1
            S.op("pe", lambda e: e.transpose(ps[0:npart, b, 0:n], pst[0:n, 0:npart], idf[0:n, 0:n]),
                 reads=[pstB, idfB], writes=[pb[b]])
            S.op("dve", lambda e: e.tensor_copy(out=cols[0:npart, l, off:off + n], in_=ps[0:npart, b, 0:n]),
                 reads=[pb[b]], writes=[colsB])

        for l in range(depth):
            load_cols(l, norm1_g[l].rearrange("(c p) -> c p", p=128), 8, C_N1)
            load_cols(l, norm2_g[l].rearrange("(c p) -> c p", p=128), 8, C_N2)
            load_cols(l, hg_norm_g[l].rearrange("(c p) -> c p", p=128), 1, C_HGG)
            load_cols(l, cq_norm_g[l].rearrange("(c p) -> c p", p=128), 2, C_CQG)
            load_cols(l, q_norm_g[l].rearrange("(c p) -> c p", p=64), 1, C_QG, npart=64)
            load_cols(l, k_norm_g[l].rearrange("(c p) -> c p", p=64), 1, C_KG, npart=64)
            load_cols(l, cv_dw_w[l].rearrange("j (c p) -> (j c) p", p=128), 124, C_CVW)
            load_cols(l, cv_dw_b[l].rearrange("(c p) -> c p", p=128), 4, C_CVB)
            load_cols(l, cv_ln_g[l].rearrange("(c p) -> c p", p=128), 4, C_LNG)
            load_cols(l, cv_ln_b[l].rearrange("(c p) -> c p", p=128), 4, C_LNB)
            for j in range(3):
                load_cols(l, ffn_dw_w[l, j].rearrange("(c p) -> c p", p=128), 44, C_FW + 44 * j)
            load_cols(l, ffn_dw_b[l].rearrange("(c p) -> c p", p=128), 44, C_FB)
            load_cols(l, hgrn_lb[l].rearrange("(c p) -> c p", p=128), 4, C_LB)
            S.op("dve", lambda e: e.tensor_scalar(out=cols[0:64, l, C_QG:C_QG + 1], in0=cols[0:64, l, C_QG:C_QG + 1],
                                                  scalar1=0.125, scalar2=None, op0=ALU.mult),
                 reads=[colsB], writes=[colsB])
        S.op("pool", lambda e: e.memset(lbc[:], 0.0), writes=[lbB])
        S.op("pool", lambda e: e.memset(omlc[:], 1.0), writes=[omlB])
        if depth == 2:
            S.op("dve", lambda e: e.tensor_tensor(out=lbc[:, 1, :], in0=cols[:, 1, C_LB:C_LB + 4],
                                                  in1=cols[:, 0, C_LB:C_LB + 4], op=ALU.subtract),
                 reads=[colsB], writes=[lbB])
            S.op("act", lambda e: e.activation(out=lbc[:, 1, :], in_=lbc[:, 1, :], func=AF.Sigmoid),
                 reads=[lbB], writes=[lbB])
            S.op("dve", lambda e: e.tensor_scalar(out=omlc[:, 1, :], in0=lbc[:, 1, :], scalar1=-1.0, scalar2=1.0,
                                                  op0=ALU.mult, op1=ALU.add), reads=[lbB], writes=[omlB])

        def chk(tag):
            if stop == tag:
                S.barrier()
                raise _Stop()

        chk("prologue")

        def col(l, c, n=1, npart=128):
            return cols[0:npart, l, c:c + n]

        rr = {"ev": 0}

        def evac_eng():
            rr["ev"] += 1
            return "act" if rr["ev"] % 2 == 0 else "dve"

        def copy_op(e_name, out, in_, reads, writes):
            if e_name == "act":
                S.op("act", lambda e: e.activation(out=out, in_=in_, func=AF.Copy), reads=reads, writes=writes)
            else:
                S.op(e_name, lambda e: e.tensor_copy(out=out, in_=in_), reads=reads, writes=writes)

        def load_weight(es, name, src3, kp, kc, ncols, stage, stageB, piece):
            wt, wB = alloc(es, name, [kp, kc, ncols], BF16)
            i = 0
            for c in range(kc):
                for c0 in range(0, ncols, piece):
                    w = min(piece, ncols - c0)
                    sl = i % 2
                    S.dma("sp", stage[sl][0:kp, 0:w], src3[:, c, c0:c0 + w], writes=[stageB[sl]])
                    eng = "pool" if i % 2 == 0 else "dve"
                    S.op(eng, lambda e: e.tensor_copy(out=wt[:, c, c0:c0 + w], in_=stage[sl][0:kp, 0:w]),
                         reads=[stageB[sl]], writes=[wB])
                    i += 1
            return wt, wB

        with ExitStack() as pes:
            xs, xsB = alloc(pes, "xs", [128, 4, D])
            hb = [alloc(pes, f"hbI{i}", [128, 8, 512]) for i in range(2)]
            for ti, (t0, N) in enumerate(tiles(nblk, 512)):
                h, hB = hb[ti % 2]
                nb = N // 128
                if ti == 0:
                    S.op("pool", lambda e: e.memset(h[:, :, 0:128], 0.0), writes=[hB])
                    S.dma("sp", xs[0:16, 0, :], meta[:, :], writes=[xsB])
                    for c in range(8):
                        b = c % 4
                        S.op("pe", lambda e: e.transpose(ps[:, b, 0:16], xs[0:16, 0, c * 128:(c + 1) * 128], idf[0:16, 0:16]),
                             reads=[xsB, idfB], writes=[pb[b]])
                        copy_op(evac_eng(), h[:, c, 112:128], ps[:, b, 0:16], [pb[b]], [hB])
                else:
                    r0 = t0 - 128
                    S.dma("sp", xs[:, 0:nb, :], x[r0:r0 + N, :].rearrange("(j p) d -> p j d", p=128), writes=[xsB])
                    for c in range(8):
                        b = c % 4
                        for j in range(nb):
                            S.op("pe", lambda e: e.transpose(ps[:, b, j * 128:(j + 1) * 128], xs[:, j, c * 128:(c + 1) * 128], idf[:]),
                                 reads=[xsB, idfB], writes=[pb[b]])
                        copy_op(evac_eng(), h[:, c, 0:N], ps[:, b, 0:N], [pb[b]], [hB])
                S.dma("pool", hT3[:, :, t0:t0 + N], h[:, :, 0:N], reads=[hB])
            S.barrier()
        chk("I")

        for l in range(depth):
            last = (l == depth - 1)
            with ExitStack() as les:
                kT, kTB = alloc(les, "kT", [64, TP], BF16)
                kiT, kiTB = alloc(les, "kiT", [64, TP], BF16)
                Va, VaB = alloc(les, "Va", [128, nblk, 128], BF16)
                Wx, WxB = alloc(les, "Wx", [128, nblk, 4])
                S.op("pool", lambda e: e.memset(Va[:], 1.0), writes=[VaB])

                with ExitStack() as pes:
                    h, hB = alloc(pes, "hA", [128, 8, 512])
                    hflat = h[:].rearrange("p c n -> p (c n)")
                    stage = [hflat[:, 0:1649], hflat[:, 2048:2048 + 1649]]
                    wb, wbB = load_weight(pes, "w_in_bf", w_in[l].rearrange("(c p) n -> p c n", p=128), 128, 8, NIN,
                                          stage, [hB, hB], 1649)
                    xn, xnB = alloc(pes, "xnA", [128, 8, 512], BF16)
                    rt, rtB = alloc(pes, "rtA", [128, 512])
                    rs, rsB = alloc(pes, "rsA", [128, 512])
                    zs = [alloc(pes, f"zsA{i}", [128, 512]) for i in range(4)]
                    vst = [alloc(pes, f"vstA{i}", [128, 512], BF16) for i in range(2)]
                    kraw, krawB = alloc(pes, "krawA", [64, 512])
                    ksq, ksqB = alloc(pes, "ksqA", [64, 512], BF16)
                    groups = []
                    for (wc, zr, n) in ((CQ_, ZQ, 4), (CF_, ZF, 4), (CG_, ZG, 4), (CCQ_, ZCQ, 2), (CU_, ZU, 8), (CGT_, ZGT, 24)):
                        for i in range(n):
                            groups.append((wc + i * 128, zr + i * 128))
                    gi = 0
                    for ti, (t0, N) in enumerate(tiles(nblk, 512)):
                        nb = N // 128
                        S.dma("sp", h[:, :, 0:N], hT3[:, :, t0:t0 + N], writes=[hB])
                        S.op("act", lambda e: e.activation(out=xn[:, :, 0:N], in_=h[:, :, 0:N], func=AF.Square),
                             reads=[hB], writes=[xnB])
                        for c in range(8):
                            S.op("pe", lambda e: e.matmul(ps[:, 7, 0:N], lhsT=onesb[:], rhs=xn[:, c, 0:N], start=(c == 0), stop=(c == 7)),
                                 reads=[onesB, xnB], writes=[pb[7]])
                        S.op("act", lambda e: e.activation(out=rt[:, 0:N], in_=ps[:, 7, 0:N], func=AF.Ln, bias=epsc[:], scale=1.0 / D),
                             reads=[pb[7], epsB], writes=[rtB])
                        S.op("act", lambda e: e.activation(out=rs[:, 0:N], in_=rt[:, 0:N], func=AF.Exp, scale=-0.5), reads=[rtB], writes=[rsB])
                        for c in range(8):
                            S.op("dve", lambda e: e.scalar_tensor_tensor(out=xn[:, c, 0:N], in0=h[:, c, 0:N], scalar=col(l, C_N1 + c),
                                                                         in1=rs[:, 0:N], op0=ALU.mult, op1=ALU.mult),
                                 reads=[hB, colsB, rsB], writes=[xnB])
                        for (wc, zr) in groups:
                            b = gi % 4
                            z, zB = zs[gi % 4]
                            gi += 1
                            for c in range(8):
                                S.op("pe", lambda e: e.matmul(ps[:, b, 0:N], lhsT=wb[:, c, wc:wc + 128], rhs=xn[:, c, 0:N], start=(c == 0), stop=(c == 7)),
                                     reads=[wbB, xnB], writes=[pb[b]])
                            copy_op(evac_eng(), z[:, 0:N], ps[:, b, 0:N], [pb[b]], [zB])
                            S.dma("pool", zT[zr:zr + 128, t0:t0 + N], z[:, 0:N], reads=[zB])
                        for c in range(8):
                            S.op("pe", lambda e: e.matmul(ps[0:64, 4, 0:N], lhsT=wb[:, c, CKA_:CKA_ + 64], rhs=xn[:, c, 0:N], start=(c == 0), stop=(c == 7)),
                                 reads=[wbB, xnB], writes=[pb[4]])
                        S.op("act", lambda e: e.activation(out=kraw[:, 0:N], in_=ps[0:64, 4, 0:N], func=AF.Copy), reads=[pb[4]], writes=[krawB])
                        S.op("act", lambda e: e.activation(out=ksq[:, 0:N], in_=kraw[:, 0:N], func=AF.Square), reads=[krawB], writes=[ksqB])
                        S.op("pe", lambda e: e.matmul(ps[0:64, 5, 0:N], lhsT=onesb[0:64, 0:64], rhs=ksq[:, 0:N], start=True, stop=True),
                             reads=[onesB, ksqB], writes=[pb[5]])
                        S.op("act", lambda e: e.activation(out=rt[0:64, 0:N], in_=ps[0:64, 5, 0:N], func=AF.Ln, bias=epsc[0:64, :], scale=1.0 / 64),
                             reads=[pb[5], epsB], writes=[rtB])
                        S.op("act", lambda e: e.activation(out=rs[0:64, 0:N], in_=rt[0:64, 0:N], func=AF.Exp, scale=-0.5), reads=[rtB], writes=[rsB])
                        S.op("dve", lambda e: e.scalar_tensor_tensor(out=kT[:, t0:t0 + N], in0=kraw[:, 0:N], scalar=col(l, C_KG, 1, 64),
                                                                     in1=rs[0:64, 0:N], op0=ALU.mult, op1=ALU.mult),
                             reads=[krawB, colsB, rsB], writes=[kTB])
                        for c in range(8):
                            S.op("pe", lambda e: e.matmul(ps[0:64, 6, 0:N], lhsT=wb[:, c, CKI_:CKI_ + 64], rhs=xn[:, c, 0:N], start=(c == 0), stop=(c == 7)),
                                 reads=[wbB, xnB], writes=[pb[6]])
                        S.op("act", lambda e: e.activation(out=kiT[:, t0:t0 + N], in_=ps[0:64, 6, 0:N], func=AF.Copy), reads=[pb[6]], writes=[kiTB])
                        for j in range(nb):
                            blk = t0 // 128 + j
                            b = gi % 4
                            vs, vsB = vst[gi % 2]
                            gi += 1
                            for c in range(8):
                                S.op("pe", lambda e: e.matmul(ps[:, b, :], lhsT=xn[:, c, j * 128:(j + 1) * 128], rhs=wb[:, c, CI_:CI_ + 512], start=(c == 0), stop=(c == 7)),
                                     reads=[wbB, xnB], writes=[pb[b]])
                            copy_op(evac_eng(), vs[:], ps[:, b, :], [pb[b]], [vsB])
                            S.dma("pool", vhg[t0 + j * 128:t0 + (j + 1) * 128, :], vs[:], reads=[vsB])
                            b = gi % 4
                            gi += 1
                            for c in range(8):
                                S.op("pe", lambda e: e.matmul(ps[:, b, 0:64], lhsT=xn[:, c, j * 128:(j + 1) * 128], rhs=wb[:, c, CVA_:CVA_ + 64], start=(c == 0), stop=(c == 7)),
                                     reads=[wbB, xnB], writes=[pb[b]])
                            for c in range(8):
                                S.op("pe", lambda e: e.matmul(ps[:, b, 64:68], lhsT=xn[:, c, j * 128:(j + 1) * 128], rhs=wb[:, c, CWI_:CWI_ + 4], start=(c == 0), stop=(c == 7)),
                                     reads=[wbB, xnB], writes=[pb[b]])
                            S.op("act", lambda e: e.activation(out=Va[:, blk, 0:64], in_=ps[:, b, 0:64], func=AF.Copy), reads=[pb[b]], writes=[VaB])
                            S.op("dve", lambda e: e.tensor_scalar(out=Wx[:, blk, :], in0=ps[:, b, 64:68], scalar1=1.0 / 16, scalar2=None, op0=ALU.mult),
                                 reads=[pb[b]], writes=[WxB])
                    S.barrier()
                chk(f"A{l}")

                with ExitStack() as pes:
                    stg = [alloc(pes, f"stgB2{i}", [128, 512]) for i in range(2)]
                    wuq, wuqB = load_weight(pes, "wuq", w_uq[l].rearrange("(c p) n -> p c n", p=128), 128, 2, 512,
                                            [s_[0] for s_ in stg], [s_[1] for s_ in stg], 512)
                    wqi, wqiB = load_weight(pes, "wqi", w_qi[l].rearrange("(c p) n -> p c n", p=128), 128, 2, 256,
                                            [s_[0] for s_ in stg], [s_[1] for s_ in stg], 512)
                    LMAX = max(128, (nblk - 1) * 128)
                    I, IB = alloc(pes, "Isc", [128, LMAX])
                    mbs = [alloc(pes, f"mb{i}", [128, LMAX], BF16) for i in range(2)]
                    cq, cqB = alloc(pes, "cq", [128, 2, 512])
                    cqn, cqnB = alloc(pes, "cqn", [128, 2, 512], BF16)
                    rt, rtB = alloc(pes, "rtB2", [128, 512])
                    rs, rsB = alloc(pes, "rsB2", [128, 512])
                    qraw, qrawB = alloc(pes, "qraw", [64, 512])
                    qsq, qsqB = alloc(pes, "qsq", [64, 512], BF16)
                    qds = [alloc(pes, f"qd{i}", [64, 8, 512], BF16) for i in range(2)]
                    qid, qidB = alloc(pes, "qid", [64, 4, 512], BF16)
                    rb = [alloc(pes, f"rb{i}", [128, 512]) for i in range(2)]
                    pt = [alloc(pes, f"pt{i}", [128, 1024], BF16) for i in range(2)]
                    rden, rdenB = alloc(pes, "rden", [128, 1024])
                    rsh, rshB = alloc(pes, "rsh", [64, 1024])
                    aots = [alloc(pes, f"aot{i}", [64, 8, 512], BF16) for i in range(2)]
                    sm, smB = alloc(pes, "smB2", [128, 8])
                    steps, stepsB = alloc(pes, "steps", [128, NIT])
                    S.op("pool", lambda e: e.memset(rden[:], 0.0), writes=[rdenB])
                    st8 = {"bi": 0, "pti": 0, "lgi": 0}

                    def prep(t0, N, qd, qdB):
                        S.dma("sp", cq[:, :, 0:N], zT[ZCQ:ZCQ + 256, t0:t0 + N].rearrange("(c p) t -> p c t", p=128), writes=[cqB])
                        S.op("act", lambda e: e.activation(out=cqn[:, :, 0:N], in_=cq[:, :, 0:N], func=AF.Square), reads=[cqB], writes=[cqnB])
                        for c in range(2):
                            S.op("pe", lambda e: e.matmul(ps[:, 0, 0:N], lhsT=onesb[:], rhs=cqn[:, c, 0:N], start=(c == 0), stop=(c == 1)),
                                 reads=[onesB, cqnB], writes=[pb[0]])
                        S.op("act", lambda e: e.activation(out=rt[:, 0:N], in_=ps[:, 0, 0:N], func=AF.Ln, bias=epsc[:], scale=1.0 / 256),
                             reads=[pb[0], epsB], writes=[rtB])
                        S.op("act", lambda e: e.activation(out=rs[:, 0:N], in_=rt[:, 0:N], func=AF.Exp, scale=-0.5), reads=[rtB], writes=[rsB])
                        for c in range(2):
                            S.op("dve", lambda e: e.scalar_tensor_tensor(out=cqn[:, c, 0:N], in0=cq[:, c, 0:N], scalar=col(l, C_CQG + c),
                                                                         in1=rs[:, 0:N], op0=ALU.mult, op1=ALU.mult),
                                 reads=[cqB, colsB, rsB], writes=[cqnB])
                        for hh in range(8):
                            b = st8["bi"] % 2
                            st8["bi"] += 1
                            for c in range(2):
                                S.op("pe", lambda e: e.matmul(ps[0:64, b, 0:N], lhsT=wuq[:, c, hh * 64:(hh + 1) * 64], rhs=cqn[:, c, 0:N], start=(c == 0), stop=(c == 1)),
                                     reads=[wuqB, cqnB], writes=[pb[b]])
                            S.op("act", lambda e: e.activation(out=qraw[:, 0:N], in_=ps[0:64, b, 0:N], func=AF.Copy), reads=[pb[b]], writes=[qrawB])
                            S.op("act", lambda e: e.activation(out=qsq[:, 0:N], in_=qraw[:, 0:N], func=AF.Square), reads=[qrawB], writes=[qsqB])
                            S.op("pe", lambda e: e.matmul(ps[0:64, b, 0:N], lhsT=onesb[0:64, 0:64], rhs=qsq[:, 0:N], start=True, stop=True),
                                 reads=[onesB, qsqB], writes=[pb[b]])
                            S.op("act", lambda e: e.activation(out=rt[0:64, 0:N], in_=ps[0:64, b, 0:N], func=AF.Ln, bias=epsc[0:64, :], scale=1.0 / 64),
                                 reads=[pb[b], epsB], writes=[rtB])
                            S.op("act", lambda e: e.activation(out=rs[0:64, 0:N], in_=rt[0:64, 0:N], func=AF.Exp, scale=-0.5), reads=[rtB], writes=[rsB])
                            S.op("dve", lambda e: e.scalar_tensor_tensor(out=qd[:, hh, 0:N], in0=qraw[:, 0:N], scalar=col(l, C_QG, 1, 64),
                                                                         in1=rs[0:64, 0:N], op0=ALU.mult, op1=ALU.mult),
                                 reads=[qrawB, colsB, rsB], writes=[qdB])
                        for hh in range(4):
                            b = st8["bi"] % 2
                            st8["bi"] += 1
                            for c in range(2):
                                S.op("pe", lambda e: e.matmul(ps[0:64, b, 0:N], lhsT=wqi[:, c, hh * 64:(hh + 1) * 64], rhs=cqn[:, c, 0:N], start=(c == 0), stop=(c == 1)),
                                     reads=[wqiB, cqnB], writes=[pb[b]])
                            copy_op(evac_eng(), qid[:, hh, 0:N], ps[0:64, b, 0:N], [pb[b]], [qidB])

                    def do_index(bq, qc0, qc1):
                        L = 128 * bq
                        mb, mbB = mbs[bq % 2]
                        for st in range(0, L, 512):
                            w = min(512, L - st)
                            for hh in range(4):
                                b = st8["bi"] % 2
                                st8["bi"] += 1
                                r, rB = rb[st8["bi"] % 2]
                                S.op("pe", lambda e: e.matmul(ps[:, b, 0:w], lhsT=qid[:, hh, qc0:qc1], rhs=kiT[:, 128 + st:128 + st + w], start=True, stop=True),
                                     reads=[qidB, kiTB], writes=[pb[b]])
                                S.op("act", lambda e: e.activation(out=r[:, 0:w], in_=ps[:, b, 0:w], func=AF.Relu), reads=[pb[b]], writes=[rB])
                                if hh == 0:
                                    S.op("dve", lambda e: e.tensor_scalar(out=I[:, st:st + w], in0=r[:, 0:w], scalar1=Wx[:, bq, 0:1], scalar2=None, op0=ALU.mult),
                                         reads=[rB, WxB], writes=[IB])
                                else:
                                    S.op("dve", lambda e: e.scalar_tensor_tensor(out=I[:, st:st + w], in0=r[:, 0:w], scalar=Wx[:, bq, hh:hh + 1],
                                                                                 in1=I[:, st:st + w], op0=ALU.mult, op1=ALU.add),
                                         reads=[rB, WxB, IB], writes=[IB])
                        S.op("dve", lambda e: e.tensor_reduce(out=sm[:, 0:1], in_=I[:, 0:L], axis=AX.X, op=ALU.max, apply_absolute_value=True),
                             reads=[IB], writes=[smB])
                        S.op("dve", lambda e: e.tensor_tensor(out=I[:, L - 128:L], in0=I[:, L - 128:L], in1=cmask[:], op=ALU.add),
                             reads=[IB, cmaskB], writes=[IB])
                        S.op("dve", lambda e: e.tensor_scalar(out=sm[:, 1:2], in0=sm[:, 0:1], scalar1=-1.001, scalar2=-1e-6, op0=ALU.mult, op1=ALU.add),
                             reads=[smB], writes=[smB])
                        S.op("dve", lambda e: e.tensor_scalar(out=sm[:, 2:3], in0=sm[:, 0:1], scalar1=2.002, scalar2=2e-6, op0=ALU.mult, op1=ALU.add),
                             reads=[smB], writes=[smB])
                        S.op("dve", lambda e: e.tensor_scalar(out=steps[:], in0=p2t[:], scalar1=sm[:, 2:3], scalar2=None, op0=ALU.mult),
                             reads=[p2tB, smB], writes=[stepsB])
                        for it in range(NIT):
                            S.op("dve", lambda e: e.tensor_tensor(out=sm[:, 3:4], in0=sm[:, 1:2], in1=steps[:, it:it + 1], op=ALU.add),
                                 reads=[smB, stepsB], writes=[smB])
                            S.op("dve", lambda e: e.tensor_scalar(out=mb[:, 0:L], in0=I[:, 0:L], scalar1=sm[:, 3:4], scalar2=None,
                                                                  op0=ALU.is_ge, op1=ALU.add, accum_out=sm[:, 4:5]),
                                 reads=[IB, smB], writes=[mbB, smB])
                            S.op("dve", lambda e: e.tensor_scalar(out=sm[:, 5:6], in0=sm[:, 4:5], scalar1=KSEL - 0.5, scalar2=steps[:, it:it + 1],
                                                                  op0=ALU.is_ge, op1=ALU.mult),
                                 reads=[smB, stepsB], writes=[smB])
                            S.op("dve", lambda e: e.tensor_tensor(out=sm[:, 1:2], in0=sm[:, 1:2], in1=sm[:, 5:6], op=ALU.add),
                                 reads=[smB], writes=[smB])
                        S.op("dve", lambda e: e.tensor_scalar(out=mb[:, 0:L], in0=I[:, 0:L], scalar1=sm[:, 1:2], scalar2=NEG,
                                                              op0=ALU.is_lt, op1=ALU.mult),
                             reads=[IB, smB], writes=[mbB])

                    def do_attn(bq, qc0, qc1, qd, qdB, aot, aotB, store, t0, N):
                        mb, mbB = mbs[bq % 2]

                        def emit_qk(c):
                            lp = 2 + 2 * (st8["lgi"] % 2)
                            st8["lgi"] += 1
                            has_mask = (bq == 0) or (c >= 1)
                            for half in range(2):
                                S.op("pe", lambda e: e.matmul(ps[:, lp + half, :].rearrange("p (a b) -> p a b", b=128),
                                                              lhsT=kT[:, c * 128:(c + 1) * 128], rhs=qd[:, half * 4:half * 4 + 4, qc0:qc1],
                                                              start=True, stop=(not has_mask)),
                                     reads=[kTB, qdB], writes=[pb[lp + half]])
                                if has_mask:
                                    if bq == 0:
                                        S.op("pe", lambda e: e.matmul(ps[:, lp + half, :].rearrange("p (a b) -> p a b", b=128),
                                                                      lhsT=mb0[:], rhs=id4[:], start=False, stop=True),
                                             reads=[mb0B, id4B], writes=[pb[lp + half]])
                                    else:
                                        S.op("pe", lambda e: e.matmul(ps[:, lp + half, :].rearrange("p (a b) -> p a b", b=128),
                                                                      lhsT=mb[:, (c - 1) * 128:c * 128], rhs=id4[:], start=False, stop=True),
                                             reads=[mbB, id4B], writes=[pb[lp + half]])
                            return lp

                        lp_next = emit_qk(0)
                        for c in range(bq + 1):
                            lp = lp_next
                            if c + 1 <= bq:
                                lp_next = emit_qk(c + 1)
                            p, pB = pt[st8["pti"] % 2]
                            st8["pti"] += 1
                            if c == 0 and bq >= 1:
                                S.op("act", lambda e: e.activation(out=p[:].rearrange("p (a b) -> p a b", b=512), in_=ps[:, lp:lp + 2, :], func=AF.Exp, bias=padb[:]),
                                     reads=[pb[lp], pb[lp + 1], padbB], writes=[pB])
                            else:
                                S.op("act", lambda e: e.activation(out=p[:].rearrange("p (a b) -> p a b", b=512), in_=ps[:, lp:lp + 2, :], func=AF.Exp),
                                     reads=[pb[lp], pb[lp + 1]], writes=[pB])
                            for half in range(2):
                                S.op("pe", lambda e: e.matmul(ps[:, 6 + half, :], lhsT=Va[:, c, :], rhs=p[:, half * 512:(half + 1) * 512],
                                                              start=(c == 0), stop=(c == bq)),
                                     reads=[VaB, pB], writes=[pb[6 + half]])
                        lp = 2 + 2 * (st8["lgi"] % 2)
                        st8["lgi"] += 1
                        S.op("act", lambda e: e.activation(out=rden[64:128, :].rearrange("p (a b) -> p a b", b=512), in_=ps[64:128, 6:8, :], func=AF.Ln),
                             reads=[pb[6], pb[7]], writes=[rdenB])
                        S.op("act", lambda e: e.activation(out=rden[64:128, :], in_=rden[64:128, :], func=AF.Exp, scale=-1.0),
                             reads=[rdenB], writes=[rdenB])
                        for half in range(2):
                            S.op("pe", lambda e: e.matmul(ps[0:64, lp + half, :], lhsT=shf[:], rhs=rden[:, half * 512:(half + 1) * 512], start=True, stop=True),
                                 reads=[shfB, rdenB], writes=[pb[lp + half]])
                        S.op("act", lambda e: e.activation(out=rsh[:].rearrange("p (a b) -> p a b", b=512), in_=ps[0:64, lp:lp + 2, :], func=AF.Copy),
                             reads=[pb[lp], pb[lp + 1]], writes=[rshB])
                        for half in range(2):
                            S.op("dve", lambda e: e.tensor_tensor(out=aot[:, half * 4:half * 4 + 4, qc0:qc1],
                                                                  in0=ps[0:64, 6 + half, :].rearrange("p (a b) -> p a b", b=128),
                                                                  in1=rsh[:, half * 512:(half + 1) * 512].rearrange("p (a b) -> p a b", b=128), op=ALU.mult),
                                 reads=[pb[6 + half], rshB], writes=[aotB])
                        if store:
                            S.dma("pool", aos[:, :, t0:t0 + N], aot[:, :, 0:N], reads=[aotB])

                    pending = None
                    for ti, (t0, N) in enumerate(tiles(nblk, 512)):
                        nb = N // 128
                        qd_, qdB_ = qds[ti % 2]
                        aot_, aotB_ = aots[ti % 2]
                        prep(t0, N, qd_, qdB_)
                        for j in range(nb):
                            bq = t0 // 128 + j
                            if bq >= 1:
                                do_index(bq, j * 128, (j + 1) * 128)
                            if pending is not None:
                                pending()
                            pending = (lambda bq=bq, j=j, qd_=qd_, qdB_=qdB_, aot_=aot_, aotB_=aotB_, t0=t0, N=N, nb=nb:
                                       do_attn(bq, j * 128, (j + 1) * 128, qd_, qdB_, aot_, aotB_, j == nb - 1, t0, N))
                    pending()
                    S.barrier()
                chk(f"B2{l}")

            NB1 = 256
            with ExitStack() as pes:
                stg = [alloc(pes, f"stgB1{i}", [128, 1024]) for i in range(2)]
                sT = [s[0] for s in stg]
                sB = [s[1] for s in stg]
                whg, whgB = load_weight(pes, "whg", w_hg_out[l].rearrange("(c p) n -> p c n", p=128), 128, 4, D, sT, sB, 1024)
                wat, watB = load_weight(pes, "wat", w_at_out[l].rearrange("(c p) n -> p c n", p=64), 64, 8, D, sT, sB, 1024)
                wcv, wcvB = load_weight(pes, "wcv", w_cv_out[l].rearrange("(c p) n -> p c n", p=128), 128, 4, D, sT, sB, 1024)
                wmx, wmxB = load_weight(pes, "wmx", w_mix_out[l].rearrange("(c p) n -> p c n", p=128), 128, 8, D, sT, sB, 1024)
                Sst, SstB = alloc(pes, "Sst", [128, 4, 128])
                Sp, SpB = alloc(pes, "Sp", [128, 4, 128], BF16)
                SstBs = [Buf(f"Sst_h{i}") for i in range(4)]
                SpBs = [Buf(f"Sp_h{i}") for i in range(4)]
                qtlBs = [Buf(f"qtl_h{i}") for i in range(4)]
                ktlBs = [Buf(f"ktl_h{i}") for i in range(4)]
                eeBs = [Buf(f"ee_h{i}") for i in range(4)]
                pbq = [Buf(f"psq_h{i}") for i in range(4)]
                pbu = [Buf(f"psu_h{i}") for i in range(4)]
                HB, HBB = alloc(pes, "HB", [128, 4, 30 + NB1], BF16)
                dg, dgB = alloc(pes, "dg", [128, 124, 128], BF16)
                S.op("pool", lambda e: e.memset(Sst[:], 0.0), writes=[SstB] + SstBs)
                S.op("pool", lambda e: e.memset(HB[:], 0.0), writes=[HBB])
                for idx in range(124):
                    S.op("dve" if idx % 2 == 0 else "pool",
                         lambda e: e.tensor_scalar(out=dg[:, idx, :], in0=idf[:], scalar1=col(l, C_CVW + idx), scalar2=None, op0=ALU.mult),
                         reads=[idfB, colsB], writes=[dgB])
                fq, fqB = alloc(pes, "fq", [128, 3, 4, NB1])
                vt, vtB = alloc(pes, "vt", [64, NB1 // 64, 512], BF16)
                tA, tAB = alloc(pes, "tA", [128, NB1])
                tB_, tBB = alloc(pes, "tB", [128, NB1])
                tb, tbB = alloc(pes, "tb", [128, NB1])
                tbm, tbmB = alloc(pes, "tbm", [128, NB1])
                teq, teqB = alloc(pes, "teq", [128, NB1])
                tek, tekB = alloc(pes, "tek", [128, NB1])
                tqs, tqsB = alloc(pes, "tqs", [128, NB1])
                qtl, qtlB = alloc(pes, "qtl", [128, 4, NB1], BF16)
                ktl, ktlB = alloc(pes, "ktl", [128, 4, NB1], BF16)
                ee, eeB = alloc(pes, "ee", [128, 4, 3, NB1 // 64])
                ktok = [alloc(pes, f"ktok{i}", [64, 128], BF16) for i in range(4)]
                att = [alloc(pes, f"att{i}", [64, 64], BF16) for i in range(4)]
                osq, osqB = alloc(pes, "osq", [128, NB1], BF16)
                rt, rtB = alloc(pes, "rtB1", [128, NB1])
                rs, rsB = alloc(pes, "rsB1", [128, NB1])
                gs, gsB = alloc(pes, "gs", [128, NB1])
                on, onB = alloc(pes, "on", [128, NB1])
                oT, oTB = alloc(pes, "oT", [128, 4, NB1], BF16)
                u, uB = alloc(pes, "u", [128, 8, NB1])
                sgb, sgbB = alloc(pes, "sgb", [128, 4, NB1])
                acc, accB = alloc(pes, "acc", [128, 4, NB1])
                accb, accbB = alloc(pes, "accb", [128, 4, NB1], BF16)
                sqb, sqbB = alloc(pes, "sqb", [128, 4, NB1], BF16)
                mu, muB = alloc(pes, "mu", [128, NB1])
                var, varB = alloc(pes, "var", [128, NB1])
                cvo, cvoB = alloc(pes, "cvo", [128, 4, NB1], BF16)
                aot, aotB = alloc(pes, "aotB1", [64, 8, NB1], BF16)
                gl = [alloc(pes, f"gl{i}", [128, 3, NB1]) for i in range(2)]
                m1, m1B = alloc(pes, "m1", [128, NB1])
                m2, m2B = alloc(pes, "m2", [128, NB1])
                m3, m3B = alloc(pes, "m3", [128, NB1])
                mT, mTB = alloc(pes, "mT", [128, 8, NB1], BF16)
                h, hB = alloc(pes, "hB1", [128, 8, NB1])
                zgt = zT[ZGT:ZGT + 3072, :].rearrange("(br fo p) t -> p br fo t", br=3, fo=8, p=128)
                ki = 0
                gli = 0
                for ti, (t0, N) in enumerate(tiles(nblk, NB1)):
                    nch = N // 64
                    S.dma("sp", fq[:, 0, :, 0:N], zT[ZF:ZF + 512, t0:t0 + N].rearrange("(c p) t -> p c t", p=128), writes=[fqB])
                    S.dma("sp", fq[:, 1, :, 0:N], zT[ZQ:ZQ + 512, t0:t0 + N].rearrange("(c p) t -> p c t", p=128), writes=[fqB])
                    S.dma("sp", fq[:, 2, :, 0:N], zT[ZG:ZG + 512, t0:t0 + N].rearrange("(c p) t -> p c t", p=128), writes=[fqB])
                    S.dma("sp", vt[:, 0:nch, :], vhg[t0:t0 + N, :].rearrange("(c p) v -> p c v", p=64), writes=[vtB])
                    S.dma("sp", u[:, :, 0:N], zT[ZU:ZU + 1024, t0:t0 + N].rearrange("(c p) t -> p c t", p=128), writes=[uB])
                    S.dma("sp", aot[:, :, 0:N], aos[:, :, t0:t0 + N], writes=[aotB])
                    S.dma("sp", h[:, :, 0:N], hT3[:, :, t0:t0 + N], writes=[hB])
                    for hh in range(4):
                        qtlB_h, ktlB_h, eeB_h = qtlBs[hh], ktlBs[hh], eeBs[hh]
                        S.op("act", lambda e: e.activation(out=tA[:, 0:N], in_=fq[:, 0, hh, 0:N], func=AF.Sigmoid), reads=[fqB], writes=[tAB])
                        S.op("dve", lambda e: e.tensor_scalar(out=tA[:, 0:N], in0=tA[:, 0:N], scalar1=omlc[:, l, hh:hh + 1], scalar2=lbc[:, l, hh:hh + 1],
                                                              op0=ALU.mult, op1=ALU.add), reads=[tAB, omlB, lbB], writes=[tAB])
                        S.op("act", lambda e: e.activation(out=tB_[:, 0:N], in_=tA[:, 0:N], func=AF.Ln), reads=[tAB], writes=[tBB])
                        S.op("dve", lambda e: e.tensor_scalar(out=tA[:, 0:N], in0=tA[:, 0:N], scalar1=-1.0, scalar2=1.0, op0=ALU.mult, op1=ALU.add),
                             reads=[tAB], writes=[tAB])
                        S.op("dve", lambda e: e.tensor_tensor_scan(out=tb[:, 0:N], data0=rm[:, 0:N], data1=tB_[:, 0:N], initial=0.0, op0=ALU.mult, op1=ALU.add),
                             reads=[rmB, tBB], writes=[tbB])
                        b3 = tb[:, 0:N].rearrange("p (c k) -> p c k", k=64)
                        bmid = bass.AP(tb, 31, [[NB1, 128], [64, nch], [0, 64]])
                        S.op("dve", lambda e: e.tensor_tensor(out=tbm[:, 0:N].rearrange("p (c k) -> p c k", k=64), in0=b3, in1=bmid, op=ALU.subtract),
                             reads=[tbB], writes=[tbmB])
                        S.op("act", lambda e: e.activation(out=teq[:, 0:N], in_=tbm[:, 0:N], func=AF.Exp), reads=[tbmB], writes=[teqB])
                        S.op("act", lambda e: e.activation(out=tek[:, 0:N], in_=tbm[:, 0:N], func=AF.Exp, scale=-1.0), reads=[tbmB], writes=[tekB])
                        bm3 = tbm[:, 0:N].rearrange("p (c k) -> p c k", k=64)
                        S.op("act", lambda e: e.activation(out=ee[:, hh, 0, 0:nch], in_=b3[:, :, 31], func=AF.Exp), reads=[tbB], writes=[eeB_h])
                        S.op("act", lambda e: e.activation(out=ee[:, hh, 1, 0:nch], in_=b3[:, :, 63], func=AF.Exp), reads=[tbB], writes=[eeB_h])
                        S.op("act", lambda e: e.activation(out=ee[:, hh, 2, 0:nch], in_=bm3[:, :, 63], func=AF.Exp), reads=[tbmB], writes=[eeB_h])
                        S.op("act", lambda e: e.activation(out=tqs[:, 0:N], in_=fq[:, 1, hh, 0:N], func=AF.Silu), reads=[fqB], writes=[tqsB])
                        S.op("dve", lambda e: e.scalar_tensor_tensor(out=qtl[:, hh, 0:N], in0=tqs[:, 0:N], scalar=128.0 ** -0.5, in1=teq[:, 0:N], op0=ALU.mult, op1=ALU.mult),
                             reads=[tqsB, teqB], writes=[qtlB_h])
                        S.op("dve", lambda e: e.tensor_tensor(out=ktl[:, hh, 0:N], in0=tA[:, 0:N], in1=tek[:, 0:N], op=ALU.mult), reads=[tAB, tekB], writes=[ktlB_h])
                    for ch in range(nch):
                        c0, c1 = ch * 64, (ch + 1) * 64
                        for hh in range(4):
                            qtlB_h, ktlB_h, eeB_h, SstB_h, SpB_h = qtlBs[hh], ktlBs[hh], eeBs[hh], SstBs[hh], SpBs[hh]
                            kt_, ktB_ = ktok[hh]
                            at_, atB_ = att[hh]
                            tbk = 4 + hh % 2
                            psbf = ps[:, tbk, :].bitcast(BF16)
                            S.op("pe", lambda e: e.transpose(psbf[0:64, 0:128], ktl[:, hh, c0:c1], idb[:]), reads=[ktlB_h, idbB], writes=[pb[tbk]])
                            S.op("act", lambda e: e.activation(out=kt_[:], in_=psbf[0:64, 0:128], func=AF.Copy), reads=[pb[tbk]], writes=[ktB_])
                            S.op("pe", lambda e: e.matmul(ps[0:64, 6, hh * 64:(hh + 1) * 64], lhsT=ktl[:, hh, c0:c1], rhs=qtl[:, hh, c0:c1], start=True, stop=True),
                                 reads=[ktlB_h, qtlB_h], writes=[pbq[hh]] + ([pb[6]] if ch == 0 else []))
                            S.op("dve", lambda e: e.tensor_tensor(out=at_[:], in0=ps[0:64, 6, hh * 64:(hh + 1) * 64], in1=tri[:], op=ALU.mult), reads=[pbq[hh], triB], writes=[atB_])
                            S.op("dve", lambda e: e.tensor_scalar(out=Sp[:, hh, :], in0=Sst[:, hh, :], scalar1=ee[:, hh, 0, ch:ch + 1], scalar2=None, op0=ALU.mult),
                                 reads=[SstB_h, eeB_h], writes=[SpB_h])
                            S.op("pe", lambda e: e.matmul(ps[:, hh, c0:c1], lhsT=vt[:, ch, hh * 128:(hh + 1) * 128], rhs=at_[:], start=True, stop=False),
                                 reads=[vtB, atB_], writes=[pb[hh]])
                            S.op("pe", lambda e: e.matmul(ps[:, hh, c0:c1], lhsT=Sp[:, hh, :], rhs=qtl[:, hh, c0:c1], start=False, stop=True),
                                 reads=[SpB_h, qtlB_h], writes=[pb[hh]])
                            S.op("pe", lambda e: e.matmul(ps[:, 7, hh * 128:(hh + 1) * 128], lhsT=kt_[:], rhs=vt[:, ch, hh * 128:(hh + 1) * 128], start=True, stop=True),
                                 reads=[ktB_, vtB], writes=[pbu[hh]] + ([pb[7]] if ch == 0 else []))
                            S.op("dve", lambda e: e.tensor_scalar(out=Sst[:, hh, :], in0=Sst[:, hh, :], scalar1=ee[:, hh, 1, ch:ch + 1], scalar2=None, op0=ALU.mult),
                                 reads=[SstB_h, eeB_h], writes=[SstB_h])
                            S.op("dve", lambda e: e.scalar_tensor_tensor(out=Sst[:, hh, :], in0=ps[:, 7, hh * 128:(hh + 1) * 128], scalar=ee[:, hh, 2, ch:ch + 1], in1=Sst[:, hh, :],
                                                                         op0=ALU.mult, op1=ALU.add), reads=[pbu[hh], eeB_h, SstB_h], writes=[SstB_h])
                    for hh in range(4):
                        sb_ = 4 + hh % 2
                        S.op("act", lambda e: e.activation(out=osq[:, 0:N], in_=ps[:, hh, 0:N], func=AF.Square), reads=[pb[hh]], writes=[osqB])
                        S.op("pe", lambda e: e.matmul(ps[:, sb_, 0:N], lhsT=onesb[:], rhs=osq[:, 0:N], start=True, stop=True), reads=[onesB, osqB], writes=[pb[sb_]])
                        S.op("act", lambda e: e.activation(out=rt[:, 0:N], in_=ps[:, sb_, 0:N], func=AF.Ln, bias=epsc[:], scale=1.0 / 128),
                             reads=[pb[sb_], epsB], writes=[rtB])
                        S.op("act", lambda e: e.activation(out=rs[:, 0:N], in_=rt[:, 0:N], func=AF.Exp, scale=-0.5), reads=[rtB], writes=[rsB])
                        S.op("act", lambda e: e.activation(out=gs[:, 0:N], in_=fq[:, 2, hh, 0:N], func=AF.Silu), reads=[fqB], writes=[gsB])
                        S.op("dve", lambda e: e.scalar_tensor_tensor(out=on[:, 0:N], in0=ps[:, hh, 0:N], scalar=col(l, C_HGG), in1=rs[:, 0:N], op0=ALU.mult, op1=ALU.mult),
                             reads=[pb[hh], colsB, rsB], writes=[onB])
                        S.op("dve", lambda e: e.tensor_tensor(out=oT[:, hh, 0:N], in0=on[:, 0:N], in1=gs[:, 0:N], op=ALU.mult), reads=[onB, gsB], writes=[oTB])
                    S.op("act", lambda e: e.activation(out=sgb[:, :, 0:N], in_=u[:, 4:8, 0:N], func=AF.Sigmoid), reads=[uB], writes=[sgbB])
                    S.op("dve", lambda e: e.tensor_tensor(out=HB[:, :, 30:30 + N], in0=u[:, 0:4, 0:N], in1=sgb[:, :, 0:N], op=ALU.mult),
                         reads=[uB, sgbB], writes=[HBB])
                    for c in range(4):
                        cbk = 6 + c % 2
                        for j in range(31):
                            S.op("pe", lambda e: e.matmul(ps[:, cbk, 0:N], lhsT=dg[:, j * 4 + c, :], rhs=HB[:, c, j:j + N], start=(j == 0), stop=(j == 30)),
                                 reads=[dgB, HBB], writes=[pb[cbk]] + (pbq if cbk == 6 else pbu))
                        S.op("act", lambda e: e.activation(out=acc[:, c, 0:N], in_=ps[:, cbk, 0:N], func=AF.Identity, bias=col(l, C_CVB + c)),
                             reads=[pb[cbk], colsB], writes=[accB])
                    S.op("dve", lambda e: e.tensor_copy(out=HB[:, :, 0:30], in_=HB[:, :, N:N + 30]), reads=[HBB], writes=[HBB])
                    S.op("act", lambda e: e.activation(out=accb[:, :, 0:N], in_=acc[:, :, 0:N], func=AF.Copy), reads=[accB], writes=[accbB])
                    S.op("act", lambda e: e.activation(out=sqb[:, :, 0:N], in_=acc[:, :, 0:N], func=AF.Square), reads=[accB], writes=[sqbB])
                    for c in range(4):
                        S.op("pe", lambda e: e.matmul(ps[:, 6, 0:N], lhsT=onesb[:], rhs=accb[:, c, 0:N], start=(c == 0), stop=(c == 3)), reads=[onesB, accbB], writes=[pb[6]])
                    for c in range(4):
                        S.op("pe", lambda e: e.matmul(ps[:, 7, 0:N], lhsT=onesb[:], rhs=sqb[:, c, 0:N], start=(c == 0), stop=(c == 3)), reads=[onesB, sqbB], writes=[pb[7]])
                    S.op("act", lambda e: e.activation(out=mu[:, 0:N], in_=ps[:, 6, 0:N], func=AF.Copy, scale=1.0 / 512), reads=[pb[6]], writes=[muB])
                    S.op("dve", lambda e: e.tensor_tensor(out=var[:, 0:N], in0=mu[:, 0:N], in1=mu[:, 0:N], op=ALU.mult), reads=[muB], writes=[varB])
                    S.op("dve", lambda e: e.scalar_tensor_tensor(out=var[:, 0:N], in0=ps[:, 7, 0:N], scalar=1.0 / 512, in1=var[:, 0:N], op0=ALU.mult, op1=ALU.subtract),
                         reads=[pb[7], varB], writes=[varB])
                    S.op("act", lambda e: e.activation(out=rt[:, 0:N], in_=var[:, 0:N], func=AF.Ln, bias=epsc[:]), reads=[varB, epsB], writes=[rtB])
                    S.op("act", lambda e: e.activation(out=rs[:, 0:N], in_=rt[:, 0:N], func=AF.Exp, scale=-0.5), reads=[rtB], writes=[rsB])
                    for c in range(4):
                        S.op("dve", lambda e: e.tensor_tensor(out=acc[:, c, 0:N], in0=acc[:, c, 0:N], in1=mu[:, 0:N], op=ALU.subtract), reads=[accB, muB], writes=[accB])
                        S.op("dve", lambda e: e.tensor_tensor(out=acc[:, c, 0:N], in0=acc[:, c, 0:N], in1=rs[:, 0:N], op=ALU.mult), reads=[accB, rsB], writes=[accB])
                        S.op("act", lambda e: e.activation(out=cvo[:, c, 0:N], in_=acc[:, c, 0:N], func=AF.Silu, scale=col(l, C_LNG + c), bias=col(l, C_LNB + c)),
                             reads=[accB, colsB], writes=[cvoB])
                    for fo in range(8):
                        g_, gB_ = gl[gli % 2]
                        gli += 1
                        f0, f1 = fo * 128, (fo + 1) * 128
                        S.dma("sp", g_[:, :, 0:N], zgt[:, :, fo, t0:t0 + N], writes=[gB_])
                        S.op("act", lambda e: e.activation(out=g_[:, :, 0:N], in_=g_[:, :, 0:N], func=AF.Sigmoid), reads=[gB_], writes=[gB_])
                        for c in range(4):
                            S.op("pe", lambda e: e.matmul(ps[:, 0, 0:N], lhsT=whg[:, c, f0:f1], rhs=oT[:, c, 0:N], start=(c == 0), stop=(c == 3)), reads=[whgB, oTB], writes=[pb[0]])
                        for c in range(8):
                            S.op("pe", lambda e: e.matmul(ps[:, 1, 0:N], lhsT=wat[:, c, f0:f1], rhs=aot[:, c, 0:N], start=(c == 0), stop=(c == 7)), reads=[watB, aotB], writes=[pb[1]])
                        for c in range(4):
                            S.op("pe", lambda e: e.matmul(ps[:, 2, 0:N], lhsT=wcv[:, c, f0:f1], rhs=cvo[:, c, 0:N], start=(c == 0), stop=(c == 3)), reads=[wcvB, cvoB], writes=[pb[2]])
                        S.op("dve", lambda e: e.tensor_tensor(out=m1[:, 0:N], in0=ps[:, 0, 0:N], in1=g_[:, 0, 0:N], op=ALU.mult), reads=[pb[0], gB_], writes=[m1B])
                        S.op("dve", lambda e: e.tensor_tensor(out=m2[:, 0:N], in0=ps[:, 1, 0:N], in1=g_[:, 1, 0:N], op=ALU.mult), reads=[pb[1], gB_], writes=[m2B])
                        S.op("dve", lambda e: e.tensor_tensor(out=m3[:, 0:N], in0=ps[:, 2, 0:N], in1=g_[:, 2, 0:N], op=ALU.mult), reads=[pb[2], gB_], writes=[m3B])
                        S.op("pool", lambda e: e.tensor_tensor(out=m1[:, 0:N], in0=m1[:, 0:N], in1=m2[:, 0:N], op=ALU.add), reads=[m1B, m2B], writes=[m1B])
                        S.op("pool", lambda e: e.tensor_tensor(out=mT[:, fo, 0:N], in0=m1[:, 0:N], in1=m3[:, 0:N], op=ALU.add), reads=[m1B, m3B], writes=[mTB])
                    for fo in range(8):
                        b = 3 + fo % 2
                        for c in range(8):
                            S.op("pe", lambda e: e.matmul(ps[:, b, 0:N], lhsT=wmx[:, c, fo * 128:(fo + 1) * 128], rhs=mT[:, c, 0:N], start=(c == 0), stop=(c == 7)),
                                 reads=[wmxB, mTB], writes=[pb[b]])
                        S.op("dve", lambda e: e.tensor_tensor(out=h[:, fo, 0:N], in0=h[:, fo, 0:N], in1=ps[:, b, 0:N], op=ALU.add), reads=[hB, pb[b]], writes=[hB])
                    if ti == 0:
                        S.op("pool", lambda e: e.memset(h[:, :, 0:112], 0.0), reads=[hB], writes=[hB])
                    S.dma("pool", hT3[:, :, t0:t0 + N], h[:, :, 0:N], reads=[hB])
                S.barrier()
            chk(f"B1{l}")

            NC_ = 256
            with ExitStack() as pes:
                stg = [alloc(pes, f"stgC{i}", [128, 1408]) for i in range(2)]
                sT = [s[0] for s in stg]
                sB = [s[1] for s in stg]
                wup, wupB = load_weight(pes, "wup", w_ffn_up[l].rearrange("(c p) n -> p c n", p=128), 128, 8, 2 * FF, sT, sB, 1408)
                wdn, wdnB = load_weight(pes, "wdn", w_ffn_down[l].rearrange("(c p) n -> p c n", p=128), 128, 22, D, sT, sB, 1024)
                h, hB = alloc(pes, "hC", [128, 8, NC_])
                xn, xnB = alloc(pes, "xnC", [128, 8, NC_], BF16)
                rt, rtB = alloc(pes, "rtC", [128, NC_])
                rs, rsB = alloc(pes, "rsC", [128, NC_])
                xa = [alloc(pes, f"xa{i}", [128, 2, 2 + NC_]) for i in range(3)]
                ya = [alloc(pes, f"ya{i}", [128, 2, NC_]) for i in range(3)]
                sas = [alloc(pes, f"sa{i}", [128, NC_]) for i in range(2)]
                gT, gTB = alloc(pes, "gT", [128, 22, NC_], BF16)
                hist, histB = alloc(pes, "hist", [128, 44, 2])
                ot = [alloc(pes, f"ot{i}", [128, D]) for i in range(2)]
                S.op("pool", lambda e: e.memset(hist[:], 0.0), writes=[histB])
                pi = 0
                oi = 0
                for ti, (t0, N) in enumerate(tiles(nblk, NC_)):
                    S.dma("sp", h[:, :, 0:N], hT3[:, :, t0:t0 + N], writes=[hB])
                    S.op("act", lambda e: e.activation(out=xn[:, :, 0:N], in_=h[:, :, 0:N], func=AF.Square), reads=[hB], writes=[xnB])
                    for c in range(8):
                        S.op("pe", lambda e: e.matmul(ps[:, 7, 0:N], lhsT=onesb[:], rhs=xn[:, c, 0:N], start=(c == 0), stop=(c == 7)), reads=[onesB, xnB], writes=[pb[7]])
                    S.op("act", lambda e: e.activation(out=rt[:, 0:N], in_=ps[:, 7, 0:N], func=AF.Ln, bias=epsc[:], scale=1.0 / D), reads=[pb[7], epsB], writes=[rtB])
                    S.op("act", lambda e: e.activation(out=rs[:, 0:N], in_=rt[:, 0:N], func=AF.Exp, scale=-0.5), reads=[rtB], writes=[rsB])
                    for c in range(8):
                        S.op("dve", lambda e: e.scalar_tensor_tensor(out=xn[:, c, 0:N], in0=h[:, c, 0:N], scalar=col(l, C_N2 + c), in1=rs[:, 0:N], op0=ALU.mult, op1=ALU.mult),
                             reads=[hB, colsB, rsB], writes=[xnB])
                    pend = None
                    for j in range(22):
                        xa_, xaB_ = xa[pi % 3]
                        ya_, yaB_ = ya[pi % 3]
                        b0 = (pi % 3) * 2
                        pi += 1
                        for half in range(2):
                            wc = half * FF + j * 128
                            for c in range(8):
                                S.op("pe", lambda e: e.matmul(ps[:, b0 + half, 0:N], lhsT=wup[:, c, wc:wc + 128], rhs=xn[:, c, 0:N], start=(c == 0), stop=(c == 7)),
                                     reads=[wupB, xnB], writes=[pb[b0 + half]])
                        for half in range(2):
                            cj = half * 22 + j
                            S.op("act", lambda e: e.activation(out=xa_[:, half, 2:2 + N], in_=ps[:, b0 + half, 0:N], func=AF.Copy), reads=[pb[b0 + half]], writes=[xaB_])
                            S.op("act", lambda e: e.activation(out=ya_[:, half, 0:N], in_=ps[:, b0 + half, 0:N], func=AF.Identity,
                                                               scale=col(l, C_FW + 88 + cj), bias=col(l, C_FB + cj)), reads=[pb[b0 + half], colsB], writes=[yaB_])
                            S.op("pool", lambda e: e.tensor_copy(out=xa_[:, half, 0:2], in_=hist[:, cj, :]), reads=[histB], writes=[xaB_])
                            S.op("pool", lambda e: e.tensor_copy(out=hist[:, cj, :], in_=xa_[:, half, N:N + 2]), reads=[xaB_], writes=[histB])
                        if pend is not None:
                            pend()
                        for half in range(2):
                            cj = half * 22 + j
                            S.op("dve", lambda e: e.scalar_tensor_tensor(out=ya_[:, half, 0:N], in0=xa_[:, half, 1:1 + N], scalar=col(l, C_FW + 44 + cj), in1=ya_[:, half, 0:N],
                                                                         op0=ALU.mult, op1=ALU.add), reads=[xaB_, colsB, yaB_], writes=[yaB_])
                            S.op("dve", lambda e: e.scalar_tensor_tensor(out=ya_[:, half, 0:N], in0=xa_[:, half, 0:N], scalar=col(l, C_FW + cj), in1=ya_[:, half, 0:N],
                                                                         op0=ALU.mult, op1=ALU.add), reads=[xaB_, colsB, yaB_], writes=[yaB_])

                        def pend(j=j, ya_=ya_, yaB_=yaB_):
                            sa_, saB_ = sas[j % 2]
                            S.op("act", lambda e: e.activation(out=sa_[:, 0:N], in_=ya_[:, 0, 0:N], func=AF.Silu), reads=[yaB_], writes=[saB_])
                            S.op("pool", lambda e: e.tensor_tensor(out=gT[:, j, 0:N], in0=sa_[:, 0:N], in1=ya_[:, 1, 0:N], op=ALU.mult), reads=[saB_, yaB_], writes=[gTB])
                    pend()
                    for fo in range(8):
                        b = 6 + fo % 2
                        for c in range(22):
                            S.op("pe", lambda e: e.matmul(ps[:, b, 0:N], lhsT=wdn[:, c, fo * 128:(fo + 1) * 128], rhs=gT[:, c, 0:N], start=(c == 0), stop=(c == 21)),
                                 reads=[wdnB, gTB], writes=[pb[b]])
                        S.op("dve", lambda e: e.tensor_tensor(out=h[:, fo, 0:N], in0=h[:, fo, 0:N], in1=ps[:, b, 0:N], op=ALU.add), reads=[hB, pb[b]], writes=[hB])
                    if not last:
                        if ti == 0:
                            S.op("pool", lambda e: e.memset(h[:, :, 0:112], 0.0), reads=[hB], writes=[hB])
                        S.dma("pool", hT3[:, :, t0:t0 + N], h[:, :, 0:N], reads=[hB])
                    elif ti > 0:
                        for tbk in range(N // 128):
                            o_, oB_ = ot[oi % 2]
                            oi += 1
                            for fo in range(8):
                                S.op("pe", lambda e: e.transpose(ps[:, 6 + fo // 4, (fo % 4) * 128:(fo % 4 + 1) * 128], h[:, fo, tbk * 128:(tbk + 1) * 128], idf[:]),
                                     reads=[hB, idfB], writes=[pb[6 + fo // 4]])
                            S.op("act", lambda e: e.activation(out=o_[:].rearrange("p (a b) -> p a b", b=512), in_=ps[:, 6:8, :], func=AF.Copy),
                                 reads=[pb[6], pb[7]], writes=[oB_])
                            r0 = t0 - 128 + tbk * 128
                            S.dma("pool", y[r0:r0 + 128, :], o_[:], reads=[oB_])
                S.barrier()
            chk(f"C{l}")
        print("n_inst", S.n_inst, "nsem", S.nsem)
    except _Stop:
        pass
    return nc


_NAMES = ["meta_tokens", "hgrn_lb", "norm1_g", "w_in", "hg_norm_g", "w_hg_out", "cq_norm_g", "w_uq", "w_qi",
          "q_norm_g", "k_norm_g", "w_at_out", "cv_dw_w", "cv_dw_b", "cv_ln_g", "cv_ln_b", "w_cv_out", "w_mix_out",
          "norm2_g", "w_ffn_up", "ffn_dw_w", "ffn_dw_b", "w_ffn_down"]


def kernel(_nblk=65, _stop=None, _ncores=None, **inputs):
    x = np.asarray(inputs["x"], dtype=np.float32)
    B = x.shape[0] if _ncores is None else _ncores
    nx = (_nblk - 1) * 128
    nc = build(_nblk, stop=_stop)
    shared = {k: np.ascontiguousarray(np.asarray(inputs[k], dtype=np.float32)) for k in _NAMES}
    in_maps = []
    for b in range(B):
        m = dict(shared)
        m["x"] = np.ascontiguousarray(x[b, :nx])
        in_maps.append(m)
    res = run_bass_kernel_spmd(nc, in_maps, core_ids=list(range(B)))
    return np.stack([np.asarray(r["y"], dtype=np.float32) for r in res.results], axis=0)
```
